# Optimizing a Trainium2 kernel written in Bass

```python
import math
import jax
import jax.numpy as jnp
from jax import lax
import numpy as np

D_MODEL = 4096
BATCH = 4
SEQ = 4096
DEPTH = 1

CTX_LEN = 256
GRID_W = 64

RMS_EPS = 1e-6
N_MOD = 6

RWKV_WIDTH = D_MODEL // 2
RWKV_HEAD = 64
RWKV_HEADS = RWKV_WIDTH // RWKV_HEAD
DECAY_RANK = 96
ICL_RANK = 96
GATE_RANK = 256
RWKV_GN_EPS = 64e-5
RWKV_COLS = 3 * RWKV_WIDTH + 2 * DECAY_RANK + 2 * ICL_RANK + GATE_RANK
RWKV_SPLITS = (RWKV_WIDTH, 2 * RWKV_WIDTH, 3 * RWKV_WIDTH,
               3 * RWKV_WIDTH + 2 * DECAY_RANK,
               3 * RWKV_WIDTH + 2 * DECAY_RANK + 2 * ICL_RANK)

SSM_WIDTH = D_MODEL - RWKV_WIDTH
SSM_HEAD = 64
SSM_HEADS = SSM_WIDTH // SSM_HEAD
SSM_GROUPS = 8
SSM_STATE = 128
SSM_CONV = 3
SSM_CHUNK = 128
SSM_XBC = SSM_WIDTH + 2 * SSM_GROUPS * SSM_STATE
SSM_COLS = SSM_WIDTH + SSM_XBC + 2 * SSM_HEADS

IN_COLS = RWKV_COLS + SSM_COLS

PEER_HEADS = 8
PEER_KEYS = 128
PEER_EXPERTS = PEER_KEYS * PEER_KEYS
PEER_QDIM = 256
PEER_TOPK = 16
PEER_BLOCK = 64

kernel_name = "hybrid_rwkv7_ssd_peer_dit_block"


def rmsnorm(h, g):
    hf = h.astype(jnp.float32)
    hf = hf * lax.rsqrt(jnp.mean(hf * hf, axis=-1, keepdims=True) + RMS_EPS)
    return hf.astype(h.dtype) * g


def modulate(h, g, shift, scale):
    return rmsnorm(h, g) * (1 + scale) + shift


def stack_dirs(fwd, bwd):
    return jnp.concatenate([fwd, jnp.flip(bwd, axis=1)], axis=0)


def unstack_dirs(y):
    b = y.shape[0] // 2
    return y[:b] + jnp.flip(y[b:], axis=1)


def grid_token_shift(p, rows):
    b, t, ch = p.shape
    q = p.reshape(b, rows, GRID_W, ch // 4, 4)
    left = jnp.pad(q[:, :, :-1, :, 0], ((0, 0), (0, 0), (1, 0), (0, 0)))
    right = jnp.pad(q[:, :, 1:, :, 1], ((0, 0), (0, 0), (0, 1), (0, 0)))
    up = jnp.pad(q[:, :-1, :, :, 2], ((0, 0), (1, 0), (0, 0), (0, 0)))
    down = jnp.pad(q[:, 1:, :, :, 3], ((0, 0), (0, 1), (0, 0), (0, 0)))
    return jnp.stack([left, right, up, down], axis=-1).reshape(b, t, ch)


def seq_token_shift(p):
    b, t, ch = p.shape
    q = p.reshape(b, t, ch // 2, 2)
    prev = jnp.pad(q[:, :-1, :, 0], ((0, 0), (1, 0), (0, 0)))
    nxt = jnp.pad(q[:, 1:, :, 1], ((0, 0), (0, 1), (0, 0)))
    return jnp.stack([prev, nxt], axis=-1).reshape(b, t, ch)


def rwkv_prepare(cols, shifted, lp):
    b, t, _ = cols.shape
    xm = cols + lp["rwkv_mu"] * (shifted - cols)
    r, k, v, w_lr, a_lr, g_lr = jnp.split(xm, RWKV_SPLITS, axis=-1)
    w_lr = w_lr.reshape(b, t, 2, DECAY_RANK)
    a_lr = a_lr.reshape(b, t, 2, ICL_RANK)
    w = -jax.nn.softplus(-(lp["rwkv_w0"] + jnp.einsum("btdr,drc->btdc", jnp.tanh(w_lr), lp["rwkv_w2"]))) - 0.5
    decay = jnp.exp(-jnp.exp(w))
    a = jax.nn.sigmoid(lp["rwkv_a0"] + jnp.einsum("btdr,drc->btdc", a_lr, lp["rwkv_a2"]))
    g = jax.nn.sigmoid(g_lr) @ lp["rwkv_g2"]
    k_dir = k[:, :, None, :] * (1 + (a - 1) * lp["rwkv_k_a"])
    heads = lambda z: z.reshape(*z.shape[:-1], RWKV_HEADS, RWKV_HEAD)
    r_h, v_h, decay_h, a_h, k_h = heads(r), heads(v), heads(decay), heads(a), heads(k_dir)
    kk = heads(k * lp["rwkv_k_k"]).astype(jnp.float32)
    kk = (kk * lax.rsqrt(jnp.maximum(jnp.sum(kk * kk, axis=-1, keepdims=True), 1e-24))).astype(k.dtype)
    scan_in = (stack_dirs(r_h, r_h),
               stack_dirs(decay_h[:, :, 0], decay_h[:, :, 1]),
               stack_dirs(k_h[:, :, 0], k_h[:, :, 1]),
               stack_dirs(v_h, v_h),
               stack_dirs(-kk, -kk),
               stack_dirs(kk * a_h[:, :, 0], kk * a_h[:, :, 1]))
    bonus = jnp.einsum("bthn,btdhn,hn->bth", r_h, k_h, lp["rwkv_r_k"])[..., None] * v_h
    return scan_in, bonus, g


def rwkv7_scan(r, w, k, v, a, b, s0):
    def step(s, inp):
        r_t, w_t, k_t, v_t, a_t, b_t = inp
        sa = jnp.einsum("ghij,ghj->ghi", s, a_t)
        s = s * w_t[:, :, None, :] + sa[..., None] * b_t[:, :, None, :] + v_t[..., None] * k_t[:, :, None, :]
        return s, jnp.einsum("ghij,ghj->ghi", s, r_t)
    xs = tuple(jnp.moveaxis(z, 1, 0) for z in (r, w, k, v, a, b))
    s_fin, out = lax.scan(step, s0, xs)
    return jnp.moveaxis(out, 0, 1), s_fin


def rwkv_finish(out_dirs, bonus, g, lp):
    o = unstack_dirs(out_dirs).astype(jnp.float32)
    mean = jnp.mean(o, axis=-1, keepdims=True)
    var = jnp.mean(jnp.square(o - mean), axis=-1, keepdims=True)
    o = ((o - mean) * lax.rsqrt(var + RWKV_GN_EPS)).astype(g.dtype)
    b, t = o.shape[:2]
    o = o.reshape(b, t, RWKV_WIDTH) * lp["rwkv_ln_w"] + lp["rwkv_ln_b"]
    return (o + bonus.reshape(b, t, RWKV_WIDTH)) * g


def centred_dwconv(x, w, bias):
    half = (w.shape[0] - 1) // 2
    y = lax.conv_general_dilated(x, w[:, None, :], window_strides=(1,), padding=[(half, half)],
                                 dimension_numbers=("NWC", "WIO", "NWC"),
                                 feature_group_count=x.shape[-1])
    return y + bias


def ssd_prepare(cols, lp):
    b, t, _ = cols.shape
    z, xbc, dt_raw = jnp.split(cols, (SSM_WIDTH, SSM_WIDTH + SSM_XBC), axis=-1)
    xbc = jax.nn.silu(centred_dwconv(xbc, lp["ssm_conv_w"], lp["ssm_conv_b"]))
    xs, bm, cm = jnp.split(xbc, (SSM_WIDTH, SSM_WIDTH + SSM_GROUPS * SSM_STATE), axis=-1)
    xs = xs.reshape(b, t, SSM_HEADS, SSM_HEAD)
    bm = bm.reshape(b, t, SSM_GROUPS, SSM_STATE)
    cm = cm.reshape(b, t, SSM_GROUPS, SSM_STATE)
    dt = jax.nn.softplus(dt_raw.reshape(b, t, 2, SSM_HEADS) + lp["ssm_dt_bias"])
    log_a = dt * -jnp.exp(lp["ssm_a_log"])
    xdt = xs[:, :, None] * dt[..., None]
    scan_in = (stack_dirs(xdt[:, :, 0], xdt[:, :, 1]),
               stack_dirs(log_a[:, :, 0], log_a[:, :, 1]),
               stack_dirs(bm, bm),
               stack_dirs(cm, cm))
    return scan_in, xs, z


def ssd_chunked(x, la, bm, cm, s0):
    gb, t, h, p = x.shape
    ng, n = bm.shape[2], bm.shape[3]
    r = h // ng
    L = SSM_CHUNK
    nc = t // L
    x = x.reshape(gb, nc, L, ng, r, p)
    la = la.reshape(gb, nc, L, ng, r)
    bm = bm.reshape(gb, nc, L, ng, n)
    cm = cm.reshape(gb, nc, L, ng, n)
    cum = jnp.cumsum(la.astype(jnp.float32), axis=2)
    lower = jnp.tril(jnp.ones((L, L), dtype=bool))[None, None, :, :, None, None]
    seg = cum[:, :, :, None] - cum[:, :, None, :]
    decay_ls = jnp.exp(jnp.where(lower, seg, -jnp.inf)).astype(x.dtype)
    cb = jnp.einsum("bclgn,bcsgn->bclsg", cm, bm)
    y_diag = jnp.einsum("bclsg,bclsgr,bcsgrp->bclgrp", cb, decay_ls, x)
    decay_end = jnp.exp(cum[:, :, -1:] - cum).astype(x.dtype)
    chunk_states = jnp.einsum("bclgn,bclgr,bclgrp->bcgrpn", bm, decay_end, x)
    chunk_decay = jnp.exp(cum[:, :, -1]).astype(x.dtype)

    def carry_state(s, inp):
        dec, st = inp
        return dec[..., None, None] * s + st, s

    s_fin, s_prev = lax.scan(carry_state, s0.reshape(gb, ng, r, p, n),
                             (jnp.moveaxis(chunk_decay, 1, 0), jnp.moveaxis(chunk_states, 1, 0)))
    s_prev = jnp.moveaxis(s_prev, 0, 1)
    y_off = jnp.einsum("bclgn,bcgrpn,bclgr->bclgrp", cm, s_prev, jnp.exp(cum).astype(x.dtype))
    y = (y_diag + y_off).reshape(gb, t, h, p)
    return y, s_fin.reshape(gb, h, p, n)


def ssd_finish(y_dirs, xs, z, lp):
    b, t = xs.shape[:2]
    y = unstack_dirs(y_dirs) + lp["ssm_d"][:, None] * xs
    y = y.reshape(b, t, SSM_WIDTH) * jax.nn.silu(z)
    yf = y.reshape(b, t, SSM_GROUPS, SSM_WIDTH // SSM_GROUPS).astype(jnp.float32)
    yf = yf * lax.rsqrt(jnp.mean(yf * yf, axis=-1, keepdims=True) + RMS_EPS)
    return yf.reshape(b, t, SSM_WIDTH).astype(y.dtype) * lp["ssm_norm_w"]


def hybrid_mixer(n_x, n_c, rows, lp, ctx_out):
    b = n_x.shape[0]
    p_x = n_x @ lp["w_in"]
    p_c = n_c @ lp["w_in"]
    rw_x, ss_x = p_x[..., :RWKV_COLS], p_x[..., RWKV_COLS:]
    rw_c, ss_c = p_c[..., :RWKV_COLS], p_c[..., RWKV_COLS:]
    rin_c, rb_c, rg_c = rwkv_prepare(rw_c, seq_token_shift(rw_c), lp)
    rin_x, rb_x, rg_x = rwkv_prepare(rw_x, grid_token_shift(rw_x, rows), lp)
    s0 = jnp.zeros((2 * b, RWKV_HEADS, RWKV_HEAD, RWKV_HEAD), n_x.dtype)
    ro_c, rs_c = rwkv7_scan(*rin_c, s0)
    ro_x, _ = rwkv7_scan(*rin_x, rs_c)
    sin_c, xs_c, z_c = ssd_prepare(ss_c, lp)
    sin_x, xs_x, z_x = ssd_prepare(ss_x, lp)
    h0 = jnp.zeros((2 * b, SSM_HEADS, SSM_HEAD, SSM_STATE), n_x.dtype)
    so_c, hs_c = ssd_chunked(*sin_c, h0)
    so_x, _ = ssd_chunked(*sin_x, hs_c)
    y_x = jnp.concatenate([rwkv_finish(ro_x, rb_x, rg_x, lp), ssd_finish(so_x, xs_x, z_x, lp)], axis=-1) @ lp["w_out"]
    if ctx_out:
        y_c = jnp.concatenate([rwkv_finish(ro_c, rb_c, rg_c, lp), ssd_finish(so_c, xs_c, z_c, lp)], axis=-1) @ lp["w_out"]
    else:
        y_c = None
    return y_x, y_c


def peer_ffn(xn, lp):
    b, t, d = xn.shape
    w_q, k1, k2, u_tab, v_tab = lp["peer_wq"], lp["peer_k1"], lp["peer_k2"], lp["peer_u"], lp["peer_v"]

    def block(xb):
        q = (xb @ w_q).reshape(PEER_BLOCK, PEER_HEADS, 2, PEER_QDIM // 2)
        s1 = jnp.einsum("thd,hkd->thk", q[:, :, 0], k1).astype(jnp.float32)
        s2 = jnp.einsum("thd,hkd->thk", q[:, :, 1], k2).astype(jnp.float32)
        v1, i1 = lax.top_k(s1, PEER_TOPK)
        v2, i2 = lax.top_k(s2, PEER_TOPK)
        cand = (v1[..., :, None] + v2[..., None, :]).reshape(PEER_BLOCK, PEER_HEADS, PEER_TOPK * PEER_TOPK)
        best, flat = lax.top_k(cand, PEER_TOPK)
        expert = (jnp.take_along_axis(i1, flat // PEER_TOPK, axis=-1) * PEER_KEYS
                  + jnp.take_along_axis(i2, flat % PEER_TOPK, axis=-1))
        gate = jax.nn.softmax(best, axis=-1).astype(xb.dtype)
        act = jax.nn.gelu(jnp.einsum("td,thkd->thk", xb, u_tab[expert]), approximate=False)
        return jnp.einsum("thk,thkd->td", gate * act, v_tab[expert])

    out = lax.map(block, xn.reshape(-1, PEER_BLOCK, d))
    return out.reshape(b, t, d)


def setup_inputs(seed: int = 0) -> dict:
    key = jax.random.key(seed)
    keys = list(jax.random.split(key, 40))
    nxt = lambda: keys.pop()
    nrm = lambda shape, s: jax.random.normal(nxt(), shape, jnp.float32) * s
    uni = lambda shape, lo, hi: jax.random.uniform(nxt(), shape, jnp.float32, minval=lo, maxval=hi)
    L, C = DEPTH, RWKV_WIDTH
    dt_init = jnp.exp(uni((L, 2, SSM_HEADS), math.log(1e-3), math.log(1e-1)))
    return {
        "x": nrm((BATCH, SEQ, D_MODEL), 1.0),
        "c": nrm((BATCH, D_MODEL), 1.0),
        "ctx": nrm((BATCH, CTX_LEN, D_MODEL), 1.0),
        "c_ctx": nrm((D_MODEL,), 1.0),
        "w_mod": nrm((L, D_MODEL, N_MOD * D_MODEL), 0.5 * D_MODEL ** -0.5),
        "b_mod": nrm((L, N_MOD * D_MODEL), 0.01),
        "norm1_g": 1.0 + nrm((L, D_MODEL), 0.02),
        "norm2_g": 1.0 + nrm((L, D_MODEL), 0.02),
        "w_in": nrm((L, D_MODEL, IN_COLS), D_MODEL ** -0.5),
        "w_out": nrm((L, D_MODEL, D_MODEL), D_MODEL ** -0.5),
        "rwkv_mu": uni((L, RWKV_COLS), 0.0, 1.0),
        "rwkv_w0": uni((L, 2, C), -5.0, -1.0),
        "rwkv_w2": nrm((L, 2, DECAY_RANK, C), 0.5 * DECAY_RANK ** -0.5),
        "rwkv_a0": nrm((L, 2, C), 0.1),
        "rwkv_a2": nrm((L, 2, ICL_RANK, C), 0.5 * ICL_RANK ** -0.5),
        "rwkv_g2": nrm((L, GATE_RANK, C), GATE_RANK ** -0.5),
        "rwkv_k_k": 0.85 + nrm((L, C), 0.05),
        "rwkv_k_a": 1.0 + nrm((L, C), 0.05),
        "rwkv_r_k": nrm((L, RWKV_HEADS, RWKV_HEAD), 0.1),
        "rwkv_ln_w": 1.0 + nrm((L, C), 0.02),
        "rwkv_ln_b": nrm((L, C), 0.01),
        "ssm_conv_w": nrm((L, SSM_CONV, SSM_XBC), SSM_CONV ** -0.5),
        "ssm_conv_b": nrm((L, SSM_XBC), 0.01),
        "ssm_dt_bias": dt_init + jnp.log(-jnp.expm1(-dt_init)),
        "ssm_a_log": jnp.log(uni((L, 2, SSM_HEADS), 1.0, 16.0)),
        "ssm_d": 1.0 + nrm((L, SSM_HEADS), 0.1),
        "ssm_norm_w": 1.0 + nrm((L, SSM_WIDTH), 0.02),
        "peer_wq": nrm((L, D_MODEL, PEER_HEADS * PEER_QDIM), D_MODEL ** -0.5),
        "peer_k1": nrm((L, PEER_HEADS, PEER_KEYS, PEER_QDIM // 2), (PEER_QDIM // 2) ** -0.5),
        "peer_k2": nrm((L, PEER_HEADS, PEER_KEYS, PEER_QDIM // 2), (PEER_QDIM // 2) ** -0.5),
        "peer_u": nrm((L, PEER_EXPERTS, D_MODEL), D_MODEL ** -0.5),
        "peer_v": nrm((L, PEER_EXPERTS, D_MODEL), 0.5),
        "final_g": 1.0 + nrm((D_MODEL,), 0.02),
    }


def reference(x, c, ctx, c_ctx, w_mod, b_mod, norm1_g, norm2_g, w_in, w_out,
              rwkv_mu, rwkv_w0, rwkv_w2, rwkv_a0, rwkv_a2, rwkv_g2, rwkv_k_k, rwkv_k_a,
              rwkv_r_k, rwkv_ln_w, rwkv_ln_b, ssm_conv_w, ssm_conv_b, ssm_dt_bias, ssm_a_log,
              ssm_d, ssm_norm_w, peer_wq, peer_k1, peer_k2, peer_u, peer_v, final_g):
    rows = x.shape[1] // GRID_W
    h_x, h_c = x, ctx
    s_c = jax.nn.silu(c)
    s_cc = jax.nn.silu(c_ctx)
    for l in range(DEPTH):
        last = l == DEPTH - 1
        lp = {"w_in": w_in[l], "w_out": w_out[l],
              "rwkv_mu": rwkv_mu[l], "rwkv_w0": rwkv_w0[l], "rwkv_w2": rwkv_w2[l],
              "rwkv_a0": rwkv_a0[l], "rwkv_a2": rwkv_a2[l], "rwkv_g2": rwkv_g2[l],
              "rwkv_k_k": rwkv_k_k[l], "rwkv_k_a": rwkv_k_a[l], "rwkv_r_k": rwkv_r_k[l],
              "rwkv_ln_w": rwkv_ln_w[l], "rwkv_ln_b": rwkv_ln_b[l],
              "ssm_conv_w": ssm_conv_w[l], "ssm_conv_b": ssm_conv_b[l],
              "ssm_dt_bias": ssm_dt_bias[l], "ssm_a_log": ssm_a_log[l],
              "ssm_d": ssm_d[l], "ssm_norm_w": ssm_norm_w[l],
              "peer_wq": peer_wq[l], "peer_k1": peer_k1[l], "peer_k2": peer_k2[l],
              "peer_u": peer_u[l], "peer_v": peer_v[l]}
        mod_x = (s_c @ w_mod[l] + b_mod[l])[:, None, :]
        mod_c = (s_cc @ w_mod[l] + b_mod[l])[None, None, :]
        sh1x, sc1x, gt1x, sh2x, sc2x, gt2x = jnp.split(mod_x, N_MOD, axis=-1)
        sh1c, sc1c, gt1c, sh2c, sc2c, gt2c = jnp.split(mod_c, N_MOD, axis=-1)
        n_x = modulate(h_x, norm1_g[l], sh1x, sc1x)
        n_c = modulate(h_c, norm1_g[l], sh1c, sc1c)
        o_x, o_c = hybrid_mixer(n_x, n_c, rows, lp, not last)
        h_x = h_x + gt1x * o_x
        h_x = h_x + gt2x * peer_ffn(modulate(h_x, norm2_g[l], sh2x, sc2x), lp)
        if not last:
            h_c = h_c + gt1c * o_c
            h_c = h_c + gt2c * peer_ffn(modulate(h_c, norm2_g[l], sh2c, sc2c), lp)
    return rmsnorm(h_x, final_g)
```

```python
import numpy as np
from contextlib import ExitStack
import concourse.bass as bass
import concourse.mybir as mybir
from concourse.bass_utils import run_bass_kernel_spmd

F32 = mybir.dt.float32
BF16 = mybir.dt.bfloat16
AF = mybir.ActivationFunctionType
ALU = mybir.AluOpType
AX = mybir.AxisListType

D = 4096
SEQ = 4096
CTX = 256
TT = SEQ + CTX
NCOLS = 12992
NCH = 102
NCOLP = NCH * 128
RW = 2048
RCOLS = 6784
EPS = 1e-6


class T:
    __slots__ = ("name", "ap", "lw", "rd", "dsem", "dcnt")

    def __init__(self, name, ap):
        self.name = name
        self.ap = ap
        self.lw = []
        self.rd = {}
        self.dsem = None
        self.dcnt = 0

    def __getitem__(self, idx):
        return self.ap[idx]


class S:
    def __init__(self, nc, stack):
        self.nc = nc
        self.stack = stack
        self.eng = {"pe": nc.tensor, "dve": nc.vector, "act": nc.scalar, "pool": nc.gpsimd, "sp": nc.sync}
        self.sems = {}
        self.cnt = {}
        self.known = {e: {} for e in self.eng}
        for e in self.eng:
            self.sems[e] = stack.enter_context(nc.semaphore("s_" + e))
            self.cnt[e] = 0
        self.semobj = dict(self.sems)
        self.all_dma_events = {}
        self.n_inst = 0
        self.uid = 0
        self.semstack = stack
        self.dpool = []
        self.dsc = {}
        self.downers = []

    def sb(self, name, shape, dt):
        t = self.stack.enter_context(self.nc.sbuf_tensor(name, list(shape), dt))
        return T(name, t)

    def ps(self, name, shape, dt=F32):
        t = self.stack.enter_context(self.nc.psum_tensor(name, list(shape), dt))
        return T(name, t)

    def dram(self, name, shape, dt, kind="Internal"):
        t = self.nc.dram_tensor(name, list(shape), dt, kind=kind)
        return T(name, t.ap())

    def _wait(self, e, evs):
        eng = self.eng[e]
        kn = self.known[e]
        best = {}
        for (k, v) in evs:
            if v > best.get(k, 0):
                best[k] = v
        for k, v in best.items():
            if kn.get(k, 0) >= v:
                continue
            eng.wait_ge(self.semobj[k], v)
            kn[k] = v

    def _deps(self, reads, writes):
        evs = []
        for t in reads:
            evs.extend(t.lw)
        for t in writes:
            evs.extend(t.lw)
            evs.extend(t.rd.items())
        return evs

    def op(self, e, fn, reads=(), writes=()):
        evs = self._deps(reads, writes)
        if e == "pe":
            evs = [(k, v) for (k, v) in evs if k != "pe"]
        self._wait(e, evs)
        ins = fn(self.eng[e])
        self.cnt[e] += 1
        ins.then_inc(self.sems[e], 1)
        ev = (e, self.cnt[e])
        for t in writes:
            t.lw = [ev]
            t.rd = {}
        for t in reads:
            if t not in writes:
                t.rd[e] = self.cnt[e]
        self.n_inst += 1
        return ins

    def dma(self, q, out_ap, in_ap, reads=(), writes=(), **kw):
        evs = self._deps(reads, writes)
        self._wait(q, evs)
        wt = writes[0]
        if wt.dsem is None:
            if self.dpool:
                nm = self.dpool.pop()
            else:
                self.uid += 1
                nm = "dq%d" % self.uid
                self.semobj[nm] = self.semstack.enter_context(self.nc.semaphore(nm))
                self.dsc[nm] = 0
            wt.dsem = nm
            self.downers.append(wt)
        self.dsc[wt.dsem] += 16
        wt.dcnt = self.dsc[wt.dsem]
        ins = self.eng[q].dma_start(out=out_ap, in_=in_ap, **kw)
        ins.then_inc(self.semobj[wt.dsem], 16)
        ev = (wt.dsem, wt.dcnt)
        self.all_dma_events[wt.dsem] = wt.dcnt
        for t in writes:
            t.lw = [ev]
            t.rd = {}
        for t in reads:
            t.rd[wt.dsem] = wt.dcnt
        self.n_inst += 1
        return ins

    def barrier(self):
        evs = [(e, c) for e, c in self.cnt.items() if c > 0]
        evs += list(self.all_dma_events.items())
        for e in self.eng:
            self._wait(e, evs)
        for t in self.downers:
            self.dpool.append(t.dsem)
            t.dsem = None
        self.downers = []

    def finish(self):
        evs = [(e, c) for e, c in self.cnt.items() if c > 0]
        evs += list(self.all_dma_events.items())
        self._wait("sp", evs)
        self._wait("pool", evs)


def build_program(dbg=None):
    nc = bass.Bass("TRN2", target_bir_lowering=False)
    st = ExitStack()
    s = S(nc, st)
    skip_mixer = dbg in ("tail", "peer1", "h1t")

    def din(name, shape, dt=F32):
        return s.dram(name, shape, dt, kind="ExternalInput")

    xs = din("xs", [TT, D])
    cc = din("cc", [128, 2, 32])
    w_mod = din("w_mod", [D, 6 * D])
    b_mod = din("b_mod", [1, 6 * D])
    norm1_g = din("norm1_g", [1, D])
    w_in_r = din("w_in_r", [NCH, 128, 32 * 128])
    ident_in = din("ident_in", [128, 128])

    modx = s.dram("modx", [128, 6 * D], F32)
    modc = s.dram("modc", [128, 2 * D], F32)
    kind = "ExternalOutput" if dbg == "cols" else "Internal"
    colsT = s.dram("colsT", [NCOLP, TT], F32, kind=kind)

    pb = [s.ps("pb%d" % i, [128, 512], F32) for i in range(8)]

    ident_f = s.sb("ident_f", [128, 128], F32)
    ident = s.sb("ident", [128, 128], BF16)
    s.dma("sp", ident_f[:], ident_in[:, :], reads=[ident_in], writes=[ident_f])
    s.op("dve", lambda e: e.tensor_copy(out=ident[:], in_=ident_f[:]), reads=[ident_f], writes=[ident])

    with ExitStack() as pa:
        s.stack = pa
        cct = s.sb("cct", [128, 2, 32], F32)
        sil = s.sb("sil", [128, 2, 32], F32)
        sbx = s.sb("sbx", [128, 2, 32, 128], F32)
        s.dma("sp", cct[:], cc[:, :, :], reads=[cc], writes=[cct])
        s.op("act", lambda e: e.activation(out=sil[:], in_=cct[:], func=AF.Silu), reads=[cct], writes=[sil])
        for w in range(2):
            s.op("dve", lambda e: e.tensor_copy(
                out=sbx[:, w], in_=sil[:, w, :].unsqueeze(2).to_broadcast([128, 32, 128])),
                reads=[sil], writes=[sbx])
        wts = [s.sb("wmt%d" % i, [128, 2048], F32) for i in range(3)]
        bmt = [s.sb("bmt%d" % i, [128, 2048], F32) for i in range(2)]
        mo = [s.sb("mo%d" % i, [128, 2048], F32) for i in range(2)]
        moc = [s.sb("moc%d" % i, [128, 2048], F32) for i in range(2)]
        it = 0
        for cg in range(12):
            c0 = cg * 2048
            bt = bmt[cg % 2]
            s.dma("act", bt[:], b_mod[0:1, c0:c0 + 2048].partition_broadcast(128), reads=[b_mod], writes=[bt])
            for kc in range(32):
                wt = wts[it % 3]
                it += 1
                q = "sp" if kc % 2 == 0 else "act"
                s.dma(q, wt[:], w_mod[kc * 128:(kc + 1) * 128, c0:c0 + 2048], reads=[w_mod], writes=[wt])
                for j in range(4):
                    s.op("pe", lambda e: e.matmul(pb[j][:, :], lhsT=sbx[:, 0, kc, :], rhs=wt[:, j * 512:(j + 1) * 512],
                                                  start=(kc == 0), stop=(kc == 31)), reads=[sbx, wt], writes=[pb[j]])
                if cg < 4:
                    for j in range(4):
                        s.op("pe", lambda e: e.matmul(pb[4 + j][:, :], lhsT=sbx[:, 1, kc, :], rhs=wt[:, j * 512:(j + 1) * 512],
                                                      start=(kc == 0), stop=(kc == 31)), reads=[sbx, wt], writes=[pb[4 + j]])
            m = mo[cg % 2]
            for j in range(4):
                s.op("dve", lambda e: e.tensor_tensor(out=m[:, j * 512:(j + 1) * 512], in0=pb[j][:, :],
                                                      in1=bt[:, j * 512:(j + 1) * 512], op=ALU.add),
                     reads=[pb[j], bt], writes=[m])
            s.dma("sp", modx[:, c0:c0 + 2048], m[:], reads=[m], writes=[modx])
            if cg < 4:
                mc = moc[cg % 2]
                for j in range(4):
                    s.op("dve", lambda e: e.tensor_tensor(out=mc[:, j * 512:(j + 1) * 512], in0=pb[4 + j][:, :],
                                                          in1=bt[:, j * 512:(j + 1) * 512], op=ALU.add),
                         reads=[pb[4 + j], bt], writes=[mc])
                s.dma("sp", modc[:, c0:c0 + 2048], mc[:], reads=[mc], writes=[modc])
        s.barrier()
    s.stack = st

    with ExitStack() as pbc:
        s.stack = pbc
        G1 = s.sb("G1", [128, D], F32)
        SH1 = s.sb("SH1", [128, D], F32)
        g1b = s.sb("g1b", [128, D], F32)
        nT = s.sb("nT", [128, 32, 1024], BF16)
        xt = [s.sb("xt%d" % i, [128, D], F32) for i in range(2)]
        xn = [s.sb("xn%d" % i, [128, D], BF16) for i in range(2)]
        sq = s.sb("sqjunk", [128, D], BF16)
        ss = [s.sb("ss%d" % i, [128, 1], F32) for i in range(2)]
        rstd = [s.sb("rstd%d" % i, [128, 1], F32) for i in range(2)]
        wt = [s.sb("wit%d" % i, [128, 32, 128], BF16) for i in range(3)]
        ot = [s.sb("ot%d" % i, [128, 512], F32) for i in range(4)]
        s.dma("sp", g1b[:], norm1_g[0:1, :].partition_broadcast(128), reads=[norm1_g], writes=[g1b])

        def load_mod(src, sc_off, sh_off):
            s.dma("sp", G1[:], src[:, sc_off:sc_off + D], reads=[src], writes=[G1])
            s.dma("act", SH1[:], src[:, sh_off:sh_off + D], reads=[src], writes=[SH1])
            s.op("dve", lambda e: e.scalar_tensor_tensor(out=G1[:], in0=G1[:], scalar=1.0, in1=g1b[:],
                                                         op0=ALU.add, op1=ALU.mult), reads=[G1, g1b], writes=[G1])

        blocks = [(0, 256)] + [(256 + i * 1024, 1024) for i in range(4)]
        if skip_mixer:
            blocks = []
        ti = 0
        wi = 0
        oi = 0
        ev = 0
        for bi, (t0, tn) in enumerate(blocks):
            if bi == 0:
                load_mod(modc, D, 0)
            elif bi == 1:
                load_mod(modx, D, 0)
            for tt in range(tn // 128):
                x_ = xt[ti % 2]
                xn_ = xn[ti % 2]
                ss_ = ss[ti % 2]
                rs_ = rstd[ti % 2]
                ti += 1
                r0 = t0 + tt * 128
                s.dma("sp", x_[:], xs[r0:r0 + 128, :], reads=[xs], writes=[x_])
                s.op("act", lambda e: e.activation(out=sq[:], in_=x_[:], func=AF.Square, accum_out=ss_[:]),
                     reads=[x_], writes=[sq, ss_])
                s.op("dve", lambda e: e.tensor_scalar(out=rs_[:], in0=ss_[:], scalar1=1.0 / D, scalar2=EPS,
                                                      op0=ALU.mult, op1=ALU.add), reads=[ss_], writes=[rs_])
                s.op("act", lambda e: e.activation(out=rs_[:], in_=rs_[:], func=AF.Sqrt), reads=[rs_], writes=[rs_])
                s.op("dve", lambda e: e.reciprocal(out=rs_[:], in_=rs_[:]), reads=[rs_], writes=[rs_])
                s.op("dve", lambda e: e.scalar_tensor_tensor(out=x_[:], in0=x_[:], scalar=rs_[:, 0:1], in1=G1[:],
                                                             op0=ALU.mult, op1=ALU.mult),
                     reads=[x_, rs_, G1], writes=[x_])
                s.op("pool", lambda e: e.tensor_tensor(out=xn_[:], in0=x_[:], in1=SH1[:], op=ALU.add),
                     reads=[x_, SH1], writes=[xn_])
                for q4 in range(4):
                    pt = pb[4 + (ev % 2)]
                    ev += 1
                    ptb = pt[:, :].bitcast(BF16)
                    for j in range(8):
                        kc = q4 * 8 + j
                        s.op("pe", lambda e: e.transpose(out=ptb[:, j * 128:(j + 1) * 128],
                                                         in_=xn_[:, kc * 128:(kc + 1) * 128], identity=ident[:]),
                             reads=[xn_, ident], writes=[pt])
                    eng = "act" if q4 % 2 == 0 else "dve"
                    dst = nT[:, q4 * 8:(q4 + 1) * 8, tt * 128:(tt + 1) * 128]
                    src = ptb.rearrange("p (j t) -> p j t", j=8)
                    if eng == "act":
                        s.op("act", lambda e: e.copy(out=dst, in_=src), reads=[pt], writes=[nT])
                    else:
                        s.op("dve", lambda e: e.tensor_copy(out=dst, in_=src), reads=[pt], writes=[nT])
            ng = (tn + 511) // 512
            gw = min(tn, 512)
            for j in range(NCH):
                w_ = wt[wi % 3]
                wi += 1
                s.dma("pool", w_[:], w_in_r[j].rearrange("p (k c) -> p k c", k=32), reads=[w_in_r], writes=[w_])
                for g in range(ng):
                    pp = pb[(j % 2) * 2 + g]
                    for kc in range(32):
                        s.op("pe", lambda e: e.matmul(pp[:, 0:gw], lhsT=w_[:, kc, :], rhs=nT[:, kc, g * 512:g * 512 + gw],
                                                      start=(kc == 0), stop=(kc == 31)), reads=[w_, nT], writes=[pp])
                    o_ = ot[oi % 4]
                    if oi % 2 == 0:
                        s.op("act", lambda e: e.copy(out=o_[:, 0:gw], in_=pp[:, 0:gw]), reads=[pp], writes=[o_])
                    else:
                        s.op("dve", lambda e: e.tensor_copy(out=o_[:, 0:gw], in_=pp[:, 0:gw]), reads=[pp], writes=[o_])
                    oi += 1
                    s.dma("sp", colsT[j * 128:(j + 1) * 128, t0 + g * 512:t0 + g * 512 + gw], o_[:, 0:gw],
                          reads=[o_], writes=[colsT])
        s.barrier()
    s.stack = st


    KAPPA = float(np.exp(-0.5))
    NB = [(i * 512, min(512, TT - i * 512)) for i in range((TT + 511) // 512)]

    def shift_mix(raw, out, u, np_):
        engs = ["dve", "pool"]
        s.op("dve", lambda e: e.tensor_scalar(out=out[0:np_, :], in0=raw[0:np_, :], scalar1=omm[0:np_, u:u + 1],
                                              scalar2=None, op0=ALU.mult), reads=[raw, omm], writes=[out])

        def acc(eng, o_ap, i_ap, sc):
            s.op(eng, lambda e: e.scalar_tensor_tensor(out=o_ap, in0=i_ap, scalar=sc, in1=o_ap,
                                                       op0=ALU.mult, op1=ALU.add), reads=[raw, musl, out], writes=[out])
        acc("dve", out[0:np_, 1:CTX], raw[0:np_, 0:CTX - 1], musl[0:np_, u, 4:5])
        acc("dve", out[0:np_, 0:CTX - 1], raw[0:np_, 1:CTX], musl[0:np_, u, 5:6])
        lo = out[0:np_, CTX:TT].rearrange("p (r c) -> p r c", c=64)
        lr = raw[0:np_, CTX:TT].rearrange("p (r c) -> p r c", c=64)
        acc("dve", lo[:, :, 1:64], lr[:, :, 0:63], musl[0:np_, u, 0:1])
        acc("dve", lo[:, :, 0:63], lr[:, :, 1:64], musl[0:np_, u, 1:2])
        acc("dve", out[0:np_, CTX + 64:TT], raw[0:np_, CTX:TT - 64], musl[0:np_, u, 2:3])
        acc("dve", out[0:np_, CTX:TT - 64], raw[0:np_, CTX + 64:TT], musl[0:np_, u, 3:4])

    mu_u = din("mu_u", [128, 54])
    slotm = din("slotm", [128, 6])
    w2r = din("w2r", [96, 2, RW])
    a2r = din("a2r", [96, 2, RW])
    g2r = din("g2r", [128, 2, RW])
    chp_in = din("chp", [128, 16, 9])
    msk_in = din("msk", [128, 4, 128])
    blk_in = din("blk", [128, 128])
    sgT = s.dram("sgT", [2, RW, TT], F32)
    asigT = s.dram("asigT", [2, RW, TT], BF16)
    gT = s.dram("gT", [RW, TT], BF16)
    kind = "ExternalOutput" if dbg in ("rwkv", "rwkv1", "ssd1", "ssd") else "Internal"
    if skip_mixer:
        kind = "ExternalInput"
    yT = s.dram("yT", [D, SEQ], BF16, kind=kind)

    omm = s.sb("omm", [128, 54], F32)
    musl = s.sb("musl", [128, 54, 6], F32)
    chp = s.sb("chp_sb", [128, 16, 9], F32)
    omka = s.sb("omka", [128, 16], F32)
    omka2 = s.sb("omka2", [128, 16], F32)
    bonD = s.dram("bonD", [RW, SEQ], BF16)
    msk = s.sb("msk_sb", [128, 4, 128], BF16)
    blk = s.sb("blk_sb", [128, 128], BF16)
    with ExitStack() as pc:
        s.stack = pc
        mut = s.sb("mut", [128, 54], F32)
        slt = s.sb("slt", [128, 6], F32)
        s.dma("sp", mut[:], mu_u[:, :], reads=[mu_u], writes=[mut])
        s.dma("sp", slt[:], slotm[:, :], reads=[slotm], writes=[slt])
        s.dma("sp", chp[:], chp_in[:, :, :], reads=[chp_in], writes=[chp])
        s.dma("pool", msk[:], msk_in[:, :, :], reads=[msk_in], writes=[msk])
        s.dma("pool", blk[:], blk_in[:, :], reads=[blk_in], writes=[blk])
        s.op("dve", lambda e: e.tensor_scalar(out=omm[:], in0=mut[:], scalar1=-1.0, scalar2=1.0,
                                              op0=ALU.mult, op1=ALU.add), reads=[mut], writes=[omm])
        for k in range(6):
            s.op("dve", lambda e: e.tensor_scalar(out=musl[:, :, k], in0=mut[:], scalar1=slt[:, k:k + 1], scalar2=None,
                                                  op0=ALU.mult), reads=[mut, slt], writes=[musl])
        s.op("dve", lambda e: e.tensor_scalar(out=omka[:], in0=chp[:, :, 5], scalar1=-1.0, scalar2=1.0,
                                              op0=ALU.mult, op1=ALU.add), reads=[chp], writes=[omka])
        s.op("dve", lambda e: e.tensor_scalar(out=omka2[:], in0=chp[:, :, 5], scalar1=-2.0, scalar2=2.0,
                                              op0=ALU.mult, op1=ALU.add), reads=[chp], writes=[omka2])
        s.barrier()
    s.stack = st

    with ExitStack() as pd1:
        s.stack = pd1
        tmpA = s.sb("d1A", [128, TT], F32)
        tmpB = s.sb("d1B", [128, TT], F32)
        lr_w = [s.sb("lr_w%d" % d, [96, TT], BF16) for d in range(2)]
        lr_a = [s.sb("lr_a%d" % d, [96, TT], BF16) for d in range(2)]
        lr_g = s.sb("lr_g", [128, 2, TT], BF16)
        w2b = s.sb("w2b", [96, 2, RW], BF16)
        a2b = s.sb("a2b", [96, 2, RW], BF16)
        g2b = s.sb("g2b", [128, 2, RW], BF16)
        s.dma("pool", w2b[:], w2r[:, :, :], reads=[w2r], writes=[w2b])
        s.dma("pool", a2b[:], a2r[:, :, :], reads=[a2r], writes=[a2b])
        s.dma("pool", g2b[:], g2r[:, :, :], reads=[g2r], writes=[g2b])
        units = [(6144 + 96 * d, 96, 48 + d, lr_w[d][:, :], AF.Tanh) for d in range(2)]
        units += [(6336 + 96 * d, 96, 50 + d, lr_a[d][:, :], AF.Copy) for d in range(2)]
        units += [(6528 + 128 * i, 128, 52 + i, lr_g[:, i, :], AF.Sigmoid) for i in range(2)]
        lr_t = {id(lr_w[0][:, :]): lr_w[0]}
        lr_tiles = [lr_w[0], lr_w[1], lr_a[0], lr_a[1], lr_g, lr_g]
        if skip_mixer:
            units = []
        for ui, (row0, np_, u, dst, fn) in enumerate(units):
            s.dma("sp", tmpA[0:np_, :], colsT[row0:row0 + np_, :], reads=[colsT], writes=[tmpA])
            shift_mix(tmpA, tmpB, u, np_)
            s.op("act", lambda e: e.activation(out=dst[0:np_], in_=tmpB[0:np_, :], func=fn),
                 reads=[tmpB], writes=[lr_tiles[ui]])
        sgs = [s.sb("sgs%d" % i, [128, TT], F32) for i in range(2)]
        asg = [s.sb("asg%d" % i, [128, TT], BF16) for i in range(2)]
        gs = s.sb("gs", [128, TT], BF16)
        bk = 0
        for cj in range(0 if skip_mixer else 16):
            csl = slice(cj * 128, (cj + 1) * 128)
            for d in range(2):
                for (b0, bn) in NB:
                    pp = pb[bk % 4]; bk += 1
                    s.op("pe", lambda e: e.matmul(pp[:, 0:bn], lhsT=w2b[:, d, csl], rhs=lr_w[d][:, b0:b0 + bn],
                                                  start=True, stop=True), reads=[w2b, lr_w[d]], writes=[pp])
                    s.op("act", lambda e: e.activation(out=sgs[d][:, b0:b0 + bn], in_=pp[:, 0:bn], func=AF.Sigmoid,
                                                       bias=chp[:, cj, d:d + 1]), reads=[pp, chp], writes=[sgs[d]])
                s.dma("sp", sgT[d, csl, :], sgs[d][:], reads=[sgs[d]], writes=[sgT])
                for (b0, bn) in NB:
                    pp = pb[bk % 4]; bk += 1
                    s.op("pe", lambda e: e.matmul(pp[:, 0:bn], lhsT=a2b[:, d, csl], rhs=lr_a[d][:, b0:b0 + bn],
                                                  start=True, stop=True), reads=[a2b, lr_a[d]], writes=[pp])
                    s.op("act", lambda e: e.activation(out=asg[d][:, b0:b0 + bn], in_=pp[:, 0:bn], func=AF.Sigmoid,
                                                       bias=chp[:, cj, 2 + d:3 + d]), reads=[pp, chp], writes=[asg[d]])
                s.dma("sp", asigT[d, csl, :], asg[d][:], reads=[asg[d]], writes=[asigT])
            for (b0, bn) in NB:
                pp = pb[bk % 4]; bk += 1
                for i in range(2):
                    s.op("pe", lambda e: e.matmul(pp[:, 0:bn], lhsT=g2b[:, i, csl], rhs=lr_g[:, i, b0:b0 + bn],
                                                  start=(i == 0), stop=(i == 1)), reads=[g2b, lr_g], writes=[pp])
                s.op("dve", lambda e: e.tensor_copy(out=gs[:, b0:b0 + bn], in_=pp[:, 0:bn]), reads=[pp], writes=[gs])
            s.dma("sp", gT[csl, :], gs[:], reads=[gs], writes=[gT])
        s.barrier()
    s.stack = st

    NCK = TT // 128
    with ExitStack() as pe_:
        s.stack = pe_
        tA = s.sb("eA", [128, TT], F32)
        tB = s.sb("eB", [128, TT], F32)
        r_m = s.sb("r_m", [128, TT], BF16)
        v_m = s.sb("v_m", [128, TT], BF16)
        kkn = s.sb("kkn", [128, TT], BF16)
        k_m = s.sb("k_m", [128, TT], BF16)
        kdx = s.sb("kdx", [128, TT], BF16)
        bdx = s.sb("bdx", [128, TT], BF16)
        kdir = [kdx, kdx]
        bdir = [bdx, bdx]
        Ebuf = s.sb("Ebuf", [128, TT], BF16)
        AR = s.sb("AR", [128, 2, TT], BF16)
        BK = s.sb("BK", [128, 2, TT], BF16)
        BKhT = s.sb("BKhT", [128, 2, NCK, 128], BF16)
        BKh_ap = tB[:, :].bitcast(BF16).rearrange("p (w t) -> p w t", w=2)
        yst_ap = AR[:, 0, 0:SEQ]
        gld_ap = BK[:, 0, 0:SEQ]
        bon_ap = BK[:, 1, 0:SEQ]
        VtA = s.sb("VtA", [128, NCK, 128], BF16)
        Oacc = s.sb("Oacc", [128, 32, 128], F32)
        segm = s.sb("segm", [128, TT], BF16)
        tot = s.sb("tot", [128, NCK], F32)
        PL = s.sb("PL", [128, NCK], F32)
        STf = s.sb("STf", [128, 64], F32)
        STb = s.sb("STb", [128, 64], BF16)
        s.op("pool", lambda e: e.memset(segm[:], 1.0), writes=[segm])
        s.op("pool", lambda e: e.memset(segm[:, :].rearrange("p (c t) -> p c t", t=128)[:, :, 0:1], 0.0), writes=[segm])
        NW = 6
        w_Mb = [s.sb("w_Mb%d" % i, [128, 256], BF16) for i in range(4)]
        w_Mk = [s.sb("w_Mk%d" % i, [128, 256], BF16) for i in range(4)]
        w_A = [s.sb("w_A%d" % i, [128, 128], BF16) for i in range(NW)]
        w_B = [s.sb("w_B%d" % i, [128, 128], BF16) for i in range(NW)]
        w_X = [s.sb("w_X%d" % i, [128, 128], BF16) for i in range(NW)]
        w_XF = [s.sb("w_XF%d" % i, [128, 128], BF16) for i in range(4)]
        w_R = [s.sb("w_R%d" % i, [128, 128], BF16) for i in range(2)]
        w_U = [s.sb("w_U%d" % i, [128, 128], BF16) for i in range(2)]
        osb = [s.sb("osb%d" % i, [128, 128], F32) for i in range(2)]
        onb = [s.sb("onb%d" % i, [128, 128], BF16) for i in range(2)]
        stat = [s.sb("stat%d" % i, [128, 8], F32) for i in range(2)]
        cnt = {"bank": 0, "A": 0, "B": 0, "X": 0, "M": 0, "R": 0, "ev": 0}

        def nbank():
            cnt["bank"] += 1
            return pb[cnt["bank"] % 7]

        def rot(lst, key):
            cnt[key] += 1
            return lst[cnt[key] % len(lst)]

        def evac_copy(dst_t, dst_ap, src_t, src_ap):
            cnt["ev"] += 1
            if cnt["ev"] % 2 == 0:
                s.op("act", lambda e: e.copy(out=dst_ap, in_=src_ap), reads=[src_t], writes=[dst_t])
            else:
                s.op("dve", lambda e: e.tensor_copy(out=dst_ap, in_=src_ap), reads=[src_t], writes=[dst_t])

        ptr = pb[7]
        ptrb = ptr[:, :].bitcast(BF16)

        for hp in range(16):
            if dbg in ('ssd1', 'ssd') or skip_mixer:
                break
            csl = slice(hp * 128, (hp + 1) * 128)
            for which, dst in ((0, r_m), (2, v_m), (1, k_m)):
                row0 = which * RW + hp * 128
                s.dma("sp", tA[:], colsT[row0:row0 + 128, :], reads=[colsT], writes=[tA])
                shift_mix(tA, tB, which * 16 + hp, 128)
                if dst is not None:
                    s.op("act", lambda e: e.copy(out=dst[:], in_=tB[:]), reads=[tB], writes=[dst])
            s.op("dve", lambda e: e.tensor_scalar(out=tA[:], in0=tB[:], scalar1=chp[:, hp, 4:5], scalar2=None,
                                                  op0=ALU.mult), reads=[tB, chp], writes=[tA])
            s.op("act", lambda e: e.activation(out=Ebuf[:], in_=tA[:], func=AF.Square), reads=[tA], writes=[Ebuf])
            for (b0, bn) in NB:
                pp = nbank()
                s.op("pe", lambda e: e.matmul(pp[:, 0:bn], lhsT=blk[:], rhs=Ebuf[:, b0:b0 + bn], start=True, stop=True),
                     reads=[blk, Ebuf], writes=[pp])
                s.op("act", lambda e: e.activation(out=tB[:, b0:b0 + bn], in_=pp[:, 0:bn], func=AF.Sqrt),
                     reads=[pp], writes=[tB])
            s.op("dve", lambda e: e.tensor_scalar(out=tB[:], in0=tB[:], scalar1=1e-12, scalar2=None,
                                                  op0=ALU.max), reads=[tB], writes=[tB])
            s.op("dve", lambda e: e.reciprocal(out=tB[:], in_=tB[:]), reads=[tB], writes=[tB])
            s.op("dve", lambda e: e.tensor_tensor(out=kkn[:], in0=tA[:], in1=tB[:], op=ALU.mult),
                 reads=[tA, tB], writes=[kkn])
            s.dma("sp", bdx[:], asigT[0, csl, :], reads=[asigT], writes=[bdx])
            s.dma("sp", kdx[:], asigT[1, csl, :], reads=[asigT], writes=[kdx])
            s.op("pool", lambda e: e.tensor_tensor(out=Ebuf[:], in0=bdx[:], in1=kdx[:], op=ALU.add),
                 reads=[bdx, kdx], writes=[Ebuf])
            s.op("dve", lambda e: e.tensor_scalar(out=Ebuf[:], in0=Ebuf[:], scalar1=chp[:, hp, 5:6], scalar2=omka2[:, hp:hp + 1],
                                                  op0=ALU.mult, op1=ALU.add), reads=[Ebuf, chp, omka2], writes=[Ebuf])
            s.op("dve", lambda e: e.tensor_tensor(out=Ebuf[:], in0=Ebuf[:], in1=k_m[:], op=ALU.mult),
                 reads=[Ebuf, k_m], writes=[Ebuf])
            s.op("dve", lambda e: e.scalar_tensor_tensor(out=Ebuf[:], in0=r_m[:], scalar=chp[:, hp, 6:7], in1=Ebuf[:],
                                                         op0=ALU.mult, op1=ALU.mult), reads=[r_m, chp, Ebuf], writes=[Ebuf])
            for (b0, bn) in [(CTX + i * 512, 512) for i in range(8)]:
                pp = nbank()
                s.op("pe", lambda e: e.matmul(pp[:, 0:bn], lhsT=blk[:], rhs=Ebuf[:, b0:b0 + bn], start=True, stop=True),
                     reads=[blk, Ebuf], writes=[pp])
                s.op("dve", lambda e: e.tensor_tensor(out=yst_ap[:, b0 - CTX:b0 - CTX + bn], in0=pp[:, 0:bn],
                                                      in1=v_m[:, b0:b0 + bn], op=ALU.mult), reads=[pp, v_m], writes=[AR])
            s.dma("sp", bonD[csl, :], yst_ap, reads=[AR], writes=[bonD])
            for q in range((NCK + 7) // 8):
                n8 = min(8, NCK - q * 8)
                for j in range(n8):
                    ci = q * 8 + j
                    s.op("pe", lambda e: e.transpose(out=ptrb[:, j * 128:(j + 1) * 128], in_=v_m[:, ci * 128:(ci + 1) * 128],
                                                     identity=ident[:]), reads=[v_m, ident], writes=[ptr])
                evac_copy(VtA, VtA[:, q * 8:q * 8 + n8, :], ptr, ptrb[:, 0:n8 * 128].rearrange("p (j t) -> p j t", j=n8))

            for d in range(2):
                s.dma("sp", bdx[:], asigT[d, csl, :], reads=[asigT], writes=[bdx])
                s.op("dve", lambda e: e.tensor_scalar(out=kdx[:], in0=bdx[:], scalar1=chp[:, hp, 5:6],
                                                      scalar2=omka[:, hp:hp + 1], op0=ALU.mult, op1=ALU.add),
                     reads=[bdx, chp, omka], writes=[kdx])
                s.op("dve", lambda e: e.tensor_tensor(out=kdx[:], in0=kdx[:], in1=k_m[:], op=ALU.mult),
                     reads=[kdx, k_m], writes=[kdx])
                s.op("pool", lambda e: e.tensor_tensor(out=bdx[:], in0=bdx[:], in1=kkn[:], op=ALU.mult),
                     reads=[bdx, kkn], writes=[bdx])
                s.dma("sp", tA[:], sgT[d, csl, :], reads=[sgT], writes=[tA])
                s.op("dve", lambda e: e.tensor_tensor_scan(out=tB[:], data0=segm[:], data1=tA[:], initial=0.0,
                                                           op0=ALU.mult, op1=ALU.add), reads=[segm, tA], writes=[tB])
                tB3 = tB[:, :].rearrange("p (c t) -> p c t", t=128)
                tA3 = tA[:, :].rearrange("p (c t) -> p c t", t=128)
                s.op("dve", lambda e: e.tensor_copy(out=tot[:], in_=tB3[:, :, 127]), reads=[tB], writes=[tot])
                if d == 1:
                    s.op("dve", lambda e: e.tensor_tensor(out=tB[:], in0=tA[:], in1=tB[:], op=ALU.subtract),
                         reads=[tA, tB], writes=[tB])
                    s.op("dve", lambda e: e.tensor_tensor(out=tB3, in0=tB3, in1=tot[:, :].unsqueeze(2).to_broadcast([128, NCK, 128]),
                                                          op=ALU.add), reads=[tB, tot], writes=[tB])
                s.op("act", lambda e: e.activation(out=PL[:], in_=tot[:], func=AF.Exp, scale=-KAPPA), reads=[tot], writes=[PL])
                s.op("act", lambda e: e.activation(out=Ebuf[:], in_=tB[:], func=AF.Exp, scale=-KAPPA), reads=[tB], writes=[Ebuf])
                s.op("dve", lambda e: e.tensor_tensor(out=AR[:, 1, :], in0=r_m[:], in1=Ebuf[:], op=ALU.mult),
                     reads=[r_m, Ebuf], writes=[AR])
                s.op("act", lambda e: e.activation(out=Ebuf[:], in_=tB[:], func=AF.Exp, scale=KAPPA), reads=[tB], writes=[Ebuf])
                s.op("dve", lambda e: e.tensor_tensor(out=BK[:, 0, :], in0=bdir[d][:], in1=Ebuf[:], op=ALU.mult),
                     reads=[bdir[d], Ebuf], writes=[BK])
                s.op("pool", lambda e: e.tensor_tensor(out=BK[:, 1, :], in0=kdir[d][:], in1=Ebuf[:], op=ALU.mult),
                     reads=[kdir[d], Ebuf], writes=[BK])
                s.op("dve", lambda e: e.tensor_tensor(out=tA[:], in0=tB[:], in1=tA[:], op=ALU.subtract),
                     reads=[tA, tB], writes=[tA])
                s.op("act", lambda e: e.activation(out=Ebuf[:], in_=tA[:], func=AF.Exp, scale=-KAPPA), reads=[tA], writes=[Ebuf])
                s.op("dve", lambda e: e.scalar_tensor_tensor(out=AR[:, 0, :], in0=kkn[:], scalar=-1.0, in1=Ebuf[:],
                                                             op0=ALU.mult, op1=ALU.mult), reads=[kkn, Ebuf], writes=[AR])
                s.op("dve", lambda e: e.tensor_tensor(out=tA3, in0=tot[:, :].unsqueeze(2).to_broadcast([128, NCK, 128]),
                                                      in1=tB3, op=ALU.subtract), reads=[tB, tot], writes=[tA])
                s.op("act", lambda e: e.activation(out=Ebuf[:], in_=tA[:], func=AF.Exp, scale=-KAPPA), reads=[tA], writes=[Ebuf])
                s.op("dve", lambda e: e.tensor_tensor(out=BKh_ap[:, 0, :], in0=bdir[d][:], in1=Ebuf[:], op=ALU.mult),
                     reads=[bdir[d], Ebuf], writes=[tB])
                s.op("pool", lambda e: e.tensor_tensor(out=BKh_ap[:, 1, :], in0=kdir[d][:], in1=Ebuf[:], op=ALU.mult),
                     reads=[kdir[d], Ebuf], writes=[tB])
                for w in range(2):
                    for q in range((NCK + 7) // 8):
                        n8 = min(8, NCK - q * 8)
                        for j in range(n8):
                            ci = q * 8 + j
                            s.op("pe", lambda e: e.transpose(out=ptrb[:, j * 128:(j + 1) * 128],
                                                             in_=BKh_ap[:, w, ci * 128:(ci + 1) * 128], identity=ident[:]),
                                 reads=[tB, ident], writes=[ptr])
                        evac_copy(BKhT, BKhT[:, w, q * 8:q * 8 + n8, :], ptr,
                                  ptrb[:, 0:n8 * 128].rearrange("p (j t) -> p j t", j=n8))
                s.op("pool", lambda e: e.memset(STf[:], 0.0), writes=[STf])
                s.op("pool", lambda e: e.memset(STb[:], 0.0), writes=[STb])
                order = list(range(NCK)) if d == 0 else [1, 0] + list(range(NCK - 1, 1, -1))
                mbk = msk[:, 0:2, :] if d == 0 else msk[:, 2:4, :]
                mn = msk[:, 2, :] if d == 0 else msk[:, 0, :]
                for ci in order:
                    tsl = slice(ci * 128, (ci + 1) * 128)
                    Xh = []
                    Mbh = []
                    Mkh = []
                    for hh in range(2):
                        hs = slice(hh * 64, (hh + 1) * 64)
                        pA, pB, pC = nbank(), nbank(), nbank()
                        s.op("pe", lambda e: e.matmul(pA[:, 0:256].rearrange("p (a t) -> p a t", a=2), lhsT=BK[hs, 0, tsl], rhs=AR[hs, :, tsl], start=True, stop=True),
                             reads=[BK, AR], writes=[pA])
                        s.op("pe", lambda e: e.matmul(pB[:, 0:256].rearrange("p (a t) -> p a t", a=2), lhsT=BK[hs, 1, tsl], rhs=AR[hs, :, tsl], start=True, stop=True),
                             reads=[BK, AR], writes=[pB])
                        s.op("pe", lambda e: e.matmul(pC[:, 0:128], lhsT=AR[hs, 0, tsl], rhs=BK[hs, 0, tsl], start=True, stop=True),
                             reads=[BK, AR], writes=[pC])
                        Mb = rot(w_Mb, "M")
                        Mk = w_Mk[cnt["M"] % len(w_Mk)]
                        s.op("dve", lambda e: e.tensor_tensor(out=Mb[:, :].rearrange("p (a t) -> p a t", a=2),
                                                              in0=pA[:, 0:256].rearrange("p (a t) -> p a t", a=2),
                                                              in1=mbk, op=ALU.mult), reads=[pA, msk], writes=[Mb])
                        s.op("dve", lambda e: e.tensor_tensor(out=Mk[:, :].rearrange("p (a t) -> p a t", a=2),
                                                              in0=pB[:, 0:256].rearrange("p (a t) -> p a t", a=2),
                                                              in1=mbk, op=ALU.mult), reads=[pB, msk], writes=[Mk])
                        A = rot(w_A, "A")
                        s.op("dve", lambda e: e.tensor_tensor(out=A[:], in0=pC[:, 0:128], in1=mn, op=ALU.mult),
                             reads=[pC, msk], writes=[A])
                        B = rot(w_B, "B")
                        s.op("pool", lambda e: e.tensor_copy(out=B[:], in_=Mb[:, 0:128]), reads=[Mb], writes=[B])
                        X = rot(w_X, "X")
                        s.op("pool", lambda e: e.tensor_tensor(out=X[:], in0=Mb[:, 0:128], in1=ident[:], op=ALU.add),
                             reads=[Mb, ident], writes=[X])
                        for k in range(7):
                            if k >= 1:
                                pX = nbank()
                                s.op("pe", lambda e: e.matmul(pX[:, 0:128], lhsT=A[:], rhs=X[:], start=True, stop=True),
                                     reads=[A, X], writes=[pX])
                                Xn = rot(w_X, "X") if k < 6 else w_XF[(cnt["R"] % 2) * 2 + hh]
                                s.op("dve", lambda e: e.tensor_tensor(out=Xn[:], in0=pX[:, 0:128], in1=X[:], op=ALU.add),
                                     reads=[pX, X], writes=[Xn])
                                X = Xn
                            if k < 6:
                                pA2, pB2 = nbank(), nbank()
                                s.op("pe", lambda e: e.matmul(pA2[:, 0:128], lhsT=B[:], rhs=A[:], start=True, stop=True),
                                     reads=[A, B], writes=[pA2])
                                s.op("pe", lambda e: e.matmul(pB2[:, 0:128], lhsT=A[:], rhs=B[:], start=True, stop=True),
                                     reads=[A, B], writes=[pB2])
                                An = rot(w_A, "A")
                                Bn = rot(w_B, "B")
                                s.op("act", lambda e: e.copy(out=An[:], in_=pA2[:, 0:128]), reads=[pA2], writes=[An])
                                s.op("act", lambda e: e.copy(out=Bn[:], in_=pB2[:, 0:128]), reads=[pB2], writes=[Bn])
                                A, B = An, Bn
                        Xh.append(X); Mbh.append(Mb); Mkh.append(Mk)
                    pU = nbank()
                    for hh in range(2):
                        hs = slice(hh * 64, (hh + 1) * 64)
                        s.op("pe", lambda e: e.matmul(pU[:, hs], lhsT=AR[hs, 0, tsl], rhs=STb[hs, :], start=True, stop=False),
                             reads=[AR, STb], writes=[pU])
                        s.op("pe", lambda e: e.matmul(pU[:, hs], lhsT=Mkh[hh][:, 0:128], rhs=VtA[:, ci, hs], start=False, stop=True),
                             reads=[Mkh[hh], VtA], writes=[pU])
                    Rr = rot(w_R, "R")
                    evac_copy(Rr, Rr[:], pU, pU[:, 0:128])
                    pU2 = nbank()
                    for hh in range(2):
                        hs = slice(hh * 64, (hh + 1) * 64)
                        s.op("pe", lambda e: e.matmul(pU2[:, hs], lhsT=Xh[hh][:], rhs=Rr[:, hs], start=True, stop=True),
                             reads=[Xh[hh], Rr], writes=[pU2])
                    Ub = w_U[cnt["R"] % 2]
                    evac_copy(Ub, Ub[:], pU2, pU2[:, 0:128])
                    if ci >= 2:
                        pO = nbank()
                        for hh in range(2):
                            hs = slice(hh * 64, (hh + 1) * 64)
                            s.op("pe", lambda e: e.matmul(pO[:, hs], lhsT=AR[hs, 1, tsl], rhs=STb[hs, :], start=True, stop=False),
                                 reads=[AR, STb], writes=[pO])
                            s.op("pe", lambda e: e.matmul(pO[:, hs], lhsT=Mbh[hh][:, 128:256], rhs=Ub[:, hs], start=False, stop=False),
                                 reads=[Mbh[hh], Ub], writes=[pO])
                            s.op("pe", lambda e: e.matmul(pO[:, hs], lhsT=Mkh[hh][:, 128:256], rhs=VtA[:, ci, hs], start=False, stop=True),
                                 reads=[Mkh[hh], VtA], writes=[pO])
                        if d == 0:
                            evac_copy(Oacc, Oacc[:, ci - 2, :], pO, pO[:, 0:128])
                        else:
                            s.op("dve", lambda e: e.tensor_tensor(out=Oacc[:, ci - 2, :], in0=pO[:, 0:128], in1=Oacc[:, ci - 2, :],
                                                                  op=ALU.add), reads=[pO, Oacc], writes=[Oacc])
                    pS = nbank()
                    for hh in range(2):
                        hs = slice(hh * 64, (hh + 1) * 64)
                        s.op("pe", lambda e: e.matmul(pS[hs, 0:64], lhsT=BKhT[:, 0, ci, hs], rhs=Ub[:, hs], start=True, stop=False),
                             reads=[BKhT, Ub], writes=[pS])
                        s.op("pe", lambda e: e.matmul(pS[hs, 0:64], lhsT=BKhT[:, 1, ci, hs], rhs=VtA[:, ci, hs], start=False, stop=True),
                             reads=[BKhT, VtA], writes=[pS])
                    s.op("dve", lambda e: e.scalar_tensor_tensor(out=STf[:], in0=STf[:], scalar=PL[:, ci:ci + 1], in1=pS[:, 0:64],
                                                                 op0=ALU.mult, op1=ALU.add), reads=[STf, PL, pS], writes=[STf])
                    s.op("act", lambda e: e.copy(out=STb[:], in_=STf[:]), reads=[STf], writes=[STb])
            s.dma("sp", gld_ap, gT[csl, CTX:TT], reads=[gT], writes=[BK])
            s.dma("act", bon_ap, bonD[csl, :], reads=[bonD], writes=[BK])
            for lc in range(32):
                ob = osb[lc % 2]
                on = onb[lc % 2]
                stt = stat[lc % 2]
                for hh in range(2):
                    hs = slice(hh * 64, (hh + 1) * 64)
                    s.op("dve", lambda e: e.tensor_reduce(out=stt[:, hh:hh + 1], in_=Oacc[:, lc, hs], axis=AX.X, op=ALU.add),
                         reads=[Oacc], writes=[stt])
                    s.op("dve", lambda e: e.tensor_scalar(out=stt[:, 2 + hh:3 + hh], in0=stt[:, hh:hh + 1], scalar1=-1.0 / 64,
                                                          scalar2=None, op0=ALU.mult), reads=[stt], writes=[stt])
                    s.op("dve", lambda e: e.tensor_scalar(out=ob[:, hs], in0=Oacc[:, lc, hs], scalar1=stt[:, 2 + hh:3 + hh],
                                                          scalar2=None, op0=ALU.add), reads=[Oacc, stt], writes=[ob])
                    s.op("act", lambda e: e.activation(out=on[:, hs], in_=ob[:, hs], func=AF.Square,
                                                       accum_out=stt[:, 4 + hh:5 + hh]), reads=[ob], writes=[on, stt])
                    s.op("dve", lambda e: e.tensor_scalar(out=stt[:, 6 + hh:7 + hh], in0=stt[:, 4 + hh:5 + hh], scalar1=1.0 / 64,
                                                          scalar2=64e-5, op0=ALU.mult, op1=ALU.add), reads=[stt], writes=[stt])
                    s.op("act", lambda e: e.activation(out=stt[:, 6 + hh:7 + hh], in_=stt[:, 6 + hh:7 + hh], func=AF.Sqrt),
                         reads=[stt], writes=[stt])
                    s.op("dve", lambda e: e.reciprocal(out=stt[:, 6 + hh:7 + hh], in_=stt[:, 6 + hh:7 + hh]), reads=[stt], writes=[stt])
                    s.op("dve", lambda e: e.tensor_scalar(out=on[:, hs], in0=ob[:, hs], scalar1=stt[:, 6 + hh:7 + hh],
                                                          scalar2=None, op0=ALU.mult), reads=[ob, stt], writes=[on])
                s.op("pe", lambda e: e.transpose(out=ptrb[:, 0:128], in_=on[:], identity=ident[:]), reads=[on, ident], writes=[ptr])
                tl = slice(lc * 128, (lc + 1) * 128)
                s.op("act", lambda e: e.activation(out=yst_ap[:, tl], in_=ptrb[:, 0:128], func=AF.Identity,
                                                   scale=chp[:, hp, 7:8], bias=chp[:, hp, 8:9]), reads=[ptr, chp], writes=[AR])
            s.op("dve", lambda e: e.tensor_tensor(out=yst_ap, in0=yst_ap, in1=bon_ap, op=ALU.add), reads=[AR, BK], writes=[AR])
            s.op("dve", lambda e: e.tensor_tensor(out=yst_ap, in0=yst_ap, in1=gld_ap, op=ALU.mult), reads=[AR, BK], writes=[AR])
            s.dma("sp", yT[csl, :], yst_ap, reads=[AR], writes=[yT])
            if dbg == "rwkv1":
                break
        s.barrier()
    s.stack = st


    cw_in = din("cw", [128, 32, 4])
    dtp_in = din("dtp", [64, 2])
    dbc_in = din("dbc", [1, RW])
    nwb_in = din("nwb", [1, RW])
    ZR0, XR0, BR0, CR0, DTR0 = RCOLS, RCOLS + 2048, RCOLS + 4096, RCOLS + 5120, RCOLS + 6144
    with ExitStack() as pg:
        s.stack = pg
        cw = s.sb("cw_sb", [128, 32, 4], F32)
        dtp = s.sb("dtp_sb", [64, 2], F32)
        Aneg = s.sb("Aneg", [64, 1], F32)
        dbc = s.sb("dbc_sb", [128, 256], F32)
        nwb = s.sb("nwb_sb", [128, 256], F32)
        gA = s.sb("gA", [128, TT], F32)
        gB = s.sb("gB", [128, TT], F32)
        cumF = s.sb("cumF", [64, TT], F32)
        totF = s.sb("totF", [64, NCK], F32)
        dtTok = s.sb("dtTok", [128, NCK, 64], F32)
        cumTok = s.sb("cumTok", [128, NCK, 64], F32)
        ehTok = s.sb("ehTok", [128, NCK, 64], F32)
        xg = s.sb("xg", [128, 2, TT], BF16)
        Bg = s.sb("Bg", [128, TT], BF16)
        Cg = s.sb("Cg", [128, TT], BF16)
        XTok = s.sb("XTok", [128, NCK, 256], BF16)
        BTok = s.sb("BTok", [128, NCK, 128], BF16)
        dtF = T("dtF_alias_xg", xg[:, :, :].rearrange("p a t -> p (a t)").bitcast(F32)[0:64, :])
        ehF = T("ehF_alias_XTok", XTok[:, :, :].rearrange("p a t -> p (a t)").bitcast(F32)[0:64, :])
        yacc = s.sb("yacc", [128, 32, 256], F32)
        SSf = s.sb("SSf", [128, 256], F32)
        SSb = s.sb("SSb", [128, 256], BF16)
        cbm = [s.sb("cbm%d" % i, [128, 128], BF16) for i in range(2)]
        w_d = [s.sb("wd%d" % i, [128, 128], F32) for i in range(3)]
        w_e = [s.sb("we%d" % i, [128, 128], BF16) for i in range(3)]
        w_eb = [s.sb("web%d" % i, [128, 128], F32) for i in range(3)]
        w_dcb = [s.sb("wdcb%d" % i, [128, 128], BF16) for i in range(3)]
        w_ct = [s.sb("wct%d" % i, [128, 128], BF16) for i in range(3)]
        w_bh = [s.sb("wbh%d" % i, [128, 128], BF16) for i in range(3)]
        fy = [s.sb("fy%d" % i, [128, 256], F32) for i in range(2)]
        fz = [s.sb("fz%d" % i, [128, 256], F32) for i in range(2)]
        fo = [s.sb("fo%d" % i, [128, 256], BF16) for i in range(2)]
        fsq = s.sb("fsq", [128, 256], BF16)
        fst = [s.sb("fst%d" % i, [128, 2], F32) for i in range(2)]
        gc = {"bank": 0, "w": 0, "ev": 0}

        def gbank():
            gc["bank"] += 1
            return pb[gc["bank"] % 7]

        def gev(dst_t, dst_ap, src_t, src_ap):
            gc["ev"] += 1
            if gc["ev"] % 2 == 0:
                s.op("act", lambda e: e.copy(out=dst_ap, in_=src_ap), reads=[src_t], writes=[dst_t])
            else:
                s.op("dve", lambda e: e.tensor_copy(out=dst_ap, in_=src_ap), reads=[src_t], writes=[dst_t])

        ptr = pb[7]
        ptrb = ptr[:, :].bitcast(BF16)
        s.dma("sp", cw[:], cw_in[:, :, :], reads=[cw_in], writes=[cw])
        s.dma("sp", dtp[:], dtp_in[:, :], reads=[dtp_in], writes=[dtp])
        s.op("act", lambda e: e.activation(out=Aneg[:], in_=dtp[:, 1:2], func=AF.Exp), reads=[dtp], writes=[Aneg])
        s.op("dve", lambda e: e.tensor_scalar(out=Aneg[:], in0=Aneg[:], scalar1=-1.0, scalar2=None, op0=ALU.mult),
             reads=[Aneg], writes=[Aneg])
        s.dma("sp", gA[0:64, :], colsT[DTR0:DTR0 + 64, :], reads=[colsT], writes=[gA])
        s.op("act", lambda e: e.activation(out=gB[0:64, :], in_=gA[0:64, :], func=AF.Exp, bias=dtp[:, 0:1]),
             reads=[gA, dtp], writes=[gB])
        s.op("act", lambda e: e.activation(out=dtF[:], in_=gB[0:64, :], func=AF.Ln, bias=1.0), reads=[gB], writes=[dtF])
        s.op("dve", lambda e: e.tensor_scalar(out=gA[0:64, :], in0=dtF[:], scalar1=Aneg[:, 0:1], scalar2=None, op0=ALU.mult),
             reads=[dtF, Aneg], writes=[gA])
        s.op("pool", lambda e: e.memset(gB[0:64, :], 1.0), reads=[], writes=[gB])
        s.op("pool", lambda e: e.memset(gB[0:64, :].rearrange("p (c t) -> p c t", t=128)[:, :, 0:1], 0.0), writes=[gB])
        s.op("dve", lambda e: e.tensor_tensor_scan(out=cumF[:], data0=gB[0:64, :], data1=gA[0:64, :], initial=0.0,
                                                   op0=ALU.mult, op1=ALU.add), reads=[gB, gA], writes=[cumF])
        cum3 = cumF[:, :].rearrange("p (c t) -> p c t", t=128)
        s.op("dve", lambda e: e.tensor_copy(out=totF[:], in_=cum3[:, :, 127]), reads=[cumF], writes=[totF])
        s.op("dve", lambda e: e.tensor_tensor(out=cumF[32:64, :], in0=gA[32:64, :], in1=cumF[32:64, :], op=ALU.subtract),
             reads=[gA, cumF], writes=[cumF])
        s.op("dve", lambda e: e.tensor_tensor(out=cum3[32:64], in0=cum3[32:64],
                                              in1=totF[32:64, :].unsqueeze(2).to_broadcast([32, NCK, 128]), op=ALU.add),
             reads=[cumF, totF], writes=[cumF])
        eh3 = ehF[:, :].rearrange("p (c t) -> p c t", t=128)
        s.op("dve", lambda e: e.tensor_tensor(out=eh3, in0=totF[:, :].unsqueeze(2).to_broadcast([64, NCK, 128]), in1=cum3,
                                              op=ALU.subtract), reads=[cumF, totF], writes=[ehF])
        s.op("act", lambda e: e.activation(out=ehF[:], in_=ehF[:], func=AF.Exp), reads=[ehF], writes=[ehF])
        s.op("dve", lambda e: e.tensor_tensor(out=ehF[:], in0=ehF[:], in1=dtF[:], op=ALU.mult), reads=[ehF, dtF], writes=[ehF])
        for srcF, dstT in ((dtF, dtTok), (cumF, cumTok), (ehF, ehTok)):
            for q in range((NCK + 7) // 8):
                n8 = min(8, NCK - q * 8)
                pp = gbank()
                for j in range(n8):
                    ci = q * 8 + j
                    s.op("pe", lambda e: e.transpose(out=pp[:, j * 64:(j + 1) * 64], in_=srcF[:, ci * 128:(ci + 1) * 128],
                                                     identity=ident_f[0:64, 0:64]), reads=[srcF, ident_f], writes=[pp])
                gev(dstT, dstT[:, q * 8:q * 8 + n8, :], pp, pp[:, 0:n8 * 64].rearrange("p (j t) -> p j t", j=n8))

        s.barrier()
        ysg_ap = xg[:, :, 0:SEQ]
        zg_ap = gA[:, :].bitcast(BF16)[:, 0:2 * SEQ].rearrange("p (i t) -> p i t", i=2)

        def conv_silu(row0, ch, dst_t, dst_ap):
            s.dma("sp", gA[:], colsT[row0:row0 + 128, :], reads=[colsT], writes=[gA])
            s.op("dve", lambda e: e.tensor_scalar(out=gB[:], in0=gA[:], scalar1=cw[:, ch, 1:2], scalar2=cw[:, ch, 3:4],
                                                  op0=ALU.mult, op1=ALU.add), reads=[gA, cw], writes=[gB])
            for (a, b) in ((0, CTX), (CTX, TT)):
                s.op("dve", lambda e: e.scalar_tensor_tensor(out=gB[:, a + 1:b], in0=gA[:, a:b - 1], scalar=cw[:, ch, 0:1],
                                                             in1=gB[:, a + 1:b], op0=ALU.mult, op1=ALU.add),
                     reads=[gA, cw, gB], writes=[gB])
                s.op("dve", lambda e: e.scalar_tensor_tensor(out=gB[:, a:b - 1], in0=gA[:, a + 1:b], scalar=cw[:, ch, 2:3],
                                                             in1=gB[:, a:b - 1], op0=ALU.mult, op1=ALU.add),
                     reads=[gA, cw, gB], writes=[gB])
            s.op("act", lambda e: e.activation(out=dst_ap, in_=gB[:], func=AF.Silu), reads=[gB], writes=[dst_t])

        for g in range(0 if skip_mixer else 8):
            for i in range(2):
                conv_silu(XR0 + g * 256 + i * 128, g * 2 + i, xg, xg[:, i, :])
            conv_silu(BR0 + g * 128, 16 + g, Bg, Bg[:, :])
            conv_silu(CR0 + g * 128, 24 + g, Cg, Cg[:, :])
            for q in range((NCK + 3) // 4):
                n4 = min(4, NCK - q * 4)
                for j in range(n4):
                    ci = q * 4 + j
                    for i in range(2):
                        s.op("pe", lambda e: e.transpose(out=ptrb[:, (j * 2 + i) * 128:(j * 2 + i + 1) * 128],
                                                         in_=xg[:, i, ci * 128:(ci + 1) * 128], identity=ident[:]),
                             reads=[xg, ident], writes=[ptr])
                gev(XTok, XTok[:, q * 4:q * 4 + n4, :], ptr, ptrb[:, 0:n4 * 256].rearrange("p (j t) -> p j t", j=n4))
            for q in range((NCK + 7) // 8):
                n8 = min(8, NCK - q * 8)
                for j in range(n8):
                    ci = q * 8 + j
                    s.op("pe", lambda e: e.transpose(out=ptrb[:, j * 128:(j + 1) * 128], in_=Bg[:, ci * 128:(ci + 1) * 128],
                                                     identity=ident[:]), reads=[Bg, ident], writes=[ptr])
                gev(BTok, BTok[:, q * 8:q * 8 + n8, :], ptr, ptrb[:, 0:n8 * 128].rearrange("p (j t) -> p j t", j=n8))
            for d in range(2):
                s.op("pool", lambda e: e.memset(SSf[:], 0.0), writes=[SSf])
                s.op("pool", lambda e: e.memset(SSb[:], 0.0), writes=[SSb])
                order = list(range(NCK)) if d == 0 else [1, 0] + list(range(NCK - 1, 1, -1))
                mcb = msk[:, 1, :] if d == 0 else msk[:, 3, :]
                for ci in order:
                    tsl = slice(ci * 128, (ci + 1) * 128)
                    pcb = gbank()
                    s.op("pe", lambda e: e.matmul(pcb[:, 0:128], lhsT=Bg[:, tsl], rhs=Cg[:, tsl], start=True, stop=True),
                         reads=[Bg, Cg], writes=[pcb])
                    cb = cbm[gc["w"] % 2]
                    s.op("dve", lambda e: e.tensor_tensor(out=cb[:], in0=pcb[:, 0:128], in1=mcb, op=ALU.mult),
                         reads=[pcb, msk], writes=[cb])
                    py = gbank()
                    pst = gbank()
                    ebs = []
                    for hh in range(4):
                        h = g * 4 + hh
                        dh = d * 32 + h
                        gc["w"] += 1
                        wi = gc["w"] % 3
                        hc = slice(hh * 64, (hh + 1) * 64)
                        pbc = gbank()
                        s.op("pe", lambda e: e.matmul(pbc[:, 0:128], lhsT=ident_f[0:64, dh:dh + 1].to_broadcast([64, 128]),
                                                      rhs=cumF[:, tsl], start=True, stop=True),
                             reads=[ident_f, cumF], writes=[pbc])
                        s.op("dve", lambda e: e.tensor_scalar(out=w_d[wi][:], in0=pbc[:, 0:128], scalar1=cumTok[:, ci, dh:dh + 1],
                                                              scalar2=0.0, op0=ALU.subtract, op1=ALU.min),
                             reads=[pbc, cumTok], writes=[w_d[wi]])
                        s.op("act", lambda e: e.activation(out=w_e[wi][:], in_=w_d[wi][:], func=AF.Exp),
                             reads=[w_d[wi]], writes=[w_e[wi]])
                        s.op("dve", lambda e: e.scalar_tensor_tensor(out=w_dcb[wi][:], in0=w_e[wi][:],
                                                                     scalar=dtTok[:, ci, dh:dh + 1], in1=cb[:],
                                                                     op0=ALU.mult, op1=ALU.mult),
                             reads=[w_e[wi], dtTok, cb], writes=[w_dcb[wi]])
                        s.op("act", lambda e: e.activation(out=w_eb[wi][:], in_=pbc[:, 0:128], func=AF.Exp),
                             reads=[pbc], writes=[w_eb[wi]])
                        s.op("pool", lambda e: e.tensor_tensor(out=w_ct[wi][:], in0=w_eb[wi][:], in1=Cg[:, tsl], op=ALU.mult),
                             reads=[w_eb[wi], Cg], writes=[w_ct[wi]])
                        s.op("pool", lambda e: e.tensor_scalar(out=w_bh[wi][:], in0=BTok[:, ci, :], scalar1=ehTok[:, ci, dh:dh + 1],
                                                               scalar2=None, op0=ALU.mult), reads=[BTok, ehTok], writes=[w_bh[wi]])
                        s.op("pe", lambda e: e.matmul(py[:, hc], lhsT=w_dcb[wi][:], rhs=XTok[:, ci, hc], start=True, stop=False),
                             reads=[w_dcb[wi], XTok], writes=[py])
                        s.op("pe", lambda e: e.matmul(py[:, hc], lhsT=w_ct[wi][:], rhs=SSb[:, hc], start=False, stop=True),
                             reads=[w_ct[wi], SSb], writes=[py])
                        s.op("pe", lambda e: e.matmul(pst[:, hc], lhsT=w_bh[wi][:], rhs=XTok[:, ci, hc], start=True, stop=True),
                             reads=[w_bh[wi], XTok], writes=[pst])
                        ebs.append(w_eb[wi])
                    if ci >= 2:
                        if d == 0:
                            gev(yacc, yacc[:, ci - 2, :], py, py[:, 0:256])
                        else:
                            s.op("dve", lambda e: e.tensor_tensor(out=yacc[:, ci - 2, :], in0=py[:, 0:256], in1=yacc[:, ci - 2, :],
                                                                  op=ALU.add), reads=[py, yacc], writes=[yacc])
                    col = 127 if d == 0 else 0
                    for hh in range(4):
                        hc = slice(hh * 64, (hh + 1) * 64)
                        s.op("dve", lambda e: e.scalar_tensor_tensor(out=SSf[:, hc], in0=SSf[:, hc], scalar=ebs[hh][:, col:col + 1],
                                                                     in1=pst[:, hc], op0=ALU.mult, op1=ALU.add),
                             reads=[SSf, ebs[hh], pst], writes=[SSf])
                    s.op("act", lambda e: e.copy(out=SSb[:], in_=SSf[:]), reads=[SSf], writes=[SSb])
            gsl = slice(g * 256, (g + 1) * 256)
            for i in range(2):
                s.dma("pool", zg_ap[:, i, :], colsT[ZR0 + g * 256 + i * 128:ZR0 + g * 256 + (i + 1) * 128, CTX:TT],
                      reads=[colsT], writes=[gA])
            s.dma("act", dbc[:], dbc_in[0:1, gsl].partition_broadcast(128), reads=[dbc_in], writes=[dbc])
            s.dma("act", nwb[:], nwb_in[0:1, gsl].partition_broadcast(128), reads=[nwb_in], writes=[nwb])
            for lc in range(32):
                y_, z_, o_, st_ = fy[lc % 2], fz[lc % 2], fo[lc % 2], fst[lc % 2]
                s.op("pool", lambda e: e.tensor_tensor(out=y_[:], in0=XTok[:, lc + 2, :], in1=dbc[:], op=ALU.mult),
                     reads=[XTok, dbc], writes=[y_])
                s.op("dve", lambda e: e.tensor_tensor(out=y_[:], in0=y_[:], in1=yacc[:, lc, :], op=ALU.add),
                     reads=[y_, yacc], writes=[y_])
                for i in range(2):
                    s.op("pe", lambda e: e.transpose(out=ptrb[:, i * 128:(i + 1) * 128], in_=zg_ap[:, i, lc * 128:(lc + 1) * 128],
                                                     identity=ident[:]), reads=[gA, ident], writes=[ptr])
                s.op("act", lambda e: e.activation(out=z_[:], in_=ptrb[:, 0:256], func=AF.Silu), reads=[ptr], writes=[z_])
                s.op("dve", lambda e: e.tensor_tensor(out=y_[:], in0=y_[:], in1=z_[:], op=ALU.mult), reads=[y_, z_], writes=[y_])
                s.op("act", lambda e: e.activation(out=fsq[:], in_=y_[:], func=AF.Square, accum_out=st_[:, 0:1]),
                     reads=[y_], writes=[fsq, st_])
                s.op("dve", lambda e: e.tensor_scalar(out=st_[:, 1:2], in0=st_[:, 0:1], scalar1=1.0 / 256, scalar2=EPS,
                                                      op0=ALU.mult, op1=ALU.add), reads=[st_], writes=[st_])
                s.op("act", lambda e: e.activation(out=st_[:, 1:2], in_=st_[:, 1:2], func=AF.Sqrt), reads=[st_], writes=[st_])
                s.op("dve", lambda e: e.reciprocal(out=st_[:, 1:2], in_=st_[:, 1:2]), reads=[st_], writes=[st_])
                s.op("dve", lambda e: e.scalar_tensor_tensor(out=o_[:], in0=y_[:], scalar=st_[:, 1:2], in1=nwb[:],
                                                             op0=ALU.mult, op1=ALU.mult), reads=[y_, st_, nwb], writes=[o_])
                for i in range(2):
                    s.op("pe", lambda e: e.transpose(out=ptrb[:, 256 + i * 128:256 + (i + 1) * 128], in_=o_[:, i * 128:(i + 1) * 128],
                                                     identity=ident[:]), reads=[o_, ident], writes=[ptr])
                gev(xg, ysg_ap[:, :, lc * 128:(lc + 1) * 128], ptr, ptrb[:, 256:512].rearrange("p (i t) -> p i t", i=2))
            for i in range(2):
                r0 = RW + g * 256 + i * 128
                s.dma("sp", yT[r0:r0 + 128, :], ysg_ap[:, i, :], reads=[xg], writes=[yT])
            if dbg == "ssd1":
                break
        s.barrier()
    s.stack = st


    HT = 2048
    xh = din("xh", [HT, D])
    selc_in = din("selc", [128, 2])
    w_out_r = din("w_out_r", [8, 128, 32 * 512])
    norm2_g = din("norm2_g", [1, D])
    final_g = din("final_g", [1, D])
    wq_r = din("wq_r", [16, 128, 32 * 128])
    kT_in = din("kT", [128, 2, 8, 128])
    uT_r = din("uT_r", [128, 128, 32 * 128])
    peer_v = din("peer_v", [16384, D])
    h1D = s.dram("h1D", [HT, D], F32, kind=("ExternalOutput" if dbg in ("h1", "h1t") else "Internal"))
    h2D = s.dram("h2D", [HT, D], F32)
    xn2T = s.dram("xn2T", [D, HT], BF16, kind=("ExternalOutput" if dbg in ("h1", "h1t") else "Internal"))
    WrD = s.dram("WrD", [256, 128, 128], BF16)
    OhD = s.dram("OhD", [256, 128, 128], BF16)
    outD = s.dram("out", [HT, D], F32, kind="ExternalOutput")
    selc = s.sb("selc_sb", [128, 2], F32)
    s.dma("sp", selc[:], selc_in[:, :], reads=[selc_in], writes=[selc])

    with ExitStack() as ph1:
        s.stack = ph1
        wsl = [s.sb("wsl%d" % i, [128, 32, 512], BF16) for i in range(2)]
        yTb = s.sb("yTb", [128, 32, 1024], BF16)
        la = [s.sb("la%d" % i, [128, 1024], BF16) for i in range(2)]
        lb = [s.sb("lb%d" % i, [128, 1024], BF16) for i in range(2)]
        gt1 = s.sb("gt1", [128, D], F32)
        xr = [s.sb("xr%d" % i, [128, 512], F32) for i in range(3)]
        orow = [s.sb("orow%d" % i, [128, 512], F32) for i in range(3)]
        s.dma("act", gt1[:], modx[:, 2 * D:3 * D], reads=[modx], writes=[gt1])
        wi = 0
        oi = 0
        for tb in range(2):
            for kc in range(32):
                a_, b_ = la[kc % 2], lb[kc % 2]
                s.dma("sp", a_[:], yT[kc * 128:(kc + 1) * 128, tb * 1024:(tb + 1) * 1024], reads=[yT], writes=[a_])
                s.dma("act", b_[:], yT[kc * 128:(kc + 1) * 128, HT + tb * 1024:HT + (tb + 1) * 1024], reads=[yT], writes=[b_])
                s.op("dve", lambda e: e.tensor_scalar(out=yTb[:, kc, :], in0=a_[:], scalar1=selc[:, 0:1], scalar2=None, op0=ALU.mult),
                     reads=[a_, selc], writes=[yTb])
                s.op("dve", lambda e: e.scalar_tensor_tensor(out=yTb[:, kc, :], in0=b_[:], scalar=selc[:, 1:2], in1=yTb[:, kc, :],
                                                             op0=ALU.mult, op1=ALU.add), reads=[b_, selc, yTb], writes=[yTb])
            for dg in range(8):
                w_ = wsl[wi % 2]; wi += 1
                s.dma("pool", w_[:], w_out_r[dg].rearrange("p (k c) -> p k c", k=32), reads=[w_out_r], writes=[w_])
                for tl in range(8):
                    pp = pb[oi % 4]
                    for kc in range(32):
                        s.op("pe", lambda e: e.matmul(pp[:, :], lhsT=yTb[:, kc, tl * 128:(tl + 1) * 128], rhs=w_[:, kc, :],
                                                      start=(kc == 0), stop=(kc == 31)), reads=[yTb, w_], writes=[pp])
                    r0 = tb * 1024 + tl * 128
                    x_ = xr[oi % 3]; o_ = orow[oi % 3]; oi += 1
                    s.dma("act", x_[:], xh[r0:r0 + 128, dg * 512:(dg + 1) * 512], reads=[xh], writes=[x_])
                    s.op("dve", lambda e: e.tensor_tensor(out=o_[:], in0=pp[:, :], in1=gt1[:, dg * 512:(dg + 1) * 512], op=ALU.mult),
                         reads=[pp, gt1], writes=[o_])
                    s.op("pool", lambda e: e.tensor_tensor(out=o_[:], in0=o_[:], in1=x_[:], op=ALU.add), reads=[o_, x_], writes=[o_])
                    s.dma("sp", h1D[r0:r0 + 128, dg * 512:(dg + 1) * 512], o_[:], reads=[o_], writes=[h1D])
        s.barrier()
    s.stack = st

    with ExitStack() as ph2:
        s.stack = ph2
        G2 = s.sb("G2", [128, D], F32)
        SH2 = s.sb("SH2", [128, D], F32)
        g2b_ = s.sb("g2bb", [128, D], F32)
        hx = [s.sb("hx%d" % i, [128, D], F32) for i in range(2)]
        hn = [s.sb("hn%d" % i, [128, D], BF16) for i in range(2)]
        sq2 = s.sb("sq2", [128, D], BF16)
        ss2 = [s.sb("ss2_%d" % i, [128, 2], F32) for i in range(2)]
        nT2 = [s.sb("nT2_%d" % i, [128, 32, 512], BF16) for i in range(2)]
        s.dma("sp", g2b_[:], norm2_g[0:1, :].partition_broadcast(128), reads=[norm2_g], writes=[g2b_])
        s.dma("sp", G2[:], modx[:, 4 * D:5 * D], reads=[modx], writes=[G2])
        s.dma("act", SH2[:], modx[:, 3 * D:4 * D], reads=[modx], writes=[SH2])
        s.op("dve", lambda e: e.scalar_tensor_tensor(out=G2[:], in0=G2[:], scalar=1.0, in1=g2b_[:], op0=ALU.add, op1=ALU.mult),
             reads=[G2, g2b_], writes=[G2])
        ev = 0
        for tl in range(16):
            x_, n_, s_ = hx[tl % 2], hn[tl % 2], ss2[tl % 2]
            nt = nT2[(tl // 4) % 2]
            s.dma("sp", x_[:], h1D[tl * 128:(tl + 1) * 128, :], reads=[h1D], writes=[x_])
            s.op("act", lambda e: e.activation(out=sq2[:], in_=x_[:], func=AF.Square, accum_out=s_[:, 0:1]), reads=[x_], writes=[sq2, s_])
            s.op("dve", lambda e: e.tensor_scalar(out=s_[:, 1:2], in0=s_[:, 0:1], scalar1=1.0 / D, scalar2=EPS, op0=ALU.mult, op1=ALU.add),
                 reads=[s_], writes=[s_])
            s.op("act", lambda e: e.activation(out=s_[:, 1:2], in_=s_[:, 1:2], func=AF.Sqrt), reads=[s_], writes=[s_])
            s.op("dve", lambda e: e.reciprocal(out=s_[:, 1:2], in_=s_[:, 1:2]), reads=[s_], writes=[s_])
            s.op("dve", lambda e: e.scalar_tensor_tensor(out=x_[:], in0=x_[:], scalar=s_[:, 1:2], in1=G2[:], op0=ALU.mult, op1=ALU.mult),
                 reads=[x_, s_, G2], writes=[x_])
            s.op("pool", lambda e: e.tensor_tensor(out=n_[:], in0=x_[:], in1=SH2[:], op=ALU.add), reads=[x_, SH2], writes=[n_])
            for q4 in range(4):
                pt = pb[4 + (ev % 2)]; ev += 1
                ptb = pt[:, :].bitcast(BF16)
                for j in range(8):
                    kc = q4 * 8 + j
                    s.op("pe", lambda e: e.transpose(out=ptb[:, j * 128:(j + 1) * 128], in_=n_[:, kc * 128:(kc + 1) * 128], identity=ident[:]),
                         reads=[n_, ident], writes=[pt])
                dst = nt[:, q4 * 8:(q4 + 1) * 8, (tl % 4) * 128:(tl % 4 + 1) * 128]
                srcp = ptb.rearrange("p (j t) -> p j t", j=8)
                if q4 % 2 == 0:
                    s.op("act", lambda e: e.copy(out=dst, in_=srcp), reads=[pt], writes=[nt])
                else:
                    s.op("dve", lambda e: e.tensor_copy(out=dst, in_=srcp), reads=[pt], writes=[nt])
            if tl % 4 == 3:
                t0 = (tl // 4) * 512
                for kq in range(4):
                    s.dma("sp", xn2T[kq * 1024:(kq + 1) * 1024, t0:t0 + 512].rearrange("(k p) t -> p k t", p=128),
                          nt[:, kq * 8:(kq + 1) * 8, :], reads=[nt], writes=[xn2T])
        s.barrier()
    s.stack = st

    if dbg not in ("h1", "h1t"):
      with ExitStack() as pp_:
        s.stack = pp_
        TB = 256
        xb = s.sb("xb", [128, 32, TB], BF16)
        WT = s.sb("WT", [128, 128, TB], BF16)
        wb3 = [s.sb("wb3_%d" % i, [128, 4096], BF16) for i in range(3)]
        qT = s.sb("qT", [128, 16, TB], BF16)
        kT = s.sb("kT_sb", [128, 2, 8, 128], BF16)
        sc = s.sb("sc", [128, 16, 128], F32)
        vv = s.sb("vv", [128, 16, 16], F32)
        cand = s.sb("cand", [128, 8, 256], F32)
        ec = s.sb("ec", [128, 8, 256], F32)
        E2 = s.sb("E2", [128, 8, 128], F32)
        tmpk = s.sb("tmpk", [128, 256], F32)
        m8 = s.sb("m8", [128, 8], F32)
        th = s.sb("th", [128, 8], F32)
        negm = s.sb("negm", [128, 8, 3], F32)
        Zs = s.sb("Zs", [128, 8], F32)
        c1 = s.sb("c1", [128, 8, 16], F32)
        thr = s.sb("thr", [128, 8, 16], F32)
        wst = [s.sb("wst%d" % i, [128, 16, 128], BF16) for i in range(2)]
        ost = [s.sb("ost%d" % i, [128, 16, 128], BF16) for i in range(2)]
        Wk = s.sb("Wk", [128, 16, 128], BF16)
        Ok = s.sb("Ok", [128, 16, 128], BF16)
        gl = [s.sb("gl%d" % i, [128, TB], BF16) for i in range(2)]
        hp_ = [s.sb("hp%d" % i, [128, 512], F32) for i in range(3)]
        gp_ = [s.sb("gp%d" % i, [128, 512], F32) for i in range(2)]
        op_ = [s.sb("op%d" % i, [128, 512], F32) for i in range(3)]
        s.dma("pool", kT[:], kT_in[:, :, :, :], reads=[kT_in], writes=[kT])
        pc = {"w": 0, "bank": 0, "ev": 0, "o": 0}

        def pbank():
            pc["bank"] += 1
            return pb[pc["bank"] % 8]

        def wbuf():
            pc["w"] += 1
            return wb3[pc["w"] % 3]

        def pev(dst_t, dst_ap, src_t, src_ap):
            pc["ev"] += 1
            if pc["ev"] % 2 == 0:
                s.op("act", lambda e: e.copy(out=dst_ap, in_=src_ap), reads=[src_t], writes=[dst_t])
            else:
                s.op("dve", lambda e: e.tensor_copy(out=dst_ap, in_=src_ap), reads=[src_t], writes=[dst_t])

        NBLK = HT // TB if dbg != "peer1" else 1
        for blk_i in range(NBLK):
            t0 = blk_i * TB
            for kq in range(4):
                s.dma("sp", xb[:, kq * 8:(kq + 1) * 8, :],
                      xn2T[kq * 1024:(kq + 1) * 1024, t0:t0 + TB].rearrange("(k p) t -> p k t", p=128), reads=[xn2T], writes=[xb])
            for qc in range(16):
                w_ = wbuf()
                wv = w_[:, :].rearrange("p (k c) -> p k c", k=32)
                s.dma("pool", w_[:], wq_r[qc], reads=[wq_r], writes=[w_])
                pp = pbank()
                for kc in range(32):
                    s.op("pe", lambda e: e.matmul(pp[:, 0:TB], lhsT=wv[:, kc, :], rhs=xb[:, kc, :], start=(kc == 0), stop=(kc == 31)),
                         reads=[w_, xb], writes=[pp])
                pev(qT, qT[:, qc, :], pp, pp[:, 0:TB])
            for tile in range(TB // 128):
                tt = tile * 128
                for q4 in range(4):
                    pp = pbank()
                    for j in range(4):
                        hw = q4 * 4 + j
                        s.op("pe", lambda e: e.matmul(pp[:, j * 128:(j + 1) * 128], lhsT=qT[:, hw, tt:tt + 128],
                                                      rhs=kT[:, hw % 2, hw // 2, :], start=True, stop=True), reads=[qT, kT], writes=[pp])
                    pev(sc, sc[:, q4 * 4:(q4 + 1) * 4, :], pp, pp[:, :].rearrange("p (j k) -> p j k", j=4))
                for hw in range(16):
                    s.op("dve", lambda e: e.max(out=vv[:, hw, 0:8], in_=sc[:, hw, :]), reads=[sc], writes=[vv])
                    s.op("dve", lambda e: e.match_replace(out=tmpk[:, 0:128], in_to_replace=vv[:, hw, 0:8], in_values=sc[:, hw, :],
                                                          imm_value=-1e30), reads=[sc, vv], writes=[tmpk])
                    s.op("dve", lambda e: e.max(out=vv[:, hw, 8:16], in_=tmpk[:, 0:128]), reads=[tmpk], writes=[vv])
                v4 = vv[:, :, :].rearrange("p (h w) r -> p h w r", w=2)
                cand4 = cand[:, :, :].rearrange("p h (i j) -> p h i j", j=16)
                s.op("dve", lambda e: e.tensor_tensor(out=cand4, in0=v4[:, :, 0, :].unsqueeze(3).to_broadcast([128, 8, 16, 16]),
                                                      in1=v4[:, :, 1, :].unsqueeze(2).to_broadcast([128, 8, 16, 16]), op=ALU.add),
                     reads=[vv], writes=[cand])
                for h in range(8):
                    s.op("dve", lambda e: e.max(out=m8[:], in_=cand[:, h, :]), reads=[cand], writes=[m8])
                    s.op("dve", lambda e: e.match_replace(out=tmpk[:], in_to_replace=m8[:], in_values=cand[:, h, :], imm_value=-1e30),
                         reads=[cand, m8], writes=[tmpk])
                    s.op("dve", lambda e: e.max(out=m8[:], in_=tmpk[:]), reads=[tmpk], writes=[m8])
                    s.op("dve", lambda e: e.tensor_copy(out=th[:, h:h + 1], in_=m8[:, 7:8]), reads=[m8], writes=[th])
                s.op("dve", lambda e: e.tensor_scalar(out=negm[:, :, 0:2], in0=v4[:, :, :, 0], scalar1=-1.0, scalar2=None, op0=ALU.mult),
                     reads=[vv], writes=[negm])
                s.op("dve", lambda e: e.tensor_tensor(out=negm[:, :, 2], in0=negm[:, :, 0], in1=negm[:, :, 1], op=ALU.add),
                     reads=[negm], writes=[negm])
                for h in range(8):
                    s.op("act", lambda e: e.activation(out=ec[:, h, :], in_=cand[:, h, :], func=AF.Exp, bias=negm[:, h, 2:3]),
                         reads=[cand, negm], writes=[ec])
                    s.op("dve", lambda e: e.scalar_tensor_tensor(out=tmpk[:], in0=cand[:, h, :], scalar=th[:, h:h + 1], in1=ec[:, h, :],
                                                                 op0=ALU.is_ge, op1=ALU.mult, accum_out=Zs[:, h:h + 1]),
                         reads=[cand, th, ec], writes=[tmpk, Zs])
                    s.op("act", lambda e: e.activation(out=E2[:, h, :], in_=sc[:, 2 * h + 1, :], func=AF.Exp, bias=negm[:, h, 1:2]),
                         reads=[sc, negm], writes=[E2])
                s.op("dve", lambda e: e.reciprocal(out=Zs[:], in_=Zs[:]), reads=[Zs], writes=[Zs])
                s.op("dve", lambda e: e.tensor_tensor(out=c1[:], in0=v4[:, :, 0, :], in1=negm[:, :, 0:1].to_broadcast([128, 8, 16]), op=ALU.add),
                     reads=[vv, negm], writes=[c1])
                s.op("act", lambda e: e.activation(out=c1[:], in_=c1[:], func=AF.Exp), reads=[c1], writes=[c1])
                s.op("dve", lambda e: e.tensor_tensor(out=c1[:], in0=c1[:], in1=Zs[:, :].unsqueeze(2).to_broadcast([128, 8, 16]), op=ALU.mult),
                     reads=[c1, Zs], writes=[c1])
                s.op("dve", lambda e: e.tensor_tensor(out=thr[:], in0=th[:, :].unsqueeze(2).to_broadcast([128, 8, 16]), in1=v4[:, :, 0, :],
                                                      op=ALU.subtract), reads=[th, vv], writes=[thr])
                for h in range(8):
                    ws_, os_ = wst[h % 2], ost[h % 2]
                    for r in range(16):
                        s.op("dve", lambda e: e.scalar_tensor_tensor(out=ws_[:, r, :], in0=sc[:, 2 * h + 1, :], scalar=thr[:, h, r:r + 1],
                                                                     in1=E2[:, h, :], op0=ALU.is_ge, op1=ALU.mult),
                             reads=[sc, thr, E2], writes=[ws_])
                        s.op("pool", lambda e: e.tensor_scalar(out=os_[:, r, :], in0=sc[:, 2 * h, :], scalar1=vv[:, 2 * h, r:r + 1],
                                                               scalar2=c1[:, h, r:r + 1], op0=ALU.is_equal, op1=ALU.mult),
                             reads=[sc, vv, c1], writes=[os_])
                    s.dma("sp", WrD[tt:tt + 128, h * 16:(h + 1) * 16, :], ws_[:], reads=[ws_], writes=[WrD])
                    s.dma("act", OhD[tt:tt + 128, h * 16:(h + 1) * 16, :], os_[:], reads=[os_], writes=[OhD])
                for sub in range(8):
                    tk = tt + sub * 16
                    s.dma("sp", Wk[:], WrD[tk:tk + 16].rearrange("t k e -> k t e"), reads=[WrD], writes=[Wk])
                    s.dma("act", Ok[:], OhD[tk:tk + 16].rearrange("t k e -> k t e"), reads=[OhD], writes=[Ok])
                    for q in range(4):
                        pp = pbank()
                        for j in range(4):
                            t = q * 4 + j
                            s.op("pe", lambda e: e.matmul(pp[:, j * 128:(j + 1) * 128], lhsT=Wk[:, t, :], rhs=Ok[:, t, :], start=True, stop=True),
                                 reads=[Wk, Ok], writes=[pp])
                        pev(WT, WT[:, :, tk + q * 4:tk + q * 4 + 4], pp, pp[:, :].rearrange("p (t e) -> p e t", t=4))
            for e1 in range(128):
                w_ = wbuf()
                wv = w_[:, :].rearrange("p (k c) -> p k c", k=32)
                s.dma("pool", w_[:], uT_r[e1], reads=[uT_r], writes=[w_])
                pp = pbank()
                for kc in range(32):
                    s.op("pe", lambda e: e.matmul(pp[:, 0:TB], lhsT=wv[:, kc, :], rhs=xb[:, kc, :], start=(kc == 0), stop=(kc == 31)),
                         reads=[w_, xb], writes=[pp])
                g_ = gl[e1 % 2]
                s.op("act", lambda e: e.activation(out=g_[:], in_=pp[:, 0:TB], func=AF.Gelu), reads=[pp], writes=[g_])
                s.op("dve", lambda e: e.tensor_tensor(out=WT[:, e1, :], in0=WT[:, e1, :], in1=g_[:], op=ALU.mult), reads=[WT, g_], writes=[WT])
            for dh in range(2):
                for e1 in range(128):
                    w_ = wbuf()
                    s.dma("pool", w_[:, 0:2048], peer_v[e1 * 128:(e1 + 1) * 128, dh * 2048:(dh + 1) * 2048], reads=[peer_v], writes=[w_])
                    for tile in range(2):
                        for dg in range(4):
                            pp = pb[tile * 4 + dg]
                            s.op("pe", lambda e: e.matmul(pp[:, :], lhsT=WT[:, e1, tile * 128:(tile + 1) * 128], rhs=w_[:, dg * 512:(dg + 1) * 512],
                                                          start=(e1 == 0), stop=(e1 == 127)), reads=[WT, w_], writes=[pp])
                for dg in range(4):
                    c0 = dh * 2048 + dg * 512
                    g2_ = gp_[dg % 2]
                    s.dma("act", g2_[:], modx[:, 5 * D + c0:5 * D + c0 + 512], reads=[modx], writes=[g2_])
                    for tile in range(2):
                        pp = pb[tile * 4 + dg]
                        r0 = t0 + tile * 128
                        h_ = hp_[pc["o"] % 3]; o_ = op_[pc["o"] % 3]; pc["o"] += 1
                        s.dma("sp", h_[:], h1D[r0:r0 + 128, c0:c0 + 512], reads=[h1D], writes=[h_])
                        s.op("dve", lambda e: e.tensor_tensor(out=o_[:], in0=pp[:, :], in1=g2_[:], op=ALU.mult), reads=[pp, g2_], writes=[o_])
                        s.op("pool", lambda e: e.tensor_tensor(out=o_[:], in0=o_[:], in1=h_[:], op=ALU.add), reads=[o_, h_], writes=[o_])
                        s.dma("sp", h2D[r0:r0 + 128, c0:c0 + 512], o_[:], reads=[o_], writes=[h2D])
        s.barrier()
      s.stack = st

      with ExitStack() as pf:
        s.stack = pf
        fgb = s.sb("fgb", [128, D], F32)
        fx = [s.sb("fx%d" % i, [128, D], F32) for i in range(2)]
        fsq2 = s.sb("fsq2", [128, D], BF16)
        fs = [s.sb("fs%d" % i, [128, 2], F32) for i in range(2)]
        s.dma("sp", fgb[:], final_g[0:1, :].partition_broadcast(128), reads=[final_g], writes=[fgb])
        for tl in range(16 if dbg != "peer1" else 2):
            x_, s_ = fx[tl % 2], fs[tl % 2]
            s.dma("sp", x_[:], h2D[tl * 128:(tl + 1) * 128, :], reads=[h2D], writes=[x_])
            s.op("act", lambda e: e.activation(out=fsq2[:], in_=x_[:], func=AF.Square, accum_out=s_[:, 0:1]), reads=[x_], writes=[fsq2, s_])
            s.op("dve", lambda e: e.tensor_scalar(out=s_[:, 1:2], in0=s_[:, 0:1], scalar1=1.0 / D, scalar2=EPS, op0=ALU.mult, op1=ALU.add),
                 reads=[s_], writes=[s_])
            s.op("act", lambda e: e.activation(out=s_[:, 1:2], in_=s_[:, 1:2], func=AF.Sqrt), reads=[s_], writes=[s_])
            s.op("dve", lambda e: e.reciprocal(out=s_[:, 1:2], in_=s_[:, 1:2]), reads=[s_], writes=[s_])
            s.op("dve", lambda e: e.scalar_tensor_tensor(out=x_[:], in0=x_[:], scalar=s_[:, 1:2], in1=fgb[:], op0=ALU.mult, op1=ALU.mult),
                 reads=[x_, s_, fgb], writes=[x_])
            s.dma("act", outD[tl * 128:(tl + 1) * 128, :], x_[:], reads=[x_], writes=[outD])
        s.barrier()
      s.stack = st

    s.finish()
    st.close()
    return nc


def make_shared(inp):
    f = lambda a: np.ascontiguousarray(np.asarray(a, dtype=np.float32))
    m = {}
    m["w_mod"] = f(inp["w_mod"][0])
    m["b_mod"] = f(inp["b_mod"])
    m["norm1_g"] = f(inp["norm1_g"])
    w = np.zeros((D, NCOLP), np.float32)
    w[:, :NCOLS] = np.asarray(inp["w_in"][0])
    m["w_in_r"] = f(w.reshape(32, 128, NCH, 128).transpose(2, 1, 0, 3).reshape(NCH, 128, 32 * 128))
    m["ident_in"] = np.eye(128, dtype=np.float32)
    mu = np.asarray(inp["rwkv_mu"][0])
    mu_u = np.zeros((128, 54), np.float32)
    for which in range(3):
        for hp in range(16):
            mu_u[:, which * 16 + hp] = mu[which * RW + hp * 128: which * RW + (hp + 1) * 128]
    for d in range(2):
        mu_u[:96, 48 + d] = mu[6144 + 96 * d: 6144 + 96 * (d + 1)]
        mu_u[:96, 50 + d] = mu[6336 + 96 * d: 6336 + 96 * (d + 1)]
        mu_u[:, 52 + d] = mu[6528 + 128 * d: 6528 + 128 * (d + 1)]
    m["mu_u"] = mu_u
    p = np.arange(128)
    slotm = np.zeros((128, 6), np.float32)
    for k in range(4):
        slotm[:, k] = (p % 4 == k)
    for k in range(2):
        slotm[:, 4 + k] = (p % 2 == k)
    m["slotm"] = slotm
    m["w2r"] = f(np.asarray(inp["rwkv_w2"][0]).transpose(1, 0, 2))
    m["a2r"] = f(np.asarray(inp["rwkv_a2"][0]).transpose(1, 0, 2))
    m["g2r"] = f(np.asarray(inp["rwkv_g2"][0]).reshape(2, 128, RW).transpose(1, 0, 2))
    cols = [np.asarray(inp["rwkv_w0"][0])[0], np.asarray(inp["rwkv_w0"][0])[1],
            np.asarray(inp["rwkv_a0"][0])[0], np.asarray(inp["rwkv_a0"][0])[1],
            np.asarray(inp["rwkv_k_k"][0]), np.asarray(inp["rwkv_k_a"][0]),
            np.asarray(inp["rwkv_r_k"][0]).reshape(RW), np.asarray(inp["rwkv_ln_w"][0]), np.asarray(inp["rwkv_ln_b"][0])]
    m["chp"] = f(np.stack([c.reshape(16, 128).T for c in cols], axis=2))
    o = np.ones((128, 128), np.float32)
    m["msk"] = f(np.stack([np.triu(o, 1), np.triu(o, 0), np.tril(o, -1), np.tril(o, 0)], axis=1))
    m["blk"] = f((p[:, None] // 64 == p[None, :] // 64))
    cwt = np.concatenate([np.asarray(inp["ssm_conv_w"][0]), np.asarray(inp["ssm_conv_b"])], axis=0)
    m["cw"] = f(cwt.reshape(4, 32, 128).transpose(2, 1, 0))
    m["dtp"] = f(np.stack([np.asarray(inp["ssm_dt_bias"][0]).reshape(64), np.asarray(inp["ssm_a_log"][0]).reshape(64)], axis=1))
    m["dbc"] = f(np.repeat(np.asarray(inp["ssm_d"][0]), 64)[None, :])
    m["nwb"] = f(np.asarray(inp["ssm_norm_w"]))
    m["w_out_r"] = f(np.asarray(inp["w_out"][0]).reshape(32, 128, 8, 512).transpose(2, 1, 0, 3).reshape(8, 128, 32 * 512))
    m["norm2_g"] = f(inp["norm2_g"])
    m["final_g"] = f(np.asarray(inp["final_g"])[None, :])
    m["wq_r"] = f(np.asarray(inp["peer_wq"][0]).reshape(32, 128, 16, 128).transpose(2, 1, 0, 3).reshape(16, 128, 32 * 128))
    k1 = np.asarray(inp["peer_k1"][0]); k2 = np.asarray(inp["peer_k2"][0])
    m["kT"] = f(np.stack([k1.transpose(2, 0, 1), k2.transpose(2, 0, 1)], axis=1))
    u = np.asarray(inp["peer_u"][0])
    m["uT_r"] = f(u.reshape(128, 128, 32, 128).transpose(0, 3, 2, 1).reshape(128, 128, 32 * 128))
    m["peer_v"] = f(inp["peer_v"][0])
    return m


def make_inputs(inp, core, shared=None):
    b, hf = core // 2, core % 2
    f = lambda a: np.ascontiguousarray(np.asarray(a, dtype=np.float32))
    m = dict(shared if shared is not None else make_shared(inp))
    m["xs"] = f(np.concatenate([inp["ctx"][b], inp["x"][b]], axis=0))
    cc = np.stack([np.asarray(inp["c"])[b].reshape(32, 128).T, np.asarray(inp["c_ctx"]).reshape(32, 128).T], axis=1)
    m["cc"] = f(cc)
    m["xh"] = f(np.asarray(inp["x"])[b, hf * 2048:(hf + 1) * 2048])
    sel = np.zeros((128, 2), np.float32); sel[:, hf] = 1.0
    m["selc"] = sel
    return m


def kernel(**inputs):
    nc = build_program()
    shared = make_shared(inputs)
    in_maps = [make_inputs(inputs, c, shared) for c in range(8)]
    res = run_bass_kernel_spmd(nc, in_maps, core_ids=list(range(8)))
    out = np.zeros((4, SEQ, D), np.float32)
    for c in range(8):
        b, hf = c // 2, c % 2
        out[b, hf * 2048:(hf + 1) * 2048] = res.results[c]["out"]
    return out
```

```python
import numpy as np
from contextlib import ExitStack
import concourse.bass as bass
import concourse.mybir as mybir
from concourse.bass_utils import run_bass_kernel_spmd

F32 = mybir.dt.float32
BF16 = mybir.dt.bfloat16
AF = mybir.ActivationFunctionType
ALU = mybir.AluOpType
AX = mybir.AxisListType

D = 4096
SEQ = 4096
CTX = 256
TT = SEQ + CTX
NCOLS = 12992
NCH = 102
NCOLP = NCH * 128
RW = 2048
RCOLS = 6784
EPS = 1e-6


class T:
    __slots__ = ("name", "ap", "lw", "rd", "dsem", "dcnt", "excl")

    def __init__(self, name, ap):
        self.name = name
        self.ap = ap
        self.lw = []
        self.rd = {}
        self.dsem = None
        self.dcnt = 0
        self.excl = False

    def __getitem__(self, idx):
        return self.ap[idx]


class S:
    def __init__(self, nc, stack):
        self.nc = nc
        self.stack = stack
        self.eng = {"pe": nc.tensor, "dve": nc.vector, "act": nc.scalar, "pool": nc.gpsimd, "sp": nc.sync}
        self.sems = {}
        self.cnt = {}
        self.known = {e: {} for e in self.eng}
        for e in self.eng:
            self.sems[e] = stack.enter_context(nc.semaphore("s_" + e))
            self.cnt[e] = 0
        self.semobj = dict(self.sems)
        self.all_dma_events = {}
        self.n_inst = 0
        self.uid = 0
        self.semstack = stack
        self.dpool = []
        self.dsc = {}
        self.downers = []

    def sb(self, name, shape, dt):
        t = self.stack.enter_context(self.nc.sbuf_tensor(name, list(shape), dt))
        return T(name, t)

    def ps(self, name, shape, dt=F32):
        t = self.stack.enter_context(self.nc.psum_tensor(name, list(shape), dt))
        tt = T(name, t)
        tt.excl = True
        return tt

    def dram(self, name, shape, dt, kind="Internal"):
        t = self.nc.dram_tensor(name, list(shape), dt, kind=kind)
        return T(name, t.ap())

    def _wait(self, e, evs):
        eng = self.eng[e]
        kn = self.known[e]
        best = {}
        for (k, v) in evs:
            if v > best.get(k, 0):
                best[k] = v
        for k, v in best.items():
            if kn.get(k, 0) >= v:
                continue
            eng.wait_ge(self.semobj[k], v)
            kn[k] = v

    def _deps(self, reads, writes):
        evs = []
        for t in reads:
            evs.extend(t.lw)
            if t.excl:
                evs.extend(t.rd.items())
        for t in writes:
            evs.extend(t.lw)
            evs.extend(t.rd.items())
        return evs

    def op(self, e, fn, reads=(), writes=()):
        evs = self._deps(reads, writes)
        if e == "pe":
            evs = [(k, v) for (k, v) in evs if k != "pe"]
        self._wait(e, evs)
        ins = fn(self.eng[e])
        self.cnt[e] += 1
        ins.then_inc(self.sems[e], 1)
        ev = (e, self.cnt[e])
        for t in writes:
            t.lw = [ev]
            t.rd = {}
        for t in reads:
            if t not in writes:
                t.rd[e] = self.cnt[e]
        self.n_inst += 1
        return ins

    def dma(self, q, out_ap, in_ap, reads=(), writes=(), **kw):
        evs = self._deps(reads, writes)
        self._wait(q, evs)
        wt = writes[0]
        if wt.dsem is None:
            if self.dpool:
                nm = self.dpool.pop()
            else:
                self.uid += 1
                nm = "dq%d" % self.uid
                self.semobj[nm] = self.semstack.enter_context(self.nc.semaphore(nm))
                self.dsc[nm] = 0
            wt.dsem = nm
            self.downers.append(wt)
        self.dsc[wt.dsem] += 16
        wt.dcnt = self.dsc[wt.dsem]
        ins = self.eng[q].dma_start(out=out_ap, in_=in_ap, **kw)
        ins.then_inc(self.semobj[wt.dsem], 16)
        ev = (wt.dsem, wt.dcnt)
        self.all_dma_events[wt.dsem] = wt.dcnt
        for t in writes:
            t.lw = [ev]
            t.rd = {}
        for t in reads:
            t.rd[wt.dsem] = wt.dcnt
        self.n_inst += 1
        return ins

    def barrier(self):
        evs = [(e, c) for e, c in self.cnt.items() if c > 0]
        evs += list(self.all_dma_events.items())
        for e in self.eng:
            self._wait(e, evs)
        for t in self.downers:
            self.dpool.append(t.dsem)
            t.dsem = None
        self.downers = []

    def finish(self):
        evs = [(e, c) for e, c in self.cnt.items() if c > 0]
        evs += list(self.all_dma_events.items())
        self._wait("sp", evs)
        self._wait("pool", evs)


def build_program(dbg=None):
    nc = bass.Bass("TRN2", target_bir_lowering=False)
    st = ExitStack()
    s = S(nc, st)
    skip_mixer = dbg in ("tail", "peer1", "h1t")

    def din(name, shape, dt=F32):
        return s.dram(name, shape, dt, kind="ExternalInput")

    xs = din("xs", [TT, D])
    cc = din("cc", [128, 2, 32])
    w_mod = din("w_mod", [D, 6 * D])
    b_mod = din("b_mod", [1, 6 * D])
    norm1_g = din("norm1_g", [1, D])
    w_in_r = din("w_in_r", [NCH, 128, 32 * 128])
    ident_in = din("ident_in", [128, 128])

    modx = s.dram("modx", [128, 6 * D], F32)
    modc = s.dram("modc", [128, 2 * D], F32)
    kind = "ExternalOutput" if dbg == "cols" else "Internal"
    colsT = s.dram("colsT", [NCOLP, TT], F32, kind=kind)

    pb = [s.ps("pb%d" % i, [128, 512], F32) for i in range(8)]

    ident_f = s.sb("ident_f", [128, 128], F32)
    ident = s.sb("ident", [128, 128], BF16)
    s.dma("sp", ident_f[:], ident_in[:, :], reads=[ident_in], writes=[ident_f])
    s.op("dve", lambda e: e.tensor_copy(out=ident[:], in_=ident_f[:]), reads=[ident_f], writes=[ident])

    with ExitStack() as pa:
        s.stack = pa
        cct = s.sb("cct", [128, 2, 32], F32)
        sil = s.sb("sil", [128, 2, 32], F32)
        sbx = s.sb("sbx", [128, 2, 32, 128], F32)
        s.dma("sp", cct[:], cc[:, :, :], reads=[cc], writes=[cct])
        s.op("act", lambda e: e.activation(out=sil[:], in_=cct[:], func=AF.Silu), reads=[cct], writes=[sil])
        for w in range(2):
            s.op("dve", lambda e: e.tensor_copy(
                out=sbx[:, w], in_=sil[:, w, :].unsqueeze(2).to_broadcast([128, 32, 128])),
                reads=[sil], writes=[sbx])
        wts = [s.sb("wmt%d" % i, [128, 2048], F32) for i in range(3)]
        bmt = [s.sb("bmt%d" % i, [128, 2048], F32) for i in range(2)]
        mo = [s.sb("mo%d" % i, [128, 2048], F32) for i in range(2)]
        moc = [s.sb("moc%d" % i, [128, 2048], F32) for i in range(2)]
        it = 0
        for cg in range(12):
            c0 = cg * 2048
            bt = bmt[cg % 2]
            s.dma("act", bt[:], b_mod[0:1, c0:c0 + 2048].partition_broadcast(128), reads=[b_mod], writes=[bt])
            for kc in range(32):
                wt = wts[it % 3]
                it += 1
                q = "sp" if kc % 2 == 0 else "act"
                s.dma(q, wt[:], w_mod[kc * 128:(kc + 1) * 128, c0:c0 + 2048], reads=[w_mod], writes=[wt])
                for j in range(4):
                    s.op("pe", lambda e: e.matmul(pb[j][:, :], lhsT=sbx[:, 0, kc, :], rhs=wt[:, j * 512:(j + 1) * 512],
                                                  start=(kc == 0), stop=(kc == 31)), reads=[sbx, wt], writes=[pb[j]])
                if cg < 4:
                    for j in range(4):
                        s.op("pe", lambda e: e.matmul(pb[4 + j][:, :], lhsT=sbx[:, 1, kc, :], rhs=wt[:, j * 512:(j + 1) * 512],
                                                      start=(kc == 0), stop=(kc == 31)), reads=[sbx, wt], writes=[pb[4 + j]])
            m = mo[cg % 2]
            for j in range(4):
                s.op("dve", lambda e: e.tensor_tensor(out=m[:, j * 512:(j + 1) * 512], in0=pb[j][:, :],
                                                      in1=bt[:, j * 512:(j + 1) * 512], op=ALU.add),
                     reads=[pb[j], bt], writes=[m])
            s.dma("sp", modx[:, c0:c0 + 2048], m[:], reads=[m], writes=[modx])
            if cg < 4:
                mc = moc[cg % 2]
                for j in range(4):
                    s.op("dve", lambda e: e.tensor_tensor(out=mc[:, j * 512:(j + 1) * 512], in0=pb[4 + j][:, :],
                                                          in1=bt[:, j * 512:(j + 1) * 512], op=ALU.add),
                         reads=[pb[4 + j], bt], writes=[mc])
                s.dma("sp", modc[:, c0:c0 + 2048], mc[:], reads=[mc], writes=[modc])
        s.barrier()
    s.stack = st

    with ExitStack() as pbc:
        s.stack = pbc
        G1 = s.sb("G1", [128, D], F32)
        SH1 = s.sb("SH1", [128, D], F32)
        g1b = s.sb("g1b", [128, D], F32)
        nT = s.sb("nT", [128, 32, 1024], BF16)
        xt = [s.sb("xt%d" % i, [128, D], F32) for i in range(2)]
        xn = [s.sb("xn%d" % i, [128, D], BF16) for i in range(2)]
        sq = s.sb("sqjunk", [128, D], BF16)
        ss = [s.sb("ss%d" % i, [128, 1], F32) for i in range(2)]
        rstd = [s.sb("rstd%d" % i, [128, 1], F32) for i in range(2)]
        wt = [s.sb("wit%d" % i, [128, 32, 128], BF16) for i in range(3)]
        ot = [s.sb("ot%d" % i, [128, 512], F32) for i in range(4)]
        s.dma("sp", g1b[:], norm1_g[0:1, :].partition_broadcast(128), reads=[norm1_g], writes=[g1b])

        def load_mod(src, sc_off, sh_off):
            s.dma("sp", G1[:], src[:, sc_off:sc_off + D], reads=[src], writes=[G1])
            s.dma("act", SH1[:], src[:, sh_off:sh_off + D], reads=[src], writes=[SH1])
            s.op("dve", lambda e: e.scalar_tensor_tensor(out=G1[:], in0=G1[:], scalar=1.0, in1=g1b[:],
                                                         op0=ALU.add, op1=ALU.mult), reads=[G1, g1b], writes=[G1])

        blocks = [(0, 256)] + [(256 + i * 1024, 1024) for i in range(4)]
        if skip_mixer:
            blocks = []
        ti = 0
        wi = 0
        oi = 0
        ev = 0
        for bi, (t0, tn) in enumerate(blocks):
            if bi == 0:
                load_mod(modc, D, 0)
            elif bi == 1:
                load_mod(modx, D, 0)
            for tt in range(tn // 128):
                x_ = xt[ti % 2]
                xn_ = xn[ti % 2]
                ss_ = ss[ti % 2]
                rs_ = rstd[ti % 2]
                ti += 1
                r0 = t0 + tt * 128
                s.dma("sp", x_[:], xs[r0:r0 + 128, :], reads=[xs], writes=[x_])
                s.op("act", lambda e: e.activation(out=sq[:], in_=x_[:], func=AF.Square, accum_out=ss_[:]),
                     reads=[x_], writes=[sq, ss_])
                s.op("dve", lambda e: e.tensor_scalar(out=rs_[:], in0=ss_[:], scalar1=1.0 / D, scalar2=EPS,
                                                      op0=ALU.mult, op1=ALU.add), reads=[ss_], writes=[rs_])
                s.op("act", lambda e: e.activation(out=rs_[:], in_=rs_[:], func=AF.Sqrt), reads=[rs_], writes=[rs_])
                s.op("dve", lambda e: e.reciprocal(out=rs_[:], in_=rs_[:]), reads=[rs_], writes=[rs_])
                s.op("dve", lambda e: e.scalar_tensor_tensor(out=x_[:], in0=x_[:], scalar=rs_[:, 0:1], in1=G1[:],
                                                             op0=ALU.mult, op1=ALU.mult),
                     reads=[x_, rs_, G1], writes=[x_])
                s.op("pool", lambda e: e.tensor_tensor(out=xn_[:], in0=x_[:], in1=SH1[:], op=ALU.add),
                     reads=[x_, SH1], writes=[xn_])
                for q4 in range(4):
                    pt = pb[4 + (ev % 2)]
                    ev += 1
                    ptb = pt[:, :].bitcast(BF16)
                    for j in range(8):
                        kc = q4 * 8 + j
                        s.op("pe", lambda e: e.transpose(out=ptb[:, j * 128:(j + 1) * 128],
                                                         in_=xn_[:, kc * 128:(kc + 1) * 128], identity=ident[:]),
                             reads=[xn_, ident], writes=[pt])
                    eng = "act" if q4 % 2 == 0 else "dve"
                    dst = nT[:, q4 * 8:(q4 + 1) * 8, tt * 128:(tt + 1) * 128]
                    src = ptb.rearrange("p (j t) -> p j t", j=8)
                    if eng == "act":
                        s.op("act", lambda e: e.copy(out=dst, in_=src), reads=[pt], writes=[nT])
                    else:
                        s.op("dve", lambda e: e.tensor_copy(out=dst, in_=src), reads=[pt], writes=[nT])
            ng = (tn + 511) // 512
            gw = min(tn, 512)
            for j in range(NCH):
                w_ = wt[wi % 3]
                wi += 1
                s.dma("pool", w_[:], w_in_r[j].rearrange("p (k c) -> p k c", k=32), reads=[w_in_r], writes=[w_])
                for g in range(ng):
                    pp = pb[(j % 2) * 2 + g]
                    for kc in range(32):
                        s.op("pe", lambda e: e.matmul(pp[:, 0:gw], lhsT=w_[:, kc, :], rhs=nT[:, kc, g * 512:g * 512 + gw],
                                                      start=(kc == 0), stop=(kc == 31)), reads=[w_, nT], writes=[pp])
                    o_ = ot[oi % 4]
                    if oi % 2 == 0:
                        s.op("act", lambda e: e.copy(out=o_[:, 0:gw], in_=pp[:, 0:gw]), reads=[pp], writes=[o_])
                    else:
                        s.op("dve", lambda e: e.tensor_copy(out=o_[:, 0:gw], in_=pp[:, 0:gw]), reads=[pp], writes=[o_])
                    oi += 1
                    s.dma("sp", colsT[j * 128:(j + 1) * 128, t0 + g * 512:t0 + g * 512 + gw], o_[:, 0:gw],
                          reads=[o_], writes=[colsT])
        s.barrier()
    s.stack = st


    KAPPA = float(np.exp(-0.5))
    NB = [(i * 512, min(512, TT - i * 512)) for i in range((TT + 511) // 512)]

    def shift_mix(raw, out, u, np_):
        engs = ["dve", "pool"]
        s.op("dve", lambda e: e.tensor_scalar(out=out[0:np_, :], in0=raw[0:np_, :], scalar1=omm[0:np_, u:u + 1],
                                              scalar2=None, op0=ALU.mult), reads=[raw, omm], writes=[out])

        def acc(eng, o_ap, i_ap, sc):
            s.op(eng, lambda e: e.scalar_tensor_tensor(out=o_ap, in0=i_ap, scalar=sc, in1=o_ap,
                                                       op0=ALU.mult, op1=ALU.add), reads=[raw, musl, out], writes=[out])
        acc("dve", out[0:np_, 1:CTX], raw[0:np_, 0:CTX - 1], musl[0:np_, u, 4:5])
        acc("dve", out[0:np_, 0:CTX - 1], raw[0:np_, 1:CTX], musl[0:np_, u, 5:6])
        lo = out[0:np_, CTX:TT].rearrange("p (r c) -> p r c", c=64)
        lr = raw[0:np_, CTX:TT].rearrange("p (r c) -> p r c", c=64)
        acc("dve", lo[:, :, 1:64], lr[:, :, 0:63], musl[0:np_, u, 0:1])
        acc("dve", lo[:, :, 0:63], lr[:, :, 1:64], musl[0:np_, u, 1:2])
        acc("dve", out[0:np_, CTX + 64:TT], raw[0:np_, CTX:TT - 64], musl[0:np_, u, 2:3])
        acc("dve", out[0:np_, CTX:TT - 64], raw[0:np_, CTX + 64:TT], musl[0:np_, u, 3:4])

    mu_u = din("mu_u", [128, 54])
    slotm = din("slotm", [128, 6])
    w2r = din("w2r", [96, 2, RW])
    a2r = din("a2r", [96, 2, RW])
    g2r = din("g2r", [128, 2, RW])
    chp_in = din("chp", [128, 16, 9])
    msk_in = din("msk", [128, 4, 128])
    blk_in = din("blk", [128, 128])
    sgT = s.dram("sgT", [2, RW, TT], F32)
    asigT = s.dram("asigT", [2, RW, TT], BF16)
    gT = s.dram("gT", [RW, TT], BF16)
    kind = "ExternalOutput" if dbg in ("rwkv", "rwkv1", "ssd1", "ssd") else "Internal"
    if skip_mixer:
        kind = "ExternalInput"
    yT = s.dram("yT", [D, SEQ], BF16, kind=kind)

    omm = s.sb("omm", [128, 54], F32)
    musl = s.sb("musl", [128, 54, 6], F32)
    chp = s.sb("chp_sb", [128, 16, 9], F32)
    omka = s.sb("omka", [128, 16], F32)
    omka2 = s.sb("omka2", [128, 16], F32)
    bonD = s.dram("bonD", [RW, SEQ], BF16)
    msk = s.sb("msk_sb", [128, 4, 128], BF16)
    blk = s.sb("blk_sb", [128, 128], BF16)
    with ExitStack() as pc:
        s.stack = pc
        mut = s.sb("mut", [128, 54], F32)
        slt = s.sb("slt", [128, 6], F32)
        s.dma("sp", mut[:], mu_u[:, :], reads=[mu_u], writes=[mut])
        s.dma("sp", slt[:], slotm[:, :], reads=[slotm], writes=[slt])
        s.dma("sp", chp[:], chp_in[:, :, :], reads=[chp_in], writes=[chp])
        s.dma("pool", msk[:], msk_in[:, :, :], reads=[msk_in], writes=[msk])
        s.dma("pool", blk[:], blk_in[:, :], reads=[blk_in], writes=[blk])
        s.op("dve", lambda e: e.tensor_scalar(out=omm[:], in0=mut[:], scalar1=-1.0, scalar2=1.0,
                                              op0=ALU.mult, op1=ALU.add), reads=[mut], writes=[omm])
        for k in range(6):
            s.op("dve", lambda e: e.tensor_scalar(out=musl[:, :, k], in0=mut[:], scalar1=slt[:, k:k + 1], scalar2=None,
                                                  op0=ALU.mult), reads=[mut, slt], writes=[musl])
        s.op("dve", lambda e: e.tensor_scalar(out=omka[:], in0=chp[:, :, 5], scalar1=-1.0, scalar2=1.0,
                                              op0=ALU.mult, op1=ALU.add), reads=[chp], writes=[omka])
        s.op("dve", lambda e: e.tensor_scalar(out=omka2[:], in0=chp[:, :, 5], scalar1=-2.0, scalar2=2.0,
                                              op0=ALU.mult, op1=ALU.add), reads=[chp], writes=[omka2])
        s.barrier()
    s.stack = st

    with ExitStack() as pd1:
        s.stack = pd1
        tmpA = s.sb("d1A", [128, TT], F32)
        tmpB = s.sb("d1B", [128, TT], F32)
        lr_w = [s.sb("lr_w%d" % d, [96, TT], BF16) for d in range(2)]
        lr_a = [s.sb("lr_a%d" % d, [96, TT], BF16) for d in range(2)]
        lr_g = s.sb("lr_g", [128, 2, TT], BF16)
        w2b = s.sb("w2b", [96, 2, RW], BF16)
        a2b = s.sb("a2b", [96, 2, RW], BF16)
        g2b = s.sb("g2b", [128, 2, RW], BF16)
        s.dma("pool", w2b[:], w2r[:, :, :], reads=[w2r], writes=[w2b])
        s.dma("pool", a2b[:], a2r[:, :, :], reads=[a2r], writes=[a2b])
        s.dma("pool", g2b[:], g2r[:, :, :], reads=[g2r], writes=[g2b])
        units = [(6144 + 96 * d, 96, 48 + d, lr_w[d][:, :], AF.Tanh) for d in range(2)]
        units += [(6336 + 96 * d, 96, 50 + d, lr_a[d][:, :], AF.Copy) for d in range(2)]
        units += [(6528 + 128 * i, 128, 52 + i, lr_g[:, i, :], AF.Sigmoid) for i in range(2)]
        lr_t = {id(lr_w[0][:, :]): lr_w[0]}
        lr_tiles = [lr_w[0], lr_w[1], lr_a[0], lr_a[1], lr_g, lr_g]
        if skip_mixer:
            units = []
        for ui, (row0, np_, u, dst, fn) in enumerate(units):
            s.dma("sp", tmpA[0:np_, :], colsT[row0:row0 + np_, :], reads=[colsT], writes=[tmpA])
            shift_mix(tmpA, tmpB, u, np_)
            s.op("act", lambda e: e.activation(out=dst[0:np_], in_=tmpB[0:np_, :], func=fn),
                 reads=[tmpB], writes=[lr_tiles[ui]])
        sgs = [s.sb("sgs%d" % i, [128, TT], F32) for i in range(2)]
        asg = [s.sb("asg%d" % i, [128, TT], BF16) for i in range(2)]
        gs = s.sb("gs", [128, TT], BF16)
        bk = 0
        for cj in range(0 if skip_mixer else 16):
            csl = slice(cj * 128, (cj + 1) * 128)
            for d in range(2):
                for (b0, bn) in NB:
                    pp = pb[bk % 4]; bk += 1
                    s.op("pe", lambda e: e.matmul(pp[:, 0:bn], lhsT=w2b[:, d, csl], rhs=lr_w[d][:, b0:b0 + bn],
                                                  start=True, stop=True), reads=[w2b, lr_w[d]], writes=[pp])
                    s.op("act", lambda e: e.activation(out=sgs[d][:, b0:b0 + bn], in_=pp[:, 0:bn], func=AF.Sigmoid,
                                                       bias=chp[:, cj, d:d + 1]), reads=[pp, chp], writes=[sgs[d]])
                s.dma("sp", sgT[d, csl, :], sgs[d][:], reads=[sgs[d]], writes=[sgT])
                for (b0, bn) in NB:
                    pp = pb[bk % 4]; bk += 1
                    s.op("pe", lambda e: e.matmul(pp[:, 0:bn], lhsT=a2b[:, d, csl], rhs=lr_a[d][:, b0:b0 + bn],
                                                  start=True, stop=True), reads=[a2b, lr_a[d]], writes=[pp])
                    s.op("act", lambda e: e.activation(out=asg[d][:, b0:b0 + bn], in_=pp[:, 0:bn], func=AF.Sigmoid,
                                                       bias=chp[:, cj, 2 + d:3 + d]), reads=[pp, chp], writes=[asg[d]])
                s.dma("sp", asigT[d, csl, :], asg[d][:], reads=[asg[d]], writes=[asigT])
            for (b0, bn) in NB:
                pp = pb[bk % 4]; bk += 1
                for i in range(2):
                    s.op("pe", lambda e: e.matmul(pp[:, 0:bn], lhsT=g2b[:, i, csl], rhs=lr_g[:, i, b0:b0 + bn],
                                                  start=(i == 0), stop=(i == 1)), reads=[g2b, lr_g], writes=[pp])
                s.op("dve", lambda e: e.tensor_copy(out=gs[:, b0:b0 + bn], in_=pp[:, 0:bn]), reads=[pp], writes=[gs])
            s.dma("sp", gT[csl, :], gs[:], reads=[gs], writes=[gT])
        s.barrier()
    s.stack = st

    NCK = TT // 128
    with ExitStack() as pe_:
        s.stack = pe_
        tA = s.sb("eA", [128, TT], F32)
        tB = s.sb("eB", [128, TT], F32)
        r_m = s.sb("r_m", [128, TT], BF16)
        v_m = s.sb("v_m", [128, TT], BF16)
        kkn = s.sb("kkn", [128, TT], BF16)
        k_m = s.sb("k_m", [128, TT], BF16)
        kdx = s.sb("kdx", [128, TT], BF16)
        bdx = s.sb("bdx", [128, TT], BF16)
        kdir = [kdx, kdx]
        bdir = [bdx, bdx]
        Ebuf = s.sb("Ebuf", [128, TT], BF16)
        AR = s.sb("AR", [128, 2, TT], BF16)
        BK = s.sb("BK", [128, 2, TT], BF16)
        BKhT = s.sb("BKhT", [128, 2, NCK, 128], BF16)
        BKh_ap = tB[:, :].bitcast(BF16).rearrange("p (w t) -> p w t", w=2)
        yst_ap = AR[:, 0, 0:SEQ]
        gld_ap = BK[:, 0, 0:SEQ]
        bon_ap = BK[:, 1, 0:SEQ]
        VtA = s.sb("VtA", [128, NCK, 128], BF16)
        Oacc = s.sb("Oacc", [128, 32, 128], F32)
        segm = s.sb("segm", [128, TT], BF16)
        tot = s.sb("tot", [128, NCK], F32)
        PL = s.sb("PL", [128, NCK], F32)
        STf = s.sb("STf", [128, 64], F32)
        STb = s.sb("STb", [128, 64], BF16)
        s.op("pool", lambda e: e.memset(segm[:], 1.0), writes=[segm])
        s.op("pool", lambda e: e.memset(segm[:, :].rearrange("p (c t) -> p c t", t=128)[:, :, 0:1], 0.0), writes=[segm])
        NW = 6
        w_Mb = [s.sb("w_Mb%d" % i, [128, 256], BF16) for i in range(4)]
        w_Mk = [s.sb("w_Mk%d" % i, [128, 256], BF16) for i in range(4)]
        w_Ah = [[s.sb("w_A%d_%d" % (h_, i), [128, 128], BF16) for i in range(4)] for h_ in range(2)]
        w_Bh = [[s.sb("w_B%d_%d" % (h_, i), [128, 128], BF16) for i in range(4)] for h_ in range(2)]
        w_Xh = [[s.sb("w_X%d_%d" % (h_, i), [128, 128], BF16) for i in range(4)] for h_ in range(2)]
        w_XF = [s.sb("w_XF%d" % i, [128, 128], BF16) for i in range(4)]
        w_R = [s.sb("w_R%d" % i, [128, 128], BF16) for i in range(2)]
        w_U = [s.sb("w_U%d" % i, [128, 128], BF16) for i in range(2)]
        osb = [s.sb("osb%d" % i, [128, 128], F32) for i in range(2)]
        onb = [s.sb("onb%d" % i, [128, 128], BF16) for i in range(2)]
        stat = [s.sb("stat%d" % i, [128, 8], F32) for i in range(2)]
        cnt = {"bank": 0, "A0": 0, "B0": 0, "X0": 0, "A1": 0, "B1": 0, "X1": 0, "M": 0, "R": 0, "ev": 0}

        def nbank():
            cnt["bank"] += 1
            return pb[cnt["bank"] % 7]

        def rot(lst, key):
            cnt[key] += 1
            return lst[cnt[key] % len(lst)]

        def evac_copy(dst_t, dst_ap, src_t, src_ap):
            cnt["ev"] += 1
            if cnt["ev"] % 2 == 0:
                s.op("act", lambda e: e.copy(out=dst_ap, in_=src_ap), reads=[src_t], writes=[dst_t])
            else:
                s.op("dve", lambda e: e.tensor_copy(out=dst_ap, in_=src_ap), reads=[src_t], writes=[dst_t])

        ptr = pb[7]
        ptrb = ptr[:, :].bitcast(BF16)

        for hp in range(16):
            if dbg in ('ssd1', 'ssd') or skip_mixer:
                break
            csl = slice(hp * 128, (hp + 1) * 128)
            for which, dst in ((0, r_m), (2, v_m), (1, k_m)):
                row0 = which * RW + hp * 128
                s.dma("sp", tA[:], colsT[row0:row0 + 128, :], reads=[colsT], writes=[tA])
                shift_mix(tA, tB, which * 16 + hp, 128)
                if dst is not None:
                    s.op("act", lambda e: e.copy(out=dst[:], in_=tB[:]), reads=[tB], writes=[dst])
            s.op("dve", lambda e: e.tensor_scalar(out=tA[:], in0=tB[:], scalar1=chp[:, hp, 4:5], scalar2=None,
                                                  op0=ALU.mult), reads=[tB, chp], writes=[tA])
            s.op("act", lambda e: e.activation(out=Ebuf[:], in_=tA[:], func=AF.Square), reads=[tA], writes=[Ebuf])
            for (b0, bn) in NB:
                pp = nbank()
                s.op("pe", lambda e: e.matmul(pp[:, 0:bn], lhsT=blk[:], rhs=Ebuf[:, b0:b0 + bn], start=True, stop=True),
                     reads=[blk, Ebuf], writes=[pp])
                s.op("act", lambda e: e.activation(out=tB[:, b0:b0 + bn], in_=pp[:, 0:bn], func=AF.Sqrt),
                     reads=[pp], writes=[tB])
            s.op("dve", lambda e: e.tensor_scalar(out=tB[:], in0=tB[:], scalar1=1e-12, scalar2=None,
                                                  op0=ALU.max), reads=[tB], writes=[tB])
            s.op("dve", lambda e: e.reciprocal(out=tB[:], in_=tB[:]), reads=[tB], writes=[tB])
            s.op("dve", lambda e: e.tensor_tensor(out=kkn[:], in0=tA[:], in1=tB[:], op=ALU.mult),
                 reads=[tA, tB], writes=[kkn])
            s.dma("sp", bdx[:], asigT[0, csl, :], reads=[asigT], writes=[bdx])
            s.dma("sp", kdx[:], asigT[1, csl, :], reads=[asigT], writes=[kdx])
            s.op("pool", lambda e: e.tensor_tensor(out=Ebuf[:], in0=bdx[:], in1=kdx[:], op=ALU.add),
                 reads=[bdx, kdx], writes=[Ebuf])
            s.op("dve", lambda e: e.tensor_scalar(out=Ebuf[:], in0=Ebuf[:], scalar1=chp[:, hp, 5:6], scalar2=omka2[:, hp:hp + 1],
                                                  op0=ALU.mult, op1=ALU.add), reads=[Ebuf, chp, omka2], writes=[Ebuf])
            s.op("dve", lambda e: e.tensor_tensor(out=Ebuf[:], in0=Ebuf[:], in1=k_m[:], op=ALU.mult),
                 reads=[Ebuf, k_m], writes=[Ebuf])
            s.op("dve", lambda e: e.scalar_tensor_tensor(out=Ebuf[:], in0=r_m[:], scalar=chp[:, hp, 6:7], in1=Ebuf[:],
                                                         op0=ALU.mult, op1=ALU.mult), reads=[r_m, chp, Ebuf], writes=[Ebuf])
            for (b0, bn) in [(CTX + i * 512, 512) for i in range(8)]:
                pp = nbank()
                s.op("pe", lambda e: e.matmul(pp[:, 0:bn], lhsT=blk[:], rhs=Ebuf[:, b0:b0 + bn], start=True, stop=True),
                     reads=[blk, Ebuf], writes=[pp])
                s.op("dve", lambda e: e.tensor_tensor(out=yst_ap[:, b0 - CTX:b0 - CTX + bn], in0=pp[:, 0:bn],
                                                      in1=v_m[:, b0:b0 + bn], op=ALU.mult), reads=[pp, v_m], writes=[AR])
            s.dma("sp", bonD[csl, :], yst_ap, reads=[AR], writes=[bonD])
            for q in range((NCK + 7) // 8):
                n8 = min(8, NCK - q * 8)
                for j in range(n8):
                    ci = q * 8 + j
                    s.op("pe", lambda e: e.transpose(out=ptrb[:, j * 128:(j + 1) * 128], in_=v_m[:, ci * 128:(ci + 1) * 128],
                                                     identity=ident[:]), reads=[v_m, ident], writes=[ptr])
                evac_copy(VtA, VtA[:, q * 8:q * 8 + n8, :], ptr, ptrb[:, 0:n8 * 128].rearrange("p (j t) -> p j t", j=n8))

            for d in range(2):
                s.dma("sp", bdx[:], asigT[d, csl, :], reads=[asigT], writes=[bdx])
                s.op("dve", lambda e: e.tensor_scalar(out=kdx[:], in0=bdx[:], scalar1=chp[:, hp, 5:6],
                                                      scalar2=omka[:, hp:hp + 1], op0=ALU.mult, op1=ALU.add),
                     reads=[bdx, chp, omka], writes=[kdx])
                s.op("dve", lambda e: e.tensor_tensor(out=kdx[:], in0=kdx[:], in1=k_m[:], op=ALU.mult),
                     reads=[kdx, k_m], writes=[kdx])
                s.op("pool", lambda e: e.tensor_tensor(out=bdx[:], in0=bdx[:], in1=kkn[:], op=ALU.mult),
                     reads=[bdx, kkn], writes=[bdx])
                s.dma("sp", tA[:], sgT[d, csl, :], reads=[sgT], writes=[tA])
                s.op("dve", lambda e: e.tensor_tensor_scan(out=tB[:], data0=segm[:], data1=tA[:], initial=0.0,
                                                           op0=ALU.mult, op1=ALU.add), reads=[segm, tA], writes=[tB])
                tB3 = tB[:, :].rearrange("p (c t) -> p c t", t=128)
                tA3 = tA[:, :].rearrange("p (c t) -> p c t", t=128)
                s.op("dve", lambda e: e.tensor_copy(out=tot[:], in_=tB3[:, :, 127]), reads=[tB], writes=[tot])
                if d == 1:
                    s.op("dve", lambda e: e.tensor_tensor(out=tB[:], in0=tA[:], in1=tB[:], op=ALU.subtract),
                         reads=[tA, tB], writes=[tB])
                    s.op("dve", lambda e: e.tensor_tensor(out=tB3, in0=tB3, in1=tot[:, :].unsqueeze(2).to_broadcast([128, NCK, 128]),
                                                          op=ALU.add), reads=[tB, tot], writes=[tB])
                s.op("act", lambda e: e.activation(out=PL[:], in_=tot[:], func=AF.Exp, scale=-KAPPA), reads=[tot], writes=[PL])
                s.op("act", lambda e: e.activation(out=Ebuf[:], in_=tB[:], func=AF.Exp, scale=-KAPPA), reads=[tB], writes=[Ebuf])
                s.op("dve", lambda e: e.tensor_tensor(out=AR[:, 1, :], in0=r_m[:], in1=Ebuf[:], op=ALU.mult),
                     reads=[r_m, Ebuf], writes=[AR])
                s.op("act", lambda e: e.activation(out=Ebuf[:], in_=tB[:], func=AF.Exp, scale=KAPPA), reads=[tB], writes=[Ebuf])
                s.op("dve", lambda e: e.tensor_tensor(out=BK[:, 0, :], in0=bdir[d][:], in1=Ebuf[:], op=ALU.mult),
                     reads=[bdir[d], Ebuf], writes=[BK])
                s.op("pool", lambda e: e.tensor_tensor(out=BK[:, 1, :], in0=kdir[d][:], in1=Ebuf[:], op=ALU.mult),
                     reads=[kdir[d], Ebuf], writes=[BK])
                s.op("dve", lambda e: e.tensor_tensor(out=tA[:], in0=tB[:], in1=tA[:], op=ALU.subtract),
                     reads=[tA, tB], writes=[tA])
                s.op("act", lambda e: e.activation(out=Ebuf[:], in_=tA[:], func=AF.Exp, scale=-KAPPA), reads=[tA], writes=[Ebuf])
                s.op("dve", lambda e: e.scalar_tensor_tensor(out=AR[:, 0, :], in0=kkn[:], scalar=-1.0, in1=Ebuf[:],
                                                             op0=ALU.mult, op1=ALU.mult), reads=[kkn, Ebuf], writes=[AR])
                s.op("dve", lambda e: e.tensor_tensor(out=tA3, in0=tot[:, :].unsqueeze(2).to_broadcast([128, NCK, 128]),
                                                      in1=tB3, op=ALU.subtract), reads=[tB, tot], writes=[tA])
                s.op("act", lambda e: e.activation(out=Ebuf[:], in_=tA[:], func=AF.Exp, scale=-KAPPA), reads=[tA], writes=[Ebuf])
                s.op("dve", lambda e: e.tensor_tensor(out=BKh_ap[:, 0, :], in0=bdir[d][:], in1=Ebuf[:], op=ALU.mult),
                     reads=[bdir[d], Ebuf], writes=[tB])
                s.op("pool", lambda e: e.tensor_tensor(out=BKh_ap[:, 1, :], in0=kdir[d][:], in1=Ebuf[:], op=ALU.mult),
                     reads=[kdir[d], Ebuf], writes=[tB])
                for w in range(2):
                    for q in range((NCK + 7) // 8):
                        n8 = min(8, NCK - q * 8)
                        for j in range(n8):
                            ci = q * 8 + j
                            s.op("pe", lambda e: e.transpose(out=ptrb[:, j * 128:(j + 1) * 128],
                                                             in_=BKh_ap[:, w, ci * 128:(ci + 1) * 128], identity=ident[:]),
                                 reads=[tB, ident], writes=[ptr])
                        evac_copy(BKhT, BKhT[:, w, q * 8:q * 8 + n8, :], ptr,
                                  ptrb[:, 0:n8 * 128].rearrange("p (j t) -> p j t", j=n8))
                s.op("pool", lambda e: e.memset(STf[:], 0.0), writes=[STf])
                s.op("pool", lambda e: e.memset(STb[:], 0.0), writes=[STb])
                order = list(range(NCK)) if d == 0 else [1, 0] + list(range(NCK - 1, 1, -1))
                mbk = msk[:, 0:2, :] if d == 0 else msk[:, 2:4, :]
                mn = msk[:, 2, :] if d == 0 else msk[:, 0, :]
                def inv_part(ci, par):
                    tsl = slice(ci * 128, (ci + 1) * 128)
                    pAs, pBs, pCs = [], [], []
                    for hh in range(2):
                        hs = slice(hh * 64, (hh + 1) * 64)
                        pA, pB, pC = nbank(), nbank(), nbank()
                        s.op("pe", lambda e: e.matmul(pA[:, 0:256].rearrange("p (a t) -> p a t", a=2), lhsT=BK[hs, 0, tsl], rhs=AR[hs, :, tsl], start=True, stop=True),
                             reads=[BK, AR], writes=[pA])
                        s.op("pe", lambda e: e.matmul(pB[:, 0:256].rearrange("p (a t) -> p a t", a=2), lhsT=BK[hs, 1, tsl], rhs=AR[hs, :, tsl], start=True, stop=True),
                             reads=[BK, AR], writes=[pB])
                        s.op("pe", lambda e: e.matmul(pC[:, 0:128], lhsT=AR[hs, 0, tsl], rhs=BK[hs, 0, tsl], start=True, stop=True),
                             reads=[BK, AR], writes=[pC])
                        pAs.append(pA); pBs.append(pB); pCs.append(pC)
                    Ah, Bh, Xc, Mbh, Mkh = [], [], [], [], []
                    for hh in range(2):
                        Mb = w_Mb[par * 2 + hh]
                        Mk = w_Mk[par * 2 + hh]
                        s.op("dve", lambda e: e.tensor_tensor(out=Mb[:, :].rearrange("p (a t) -> p a t", a=2),
                                                              in0=pAs[hh][:, 0:256].rearrange("p (a t) -> p a t", a=2),
                                                              in1=mbk, op=ALU.mult), reads=[pAs[hh], msk], writes=[Mb])
                        A = rot(w_Ah[hh], "A%d" % hh)
                        s.op("dve", lambda e: e.tensor_tensor(out=A[:], in0=pCs[hh][:, 0:128], in1=mn, op=ALU.mult),
                             reads=[pCs[hh], msk], writes=[A])
                        s.op("dve", lambda e: e.tensor_tensor(out=Mk[:, :].rearrange("p (a t) -> p a t", a=2),
                                                              in0=pBs[hh][:, 0:256].rearrange("p (a t) -> p a t", a=2),
                                                              in1=mbk, op=ALU.mult), reads=[pBs[hh], msk], writes=[Mk])
                        B = rot(w_Bh[hh], "B%d" % hh)
                        s.op("pool", lambda e: e.tensor_copy(out=B[:], in_=Mb[:, 0:128]), reads=[Mb], writes=[B])
                        X = rot(w_Xh[hh], "X%d" % hh)
                        s.op("pool", lambda e: e.tensor_tensor(out=X[:], in0=Mb[:, 0:128], in1=ident[:], op=ALU.add),
                             reads=[Mb, ident], writes=[X])
                        Ah.append(A); Bh.append(B); Xc.append(X); Mbh.append(Mb); Mkh.append(Mk)
                    for k in range(7):
                        pXs, pA2s, pB2s = [None, None], [None, None], [None, None]
                        for hh in range(2):
                            A, B, X = Ah[hh], Bh[hh], Xc[hh]
                            if k >= 1:
                                pX = nbank()
                                s.op("pe", lambda e: e.matmul(pX[:, 0:128], lhsT=A[:], rhs=X[:], start=True, stop=True),
                                     reads=[A, X], writes=[pX])
                                pXs[hh] = pX
                            if k < 6:
                                pA2, pB2 = nbank(), nbank()
                                s.op("pe", lambda e: e.matmul(pA2[:, 0:128], lhsT=B[:], rhs=A[:], start=True, stop=True),
                                     reads=[A, B], writes=[pA2])
                                s.op("pe", lambda e: e.matmul(pB2[:, 0:128], lhsT=A[:], rhs=B[:], start=True, stop=True),
                                     reads=[A, B], writes=[pB2])
                                pA2s[hh], pB2s[hh] = pA2, pB2
                        for hh in range(2):
                            if k < 6:
                                An = rot(w_Ah[hh], "A%d" % hh)
                                Bn = rot(w_Bh[hh], "B%d" % hh)
                                s.op("act", lambda e: e.copy(out=An[:], in_=pA2s[hh][:, 0:128]), reads=[pA2s[hh]], writes=[An])
                                s.op("act", lambda e: e.copy(out=Bn[:], in_=pB2s[hh][:, 0:128]), reads=[pB2s[hh]], writes=[Bn])
                            if k >= 1:
                                X = Xc[hh]
                                Xn = rot(w_Xh[hh], "X%d" % hh) if k < 6 else w_XF[par * 2 + hh]
                                s.op("dve", lambda e: e.tensor_tensor(out=Xn[:], in0=pXs[hh][:, 0:128], in1=X[:], op=ALU.add),
                                     reads=[pXs[hh], X], writes=[Xn])
                                Xc[hh] = Xn
                            if k < 6:
                                Ah[hh], Bh[hh] = An, Bn
                    return (Xc, Mbh, Mkh)

                def state_part(ci, par, inv):
                    Xh, Mbh, Mkh = inv
                    tsl = slice(ci * 128, (ci + 1) * 128)
                    pU = nbank()
                    for hh in range(2):
                        hs = slice(hh * 64, (hh + 1) * 64)
                        s.op("pe", lambda e: e.matmul(pU[:, hs], lhsT=AR[hs, 0, tsl], rhs=STb[hs, :], start=True, stop=False),
                             reads=[AR, STb], writes=[pU])
                        s.op("pe", lambda e: e.matmul(pU[:, hs], lhsT=Mkh[hh][:, 0:128], rhs=VtA[:, ci, hs], start=False, stop=True),
                             reads=[Mkh[hh], VtA], writes=[pU])
                    Rr = w_R[par]
                    evac_copy(Rr, Rr[:], pU, pU[:, 0:128])
                    pU2 = nbank()
                    for hh in range(2):
                        hs = slice(hh * 64, (hh + 1) * 64)
                        s.op("pe", lambda e: e.matmul(pU2[:, hs], lhsT=Xh[hh][:], rhs=Rr[:, hs], start=True, stop=True),
                             reads=[Xh[hh], Rr], writes=[pU2])
                    Ub = w_U[par]
                    evac_copy(Ub, Ub[:], pU2, pU2[:, 0:128])
                    pS = nbank()
                    for hh in range(2):
                        hs = slice(hh * 64, (hh + 1) * 64)
                        s.op("pe", lambda e: e.matmul(pS[hs, 0:64], lhsT=BKhT[:, 0, ci, hs], rhs=Ub[:, hs], start=True, stop=False),
                             reads=[BKhT, Ub], writes=[pS])
                        s.op("pe", lambda e: e.matmul(pS[hs, 0:64], lhsT=BKhT[:, 1, ci, hs], rhs=VtA[:, ci, hs], start=False, stop=True),
                             reads=[BKhT, VtA], writes=[pS])
                    pO = None
                    if ci >= 2:
                        pO = nbank()
                        for hh in range(2):
                            hs = slice(hh * 64, (hh + 1) * 64)
                            s.op("pe", lambda e: e.matmul(pO[:, hs], lhsT=AR[hs, 1, tsl], rhs=STb[hs, :], start=True, stop=False),
                                 reads=[AR, STb], writes=[pO])
                            s.op("pe", lambda e: e.matmul(pO[:, hs], lhsT=Mkh[hh][:, 128:256], rhs=VtA[:, ci, hs], start=False, stop=False),
                                 reads=[Mkh[hh], VtA], writes=[pO])
                            s.op("pe", lambda e: e.matmul(pO[:, hs], lhsT=Mbh[hh][:, 128:256], rhs=Ub[:, hs], start=False, stop=True),
                                 reads=[Mbh[hh], Ub], writes=[pO])
                    s.op("dve", lambda e: e.scalar_tensor_tensor(out=STf[:], in0=STf[:], scalar=PL[:, ci:ci + 1], in1=pS[:, 0:64],
                                                                 op0=ALU.mult, op1=ALU.add), reads=[STf, PL, pS], writes=[STf])
                    s.op("act", lambda e: e.copy(out=STb[:], in_=STf[:]), reads=[STf], writes=[STb])
                    if ci >= 2:
                        if d == 0:
                            evac_copy(Oacc, Oacc[:, ci - 2, :], pO, pO[:, 0:128])
                        else:
                            s.op("dve", lambda e: e.tensor_tensor(out=Oacc[:, ci - 2, :], in0=pO[:, 0:128], in1=Oacc[:, ci - 2, :],
                                                                  op=ALU.add), reads=[pO, Oacc], writes=[Oacc])

                prev = inv_part(order[0], 0)
                for i, ci in enumerate(order):
                    nxt = inv_part(order[i + 1], (i + 1) % 2) if i + 1 < len(order) else None
                    state_part(ci, i % 2, prev)
                    prev = nxt
            s.dma("sp", gld_ap, gT[csl, CTX:TT], reads=[gT], writes=[BK])
            s.dma("act", bon_ap, bonD[csl, :], reads=[bonD], writes=[BK])
            for lc in range(32):
                ob = osb[lc % 2]
                on = onb[lc % 2]
                stt = stat[lc % 2]
                for hh in range(2):
                    hs = slice(hh * 64, (hh + 1) * 64)
                    s.op("dve", lambda e: e.tensor_reduce(out=stt[:, hh:hh + 1], in_=Oacc[:, lc, hs], axis=AX.X, op=ALU.add),
                         reads=[Oacc], writes=[stt])
                    s.op("dve", lambda e: e.tensor_scalar(out=stt[:, 2 + hh:3 + hh], in0=stt[:, hh:hh + 1], scalar1=-1.0 / 64,
                                                          scalar2=None, op0=ALU.mult), reads=[stt], writes=[stt])
                    s.op("dve", lambda e: e.tensor_scalar(out=ob[:, hs], in0=Oacc[:, lc, hs], scalar1=stt[:, 2 + hh:3 + hh],
                                                          scalar2=None, op0=ALU.add), reads=[Oacc, stt], writes=[ob])
                    s.op("act", lambda e: e.activation(out=on[:, hs], in_=ob[:, hs], func=AF.Square,
                                                       accum_out=stt[:, 4 + hh:5 + hh]), reads=[ob], writes=[on, stt])
                    s.op("dve", lambda e: e.tensor_scalar(out=stt[:, 6 + hh:7 + hh], in0=stt[:, 4 + hh:5 + hh], scalar1=1.0 / 64,
                                                          scalar2=64e-5, op0=ALU.mult, op1=ALU.add), reads=[stt], writes=[stt])
                    s.op("act", lambda e: e.activation(out=stt[:, 6 + hh:7 + hh], in_=stt[:, 6 + hh:7 + hh], func=AF.Sqrt),
                         reads=[stt], writes=[stt])
                    s.op("dve", lambda e: e.reciprocal(out=stt[:, 6 + hh:7 + hh], in_=stt[:, 6 + hh:7 + hh]), reads=[stt], writes=[stt])
                    s.op("dve", lambda e: e.tensor_scalar(out=on[:, hs], in0=ob[:, hs], scalar1=stt[:, 6 + hh:7 + hh],
                                                          scalar2=None, op0=ALU.mult), reads=[ob, stt], writes=[on])
                s.op("pe", lambda e: e.transpose(out=ptrb[:, 0:128], in_=on[:], identity=ident[:]), reads=[on, ident], writes=[ptr])
                tl = slice(lc * 128, (lc + 1) * 128)
                s.op("act", lambda e: e.activation(out=yst_ap[:, tl], in_=ptrb[:, 0:128], func=AF.Identity,
                                                   scale=chp[:, hp, 7:8], bias=chp[:, hp, 8:9]), reads=[ptr, chp], writes=[AR])
            s.op("dve", lambda e: e.tensor_tensor(out=yst_ap, in0=yst_ap, in1=bon_ap, op=ALU.add), reads=[AR, BK], writes=[AR])
            s.op("dve", lambda e: e.tensor_tensor(out=yst_ap, in0=yst_ap, in1=gld_ap, op=ALU.mult), reads=[AR, BK], writes=[AR])
            s.dma("sp", yT[csl, :], yst_ap, reads=[AR], writes=[yT])
            if dbg == "rwkv1":
                break
        s.barrier()
    s.stack = st


    cw_in = din("cw", [128, 32, 4])
    dtp_in = din("dtp", [64, 2])
    dbc_in = din("dbc", [1, RW])
    nwb_in = din("nwb", [1, RW])
    ZR0, XR0, BR0, CR0, DTR0 = RCOLS, RCOLS + 2048, RCOLS + 4096, RCOLS + 5120, RCOLS + 6144
    with ExitStack() as pg:
        s.stack = pg
        cw = s.sb("cw_sb", [128, 32, 4], F32)
        dtp = s.sb("dtp_sb", [64, 2], F32)
        Aneg = s.sb("Aneg", [64, 1], F32)
        dbc = s.sb("dbc_sb", [128, 256], F32)
        nwb = s.sb("nwb_sb", [128, 256], F32)
        gA = s.sb("gA", [128, TT], F32)
        gB = s.sb("gB", [128, TT], F32)
        cumF = s.sb("cumF", [64, TT], F32)
        totF = s.sb("totF", [64, NCK], F32)
        dtTok = s.sb("dtTok", [128, NCK, 64], F32)
        cumTok = s.sb("cumTok", [128, NCK, 64], F32)
        ehTok = s.sb("ehTok", [128, NCK, 64], F32)
        xg = s.sb("xg", [128, 2, TT], BF16)
        Bg = s.sb("Bg", [128, TT], BF16)
        Cg = s.sb("Cg", [128, TT], BF16)
        XTok = s.sb("XTok", [128, NCK, 256], BF16)
        BTok = s.sb("BTok", [128, NCK, 128], BF16)
        dtF = T("dtF_alias_xg", xg[:, :, :].rearrange("p a t -> p (a t)").bitcast(F32)[0:64, :])
        ehF = T("ehF_alias_XTok", XTok[:, :, :].rearrange("p a t -> p (a t)").bitcast(F32)[0:64, :])
        yacc = s.sb("yacc", [128, 32, 256], F32)
        SSf = s.sb("SSf", [128, 256], F32)
        SSb = s.sb("SSb", [128, 256], BF16)
        cbm = [s.sb("cbm%d" % i, [128, 128], BF16) for i in range(2)]
        NWS = 8
        w_d = [s.sb("wd%d" % i, [128, 128], F32) for i in range(NWS)]
        w_e = [s.sb("we%d" % i, [128, 128], BF16) for i in range(NWS)]
        w_eb = [s.sb("web%d" % i, [128, 128], F32) for i in range(NWS)]
        w_dcb = [s.sb("wdcb%d" % i, [128, 128], BF16) for i in range(NWS)]
        w_ct = [s.sb("wct%d" % i, [128, 128], BF16) for i in range(NWS)]
        w_bh = [s.sb("wbh%d" % i, [128, 128], BF16) for i in range(NWS)]
        fy = [s.sb("fy%d" % i, [128, 256], F32) for i in range(2)]
        fz = [s.sb("fz%d" % i, [128, 256], F32) for i in range(2)]
        fo = [s.sb("fo%d" % i, [128, 256], BF16) for i in range(2)]
        fsq = s.sb("fsq", [128, 256], BF16)
        fst = [s.sb("fst%d" % i, [128, 2], F32) for i in range(2)]
        gc = {"bank": 0, "w": 0, "ev": 0}

        def gbank():
            gc["bank"] += 1
            return pb[gc["bank"] % 7]

        def gev(dst_t, dst_ap, src_t, src_ap):
            gc["ev"] += 1
            if gc["ev"] % 2 == 0:
                s.op("act", lambda e: e.copy(out=dst_ap, in_=src_ap), reads=[src_t], writes=[dst_t])
            else:
                s.op("dve", lambda e: e.tensor_copy(out=dst_ap, in_=src_ap), reads=[src_t], writes=[dst_t])

        ptr = pb[7]
        ptrb = ptr[:, :].bitcast(BF16)
        s.dma("sp", cw[:], cw_in[:, :, :], reads=[cw_in], writes=[cw])
        s.dma("sp", dtp[:], dtp_in[:, :], reads=[dtp_in], writes=[dtp])
        s.op("act", lambda e: e.activation(out=Aneg[:], in_=dtp[:, 1:2], func=AF.Exp), reads=[dtp], writes=[Aneg])
        s.op("dve", lambda e: e.tensor_scalar(out=Aneg[:], in0=Aneg[:], scalar1=-1.0, scalar2=None, op0=ALU.mult),
             reads=[Aneg], writes=[Aneg])
        s.dma("sp", gA[0:64, :], colsT[DTR0:DTR0 + 64, :], reads=[colsT], writes=[gA])
        s.op("act", lambda e: e.activation(out=gB[0:64, :], in_=gA[0:64, :], func=AF.Exp, bias=dtp[:, 0:1]),
             reads=[gA, dtp], writes=[gB])
        s.op("act", lambda e: e.activation(out=dtF[:], in_=gB[0:64, :], func=AF.Ln, bias=1.0), reads=[gB], writes=[dtF])
        s.op("dve", lambda e: e.tensor_scalar(out=gA[0:64, :], in0=dtF[:], scalar1=Aneg[:, 0:1], scalar2=None, op0=ALU.mult),
             reads=[dtF, Aneg], writes=[gA])
        s.op("pool", lambda e: e.memset(gB[0:64, :], 1.0), reads=[], writes=[gB])
        s.op("pool", lambda e: e.memset(gB[0:64, :].rearrange("p (c t) -> p c t", t=128)[:, :, 0:1], 0.0), writes=[gB])
        s.op("dve", lambda e: e.tensor_tensor_scan(out=cumF[:], data0=gB[0:64, :], data1=gA[0:64, :], initial=0.0,
                                                   op0=ALU.mult, op1=ALU.add), reads=[gB, gA], writes=[cumF])
        cum3 = cumF[:, :].rearrange("p (c t) -> p c t", t=128)
        s.op("dve", lambda e: e.tensor_copy(out=totF[:], in_=cum3[:, :, 127]), reads=[cumF], writes=[totF])
        s.op("dve", lambda e: e.tensor_tensor(out=cumF[32:64, :], in0=gA[32:64, :], in1=cumF[32:64, :], op=ALU.subtract),
             reads=[gA, cumF], writes=[cumF])
        s.op("dve", lambda e: e.tensor_tensor(out=cum3[32:64], in0=cum3[32:64],
                                              in1=totF[32:64, :].unsqueeze(2).to_broadcast([32, NCK, 128]), op=ALU.add),
             reads=[cumF, totF], writes=[cumF])
        eh3 = ehF[:, :].rearrange("p (c t) -> p c t", t=128)
        s.op("dve", lambda e: e.tensor_tensor(out=eh3, in0=totF[:, :].unsqueeze(2).to_broadcast([64, NCK, 128]), in1=cum3,
                                              op=ALU.subtract), reads=[cumF, totF], writes=[ehF])
        s.op("act", lambda e: e.activation(out=ehF[:], in_=ehF[:], func=AF.Exp), reads=[ehF], writes=[ehF])
        s.op("dve", lambda e: e.tensor_tensor(out=ehF[:], in0=ehF[:], in1=dtF[:], op=ALU.mult), reads=[ehF, dtF], writes=[ehF])
        for srcF, dstT in ((dtF, dtTok), (cumF, cumTok), (ehF, ehTok)):
            for q in range((NCK + 7) // 8):
                n8 = min(8, NCK - q * 8)
                pp = gbank()
                for j in range(n8):
                    ci = q * 8 + j
                    s.op("pe", lambda e: e.transpose(out=pp[:, j * 64:(j + 1) * 64], in_=srcF[:, ci * 128:(ci + 1) * 128],
                                                     identity=ident_f[0:64, 0:64]), reads=[srcF, ident_f], writes=[pp])
                gev(dstT, dstT[:, q * 8:q * 8 + n8, :], pp, pp[:, 0:n8 * 64].rearrange("p (j t) -> p j t", j=n8))

        s.barrier()
        ysg_ap = xg[:, :, 0:SEQ]
        zg_ap = gA[:, :].bitcast(BF16)[:, 0:2 * SEQ].rearrange("p (i t) -> p i t", i=2)

        def conv_silu(row0, ch, dst_t, dst_ap):
            s.dma("sp", gA[:], colsT[row0:row0 + 128, :], reads=[colsT], writes=[gA])
            s.op("dve", lambda e: e.tensor_scalar(out=gB[:], in0=gA[:], scalar1=cw[:, ch, 1:2], scalar2=cw[:, ch, 3:4],
                                                  op0=ALU.mult, op1=ALU.add), reads=[gA, cw], writes=[gB])
            for (a, b) in ((0, CTX), (CTX, TT)):
                s.op("dve", lambda e: e.scalar_tensor_tensor(out=gB[:, a + 1:b], in0=gA[:, a:b - 1], scalar=cw[:, ch, 0:1],
                                                             in1=gB[:, a + 1:b], op0=ALU.mult, op1=ALU.add),
                     reads=[gA, cw, gB], writes=[gB])
                s.op("dve", lambda e: e.scalar_tensor_tensor(out=gB[:, a:b - 1], in0=gA[:, a + 1:b], scalar=cw[:, ch, 2:3],
                                                             in1=gB[:, a:b - 1], op0=ALU.mult, op1=ALU.add),
                     reads=[gA, cw, gB], writes=[gB])
            s.op("act", lambda e: e.activation(out=dst_ap, in_=gB[:], func=AF.Silu), reads=[gB], writes=[dst_t])

        for g in range(0 if skip_mixer else 8):
            for i in range(2):
                conv_silu(XR0 + g * 256 + i * 128, g * 2 + i, xg, xg[:, i, :])
            conv_silu(BR0 + g * 128, 16 + g, Bg, Bg[:, :])
            conv_silu(CR0 + g * 128, 24 + g, Cg, Cg[:, :])
            for q in range((NCK + 3) // 4):
                n4 = min(4, NCK - q * 4)
                for j in range(n4):
                    ci = q * 4 + j
                    for i in range(2):
                        s.op("pe", lambda e: e.transpose(out=ptrb[:, (j * 2 + i) * 128:(j * 2 + i + 1) * 128],
                                                         in_=xg[:, i, ci * 128:(ci + 1) * 128], identity=ident[:]),
                             reads=[xg, ident], writes=[ptr])
                gev(XTok, XTok[:, q * 4:q * 4 + n4, :], ptr, ptrb[:, 0:n4 * 256].rearrange("p (j t) -> p j t", j=n4))
            for q in range((NCK + 7) // 8):
                n8 = min(8, NCK - q * 8)
                for j in range(n8):
                    ci = q * 8 + j
                    s.op("pe", lambda e: e.transpose(out=ptrb[:, j * 128:(j + 1) * 128], in_=Bg[:, ci * 128:(ci + 1) * 128],
                                                     identity=ident[:]), reads=[Bg, ident], writes=[ptr])
                gev(BTok, BTok[:, q * 8:q * 8 + n8, :], ptr, ptrb[:, 0:n8 * 128].rearrange("p (j t) -> p j t", j=n8))
            for d in range(2):
                s.op("pool", lambda e: e.memset(SSf[:], 0.0), writes=[SSf])
                s.op("pool", lambda e: e.memset(SSb[:], 0.0), writes=[SSb])
                order = list(range(NCK)) if d == 0 else [1, 0] + list(range(NCK - 1, 1, -1))
                mcb = msk[:, 1, :] if d == 0 else msk[:, 3, :]
                for ci in order:
                    tsl = slice(ci * 128, (ci + 1) * 128)
                    pcb = gbank()
                    s.op("pe", lambda e: e.matmul(pcb[:, 0:128], lhsT=Bg[:, tsl], rhs=Cg[:, tsl], start=True, stop=True),
                         reads=[Bg, Cg], writes=[pcb])
                    cb = cbm[(gc["w"] // 4) % 2]
                    s.op("dve", lambda e: e.tensor_tensor(out=cb[:], in0=pcb[:, 0:128], in1=mcb, op=ALU.mult),
                         reads=[pcb, msk], writes=[cb])
                    pbcs = []
                    for hh in range(4):
                        dh = d * 32 + g * 4 + hh
                        pbc = gbank()
                        s.op("pe", lambda e: e.matmul(pbc[:, 0:128], lhsT=ident_f[0:64, dh:dh + 1].to_broadcast([64, 128]),
                                                      rhs=cumF[:, tsl], start=True, stop=True),
                             reads=[ident_f, cumF], writes=[pbc])
                        pbcs.append(pbc)
                    ebs = []
                    wis = []
                    for hh in range(4):
                        dh = d * 32 + g * 4 + hh
                        gc["w"] += 1
                        wi = gc["w"] % NWS
                        wis.append(wi)
                        pbc = pbcs[hh]
                        s.op("dve", lambda e: e.tensor_scalar(out=w_d[wi][:], in0=pbc[:, 0:128], scalar1=cumTok[:, ci, dh:dh + 1],
                                                              scalar2=0.0, op0=ALU.subtract, op1=ALU.min),
                             reads=[pbc, cumTok], writes=[w_d[wi]])
                        s.op("act", lambda e: e.activation(out=w_eb[wi][:], in_=pbc[:, 0:128], func=AF.Exp),
                             reads=[pbc], writes=[w_eb[wi]])
                        s.op("pool", lambda e: e.tensor_scalar(out=w_bh[wi][:], in0=BTok[:, ci, :], scalar1=ehTok[:, ci, dh:dh + 1],
                                                               scalar2=None, op0=ALU.mult), reads=[BTok, ehTok], writes=[w_bh[wi]])
                        ebs.append(w_eb[wi])
                    for hh in range(4):
                        dh = d * 32 + g * 4 + hh
                        wi = wis[hh]
                        s.op("act", lambda e: e.activation(out=w_e[wi][:], in_=w_d[wi][:], func=AF.Exp),
                             reads=[w_d[wi]], writes=[w_e[wi]])
                        s.op("pool", lambda e: e.tensor_tensor(out=w_ct[wi][:], in0=w_eb[wi][:], in1=Cg[:, tsl], op=ALU.mult),
                             reads=[w_eb[wi], Cg], writes=[w_ct[wi]])
                    for hh in range(4):
                        dh = d * 32 + g * 4 + hh
                        wi = wis[hh]
                        s.op("dve", lambda e: e.scalar_tensor_tensor(out=w_dcb[wi][:], in0=w_e[wi][:],
                                                                     scalar=dtTok[:, ci, dh:dh + 1], in1=cb[:],
                                                                     op0=ALU.mult, op1=ALU.mult),
                             reads=[w_e[wi], dtTok, cb], writes=[w_dcb[wi]])
                    py = gbank()
                    pst = gbank()
                    for hh in range(4):
                        wi = wis[hh]
                        hc = slice(hh * 64, (hh + 1) * 64)
                        s.op("pe", lambda e: e.matmul(pst[:, hc], lhsT=w_bh[wi][:], rhs=XTok[:, ci, hc], start=True, stop=True),
                             reads=[w_bh[wi], XTok], writes=[pst])
                    for hh in range(4):
                        wi = wis[hh]
                        hc = slice(hh * 64, (hh + 1) * 64)
                        s.op("pe", lambda e: e.matmul(py[:, hc], lhsT=w_dcb[wi][:], rhs=XTok[:, ci, hc], start=True, stop=False),
                             reads=[w_dcb[wi], XTok], writes=[py])
                        s.op("pe", lambda e: e.matmul(py[:, hc], lhsT=w_ct[wi][:], rhs=SSb[:, hc], start=False, stop=True),
                             reads=[w_ct[wi], SSb], writes=[py])
                    if ci >= 2:
                        if d == 0:
                            gev(yacc, yacc[:, ci - 2, :], py, py[:, 0:256])
                        else:
                            s.op("dve", lambda e: e.tensor_tensor(out=yacc[:, ci - 2, :], in0=py[:, 0:256], in1=yacc[:, ci - 2, :],
                                                                  op=ALU.add), reads=[py, yacc], writes=[yacc])
                    col = 127 if d == 0 else 0
                    for hh in range(4):
                        hc = slice(hh * 64, (hh + 1) * 64)
                        s.op("dve", lambda e: e.scalar_tensor_tensor(out=SSf[:, hc], in0=SSf[:, hc], scalar=ebs[hh][:, col:col + 1],
                                                                     in1=pst[:, hc], op0=ALU.mult, op1=ALU.add),
                             reads=[SSf, ebs[hh], pst], writes=[SSf])
                    s.op("act", lambda e: e.copy(out=SSb[:], in_=SSf[:]), reads=[SSf], writes=[SSb])
            gsl = slice(g * 256, (g + 1) * 256)
            for i in range(2):
                s.dma("pool", zg_ap[:, i, :], colsT[ZR0 + g * 256 + i * 128:ZR0 + g * 256 + (i + 1) * 128, CTX:TT],
                      reads=[colsT], writes=[gA])
            s.dma("act", dbc[:], dbc_in[0:1, gsl].partition_broadcast(128), reads=[dbc_in], writes=[dbc])
            s.dma("act", nwb[:], nwb_in[0:1, gsl].partition_broadcast(128), reads=[nwb_in], writes=[nwb])
            for lc in range(32):
                y_, z_, o_, st_ = fy[lc % 2], fz[lc % 2], fo[lc % 2], fst[lc % 2]
                s.op("pool", lambda e: e.tensor_tensor(out=y_[:], in0=XTok[:, lc + 2, :], in1=dbc[:], op=ALU.mult),
                     reads=[XTok, dbc], writes=[y_])
                s.op("dve", lambda e: e.tensor_tensor(out=y_[:], in0=y_[:], in1=yacc[:, lc, :], op=ALU.add),
                     reads=[y_, yacc], writes=[y_])
                for i in range(2):
                    s.op("pe", lambda e: e.transpose(out=ptrb[:, i * 128:(i + 1) * 128], in_=zg_ap[:, i, lc * 128:(lc + 1) * 128],
                                                     identity=ident[:]), reads=[gA, ident], writes=[ptr])
                s.op("act", lambda e: e.activation(out=z_[:], in_=ptrb[:, 0:256], func=AF.Silu), reads=[ptr], writes=[z_])
                s.op("dve", lambda e: e.tensor_tensor(out=y_[:], in0=y_[:], in1=z_[:], op=ALU.mult), reads=[y_, z_], writes=[y_])
                s.op("act", lambda e: e.activation(out=fsq[:], in_=y_[:], func=AF.Square, accum_out=st_[:, 0:1]),
                     reads=[y_], writes=[fsq, st_])
                s.op("dve", lambda e: e.tensor_scalar(out=st_[:, 1:2], in0=st_[:, 0:1], scalar1=1.0 / 256, scalar2=EPS,
                                                      op0=ALU.mult, op1=ALU.add), reads=[st_], writes=[st_])
                s.op("act", lambda e: e.activation(out=st_[:, 1:2], in_=st_[:, 1:2], func=AF.Sqrt), reads=[st_], writes=[st_])
                s.op("dve", lambda e: e.reciprocal(out=st_[:, 1:2], in_=st_[:, 1:2]), reads=[st_], writes=[st_])
                s.op("dve", lambda e: e.scalar_tensor_tensor(out=o_[:], in0=y_[:], scalar=st_[:, 1:2], in1=nwb[:],
                                                             op0=ALU.mult, op1=ALU.mult), reads=[y_, st_, nwb], writes=[o_])
                for i in range(2):
                    s.op("pe", lambda e: e.transpose(out=ptrb[:, 256 + i * 128:256 + (i + 1) * 128], in_=o_[:, i * 128:(i + 1) * 128],
                                                     identity=ident[:]), reads=[o_, ident], writes=[ptr])
                gev(xg, ysg_ap[:, :, lc * 128:(lc + 1) * 128], ptr, ptrb[:, 256:512].rearrange("p (i t) -> p i t", i=2))
            for i in range(2):
                r0 = RW + g * 256 + i * 128
                s.dma("sp", yT[r0:r0 + 128, :], ysg_ap[:, i, :], reads=[xg], writes=[yT])
            if dbg == "ssd1":
                break
        s.barrier()
    s.stack = st


    HT = 2048
    xh = din("xh", [HT, D])
    selc_in = din("selc", [128, 2])
    w_out_r = din("w_out_r", [8, 128, 32 * 512])
    norm2_g = din("norm2_g", [1, D])
    final_g = din("final_g", [1, D])
    wq_r = din("wq_r", [16, 128, 32 * 128])
    kT_in = din("kT", [128, 2, 8, 128])
    uT_r = din("uT_r", [128, 128, 32 * 128])
    peer_v = din("peer_v", [16384, D])
    h1D = s.dram("h1D", [HT, D], F32, kind=("ExternalOutput" if dbg in ("h1", "h1t") else "Internal"))
    h2D = s.dram("h2D", [HT, D], F32)
    xn2T = s.dram("xn2T", [D, HT], BF16, kind=("ExternalOutput" if dbg in ("h1", "h1t") else "Internal"))
    WrD = s.dram("WrD", [256, 128, 128], BF16)
    OhD = s.dram("OhD", [256, 128, 128], BF16)
    outD = s.dram("out", [HT, D], F32, kind="ExternalOutput")
    selc = s.sb("selc_sb", [128, 2], F32)
    s.dma("sp", selc[:], selc_in[:, :], reads=[selc_in], writes=[selc])

    with ExitStack() as ph1:
        s.stack = ph1
        wsl = [s.sb("wsl%d" % i, [128, 32, 512], BF16) for i in range(2)]
        yTb = s.sb("yTb", [128, 32, 1024], BF16)
        la = [s.sb("la%d" % i, [128, 1024], BF16) for i in range(2)]
        lb = [s.sb("lb%d" % i, [128, 1024], BF16) for i in range(2)]
        gt1 = s.sb("gt1", [128, D], F32)
        xr = [s.sb("xr%d" % i, [128, 512], F32) for i in range(3)]
        orow = [s.sb("orow%d" % i, [128, 512], F32) for i in range(3)]
        s.dma("act", gt1[:], modx[:, 2 * D:3 * D], reads=[modx], writes=[gt1])
        wi = 0
        oi = 0
        for tb in range(2):
            for kc in range(32):
                a_, b_ = la[kc % 2], lb[kc % 2]
                s.dma("sp", a_[:], yT[kc * 128:(kc + 1) * 128, tb * 1024:(tb + 1) * 1024], reads=[yT], writes=[a_])
                s.dma("act", b_[:], yT[kc * 128:(kc + 1) * 128, HT + tb * 1024:HT + (tb + 1) * 1024], reads=[yT], writes=[b_])
                s.op("dve", lambda e: e.tensor_scalar(out=yTb[:, kc, :], in0=a_[:], scalar1=selc[:, 0:1], scalar2=None, op0=ALU.mult),
                     reads=[a_, selc], writes=[yTb])
                s.op("dve", lambda e: e.scalar_tensor_tensor(out=yTb[:, kc, :], in0=b_[:], scalar=selc[:, 1:2], in1=yTb[:, kc, :],
                                                             op0=ALU.mult, op1=ALU.add), reads=[b_, selc, yTb], writes=[yTb])
            for dg in range(8):
                w_ = wsl[wi % 2]; wi += 1
                s.dma("pool", w_[:], w_out_r[dg].rearrange("p (k c) -> p k c", k=32), reads=[w_out_r], writes=[w_])
                for tl in range(8):
                    pp = pb[oi % 4]
                    for kc in range(32):
                        s.op("pe", lambda e: e.matmul(pp[:, :], lhsT=yTb[:, kc, tl * 128:(tl + 1) * 128], rhs=w_[:, kc, :],
                                                      start=(kc == 0), stop=(kc == 31)), reads=[yTb, w_], writes=[pp])
                    r0 = tb * 1024 + tl * 128
                    x_ = xr[oi % 3]; o_ = orow[oi % 3]; oi += 1
                    s.dma("act", x_[:], xh[r0:r0 + 128, dg * 512:(dg + 1) * 512], reads=[xh], writes=[x_])
                    s.op("dve", lambda e: e.tensor_tensor(out=o_[:], in0=pp[:, :], in1=gt1[:, dg * 512:(dg + 1) * 512], op=ALU.mult),
                         reads=[pp, gt1], writes=[o_])
                    s.op("pool", lambda e: e.tensor_tensor(out=o_[:], in0=o_[:], in1=x_[:], op=ALU.add), reads=[o_, x_], writes=[o_])
                    s.dma("sp", h1D[r0:r0 + 128, dg * 512:(dg + 1) * 512], o_[:], reads=[o_], writes=[h1D])
        s.barrier()
    s.stack = st

    with ExitStack() as ph2:
        s.stack = ph2
        G2 = s.sb("G2", [128, D], F32)
        SH2 = s.sb("SH2", [128, D], F32)
        g2b_ = s.sb("g2bb", [128, D], F32)
        hx = [s.sb("hx%d" % i, [128, D], F32) for i in range(2)]
        hn = [s.sb("hn%d" % i, [128, D], BF16) for i in range(2)]
        sq2 = s.sb("sq2", [128, D], BF16)
        ss2 = [s.sb("ss2_%d" % i, [128, 2], F32) for i in range(2)]
        nT2 = [s.sb("nT2_%d" % i, [128, 32, 512], BF16) for i in range(2)]
        s.dma("sp", g2b_[:], norm2_g[0:1, :].partition_broadcast(128), reads=[norm2_g], writes=[g2b_])
        s.dma("sp", G2[:], modx[:, 4 * D:5 * D], reads=[modx], writes=[G2])
        s.dma("act", SH2[:], modx[:, 3 * D:4 * D], reads=[modx], writes=[SH2])
        s.op("dve", lambda e: e.scalar_tensor_tensor(out=G2[:], in0=G2[:], scalar=1.0, in1=g2b_[:], op0=ALU.add, op1=ALU.mult),
             reads=[G2, g2b_], writes=[G2])
        ev = 0
        for tl in range(16):
            x_, n_, s_ = hx[tl % 2], hn[tl % 2], ss2[tl % 2]
            nt = nT2[(tl // 4) % 2]
            s.dma("sp", x_[:], h1D[tl * 128:(tl + 1) * 128, :], reads=[h1D], writes=[x_])
            s.op("act", lambda e: e.activation(out=sq2[:], in_=x_[:], func=AF.Square, accum_out=s_[:, 0:1]), reads=[x_], writes=[sq2, s_])
            s.op("dve", lambda e: e.tensor_scalar(out=s_[:, 1:2], in0=s_[:, 0:1], scalar1=1.0 / D, scalar2=EPS, op0=ALU.mult, op1=ALU.add),
                 reads=[s_], writes=[s_])
            s.op("act", lambda e: e.activation(out=s_[:, 1:2], in_=s_[:, 1:2], func=AF.Sqrt), reads=[s_], writes=[s_])
            s.op("dve", lambda e: e.reciprocal(out=s_[:, 1:2], in_=s_[:, 1:2]), reads=[s_], writes=[s_])
            s.op("dve", lambda e: e.scalar_tensor_tensor(out=x_[:], in0=x_[:], scalar=s_[:, 1:2], in1=G2[:], op0=ALU.mult, op1=ALU.mult),
                 reads=[x_, s_, G2], writes=[x_])
            s.op("pool", lambda e: e.tensor_tensor(out=n_[:], in0=x_[:], in1=SH2[:], op=ALU.add), reads=[x_, SH2], writes=[n_])
            for q4 in range(4):
                pt = pb[4 + (ev % 2)]; ev += 1
                ptb = pt[:, :].bitcast(BF16)
                for j in range(8):
                    kc = q4 * 8 + j
                    s.op("pe", lambda e: e.transpose(out=ptb[:, j * 128:(j + 1) * 128], in_=n_[:, kc * 128:(kc + 1) * 128], identity=ident[:]),
                         reads=[n_, ident], writes=[pt])
                dst = nt[:, q4 * 8:(q4 + 1) * 8, (tl % 4) * 128:(tl % 4 + 1) * 128]
                srcp = ptb.rearrange("p (j t) -> p j t", j=8)
                if q4 % 2 == 0:
                    s.op("act", lambda e: e.copy(out=dst, in_=srcp), reads=[pt], writes=[nt])
                else:
                    s.op("dve", lambda e: e.tensor_copy(out=dst, in_=srcp), reads=[pt], writes=[nt])
            if tl % 4 == 3:
                t0 = (tl // 4) * 512
                for kq in range(4):
                    s.dma("sp", xn2T[kq * 1024:(kq + 1) * 1024, t0:t0 + 512].rearrange("(k p) t -> p k t", p=128),
                          nt[:, kq * 8:(kq + 1) * 8, :], reads=[nt], writes=[xn2T])
        s.barrier()
    s.stack = st

    if dbg not in ("h1", "h1t"):
      with ExitStack() as pp_:
        s.stack = pp_
        TB = 256
        xb = s.sb("xb", [128, 32, TB], BF16)
        WT = s.sb("WT", [128, 128, TB], BF16)
        wb3 = [s.sb("wb3_%d" % i, [128, 4096], BF16) for i in range(3)]
        qT = s.sb("qT", [128, 16, TB], BF16)
        kT = s.sb("kT_sb", [128, 2, 8, 128], BF16)
        sc = s.sb("sc", [128, 16, 128], F32)
        vv = s.sb("vv", [128, 16, 16], F32)
        cand = s.sb("cand", [128, 8, 256], F32)
        ec = s.sb("ec", [128, 8, 256], F32)
        E2 = s.sb("E2", [128, 8, 128], F32)
        tmpk = s.sb("tmpk", [128, 256], F32)
        m8 = s.sb("m8", [128, 8], F32)
        th = s.sb("th", [128, 8], F32)
        negm = s.sb("negm", [128, 8, 3], F32)
        Zs = s.sb("Zs", [128, 8], F32)
        c1 = s.sb("c1", [128, 8, 16], F32)
        thr = s.sb("thr", [128, 8, 16], F32)
        wst = [s.sb("wst%d" % i, [128, 16, 128], BF16) for i in range(2)]
        ost = [s.sb("ost%d" % i, [128, 16, 128], BF16) for i in range(2)]
        Wk = s.sb("Wk", [128, 16, 128], BF16)
        Ok = s.sb("Ok", [128, 16, 128], BF16)
        gl = [s.sb("gl%d" % i, [128, TB], BF16) for i in range(2)]
        hp_ = [s.sb("hp%d" % i, [128, 512], F32) for i in range(3)]
        gp_ = [s.sb("gp%d" % i, [128, 512], F32) for i in range(2)]
        op_ = [s.sb("op%d" % i, [128, 512], F32) for i in range(3)]
        s.dma("pool", kT[:], kT_in[:, :, :, :], reads=[kT_in], writes=[kT])
        pc = {"w": 0, "bank": 0, "ev": 0, "o": 0}

        def pbank():
            pc["bank"] += 1
            return pb[pc["bank"] % 8]

        def wbuf():
            pc["w"] += 1
            return wb3[pc["w"] % 3]

        def pev(dst_t, dst_ap, src_t, src_ap):
            pc["ev"] += 1
            if pc["ev"] % 2 == 0:
                s.op("act", lambda e: e.copy(out=dst_ap, in_=src_ap), reads=[src_t], writes=[dst_t])
            else:
                s.op("dve", lambda e: e.tensor_copy(out=dst_ap, in_=src_ap), reads=[src_t], writes=[dst_t])

        NBLK = HT // TB if dbg != "peer1" else 1
        for blk_i in range(NBLK):
            t0 = blk_i * TB
            for kq in range(4):
                s.dma("sp", xb[:, kq * 8:(kq + 1) * 8, :],
                      xn2T[kq * 1024:(kq + 1) * 1024, t0:t0 + TB].rearrange("(k p) t -> p k t", p=128), reads=[xn2T], writes=[xb])
            for qc in range(16):
                w_ = wbuf()
                wv = w_[:, :].rearrange("p (k c) -> p k c", k=32)
                s.dma("pool", w_[:], wq_r[qc], reads=[wq_r], writes=[w_])
                pp = pbank()
                for kc in range(32):
                    s.op("pe", lambda e: e.matmul(pp[:, 0:TB], lhsT=wv[:, kc, :], rhs=xb[:, kc, :], start=(kc == 0), stop=(kc == 31)),
                         reads=[w_, xb], writes=[pp])
                pev(qT, qT[:, qc, :], pp, pp[:, 0:TB])
            for tile in range(TB // 128):
                tt = tile * 128
                for q4 in range(4):
                    pp = pbank()
                    for j in range(4):
                        hw = q4 * 4 + j
                        s.op("pe", lambda e: e.matmul(pp[:, j * 128:(j + 1) * 128], lhsT=qT[:, hw, tt:tt + 128],
                                                      rhs=kT[:, hw % 2, hw // 2, :], start=True, stop=True), reads=[qT, kT], writes=[pp])
                    pev(sc, sc[:, q4 * 4:(q4 + 1) * 4, :], pp, pp[:, :].rearrange("p (j k) -> p j k", j=4))
                for hw in range(16):
                    s.op("dve", lambda e: e.max(out=vv[:, hw, 0:8], in_=sc[:, hw, :]), reads=[sc], writes=[vv])
                    s.op("dve", lambda e: e.match_replace(out=tmpk[:, 0:128], in_to_replace=vv[:, hw, 0:8], in_values=sc[:, hw, :],
                                                          imm_value=-1e30), reads=[sc, vv], writes=[tmpk])
                    s.op("dve", lambda e: e.max(out=vv[:, hw, 8:16], in_=tmpk[:, 0:128]), reads=[tmpk], writes=[vv])
                v4 = vv[:, :, :].rearrange("p (h w) r -> p h w r", w=2)
                cand4 = cand[:, :, :].rearrange("p h (i j) -> p h i j", j=16)
                s.op("dve", lambda e: e.tensor_tensor(out=cand4, in0=v4[:, :, 0, :].unsqueeze(3).to_broadcast([128, 8, 16, 16]),
                                                      in1=v4[:, :, 1, :].unsqueeze(2).to_broadcast([128, 8, 16, 16]), op=ALU.add),
                     reads=[vv], writes=[cand])
                for h in range(8):
                    s.op("dve", lambda e: e.max(out=m8[:], in_=cand[:, h, :]), reads=[cand], writes=[m8])
                    s.op("dve", lambda e: e.match_replace(out=tmpk[:], in_to_replace=m8[:], in_values=cand[:, h, :], imm_value=-1e30),
                         reads=[cand, m8], writes=[tmpk])
                    s.op("dve", lambda e: e.max(out=m8[:], in_=tmpk[:]), reads=[tmpk], writes=[m8])
                    s.op("dve", lambda e: e.tensor_copy(out=th[:, h:h + 1], in_=m8[:, 7:8]), reads=[m8], writes=[th])
                s.op("dve", lambda e: e.tensor_scalar(out=negm[:, :, 0:2], in0=v4[:, :, :, 0], scalar1=-1.0, scalar2=None, op0=ALU.mult),
                     reads=[vv], writes=[negm])
                s.op("dve", lambda e: e.tensor_tensor(out=negm[:, :, 2], in0=negm[:, :, 0], in1=negm[:, :, 1], op=ALU.add),
                     reads=[negm], writes=[negm])
                for h in range(8):
                    s.op("act", lambda e: e.activation(out=ec[:, h, :], in_=cand[:, h, :], func=AF.Exp, bias=negm[:, h, 2:3]),
                         reads=[cand, negm], writes=[ec])
                    s.op("dve", lambda e: e.scalar_tensor_tensor(out=tmpk[:], in0=cand[:, h, :], scalar=th[:, h:h + 1], in1=ec[:, h, :],
                                                                 op0=ALU.is_ge, op1=ALU.mult, accum_out=Zs[:, h:h + 1]),
                         reads=[cand, th, ec], writes=[tmpk, Zs])
                    s.op("act", lambda e: e.activation(out=E2[:, h, :], in_=sc[:, 2 * h + 1, :], func=AF.Exp, bias=negm[:, h, 1:2]),
                         reads=[sc, negm], writes=[E2])
                s.op("dve", lambda e: e.reciprocal(out=Zs[:], in_=Zs[:]), reads=[Zs], writes=[Zs])
                s.op("dve", lambda e: e.tensor_tensor(out=c1[:], in0=v4[:, :, 0, :], in1=negm[:, :, 0:1].to_broadcast([128, 8, 16]), op=ALU.add),
                     reads=[vv, negm], writes=[c1])
                s.op("act", lambda e: e.activation(out=c1[:], in_=c1[:], func=AF.Exp), reads=[c1], writes=[c1])
                s.op("dve", lambda e: e.tensor_tensor(out=c1[:], in0=c1[:], in1=Zs[:, :].unsqueeze(2).to_broadcast([128, 8, 16]), op=ALU.mult),
                     reads=[c1, Zs], writes=[c1])
                s.op("dve", lambda e: e.tensor_tensor(out=thr[:], in0=th[:, :].unsqueeze(2).to_broadcast([128, 8, 16]), in1=v4[:, :, 0, :],
                                                      op=ALU.subtract), reads=[th, vv], writes=[thr])
                for h in range(8):
                    ws_, os_ = wst[h % 2], ost[h % 2]
                    for r in range(16):
                        s.op("dve", lambda e: e.scalar_tensor_tensor(out=ws_[:, r, :], in0=sc[:, 2 * h + 1, :], scalar=thr[:, h, r:r + 1],
                                                                     in1=E2[:, h, :], op0=ALU.is_ge, op1=ALU.mult),
                             reads=[sc, thr, E2], writes=[ws_])
                        s.op("pool", lambda e: e.tensor_scalar(out=os_[:, r, :], in0=sc[:, 2 * h, :], scalar1=vv[:, 2 * h, r:r + 1],
                                                               scalar2=c1[:, h, r:r + 1], op0=ALU.is_equal, op1=ALU.mult),
                             reads=[sc, vv, c1], writes=[os_])
                    s.dma("sp", WrD[tt:tt + 128, h * 16:(h + 1) * 16, :], ws_[:], reads=[ws_], writes=[WrD])
                    s.dma("act", OhD[tt:tt + 128, h * 16:(h + 1) * 16, :], os_[:], reads=[os_], writes=[OhD])
                for sub in range(8):
                    tk = tt + sub * 16
                    s.dma("sp", Wk[:], WrD[tk:tk + 16].rearrange("t k e -> k t e"), reads=[WrD], writes=[Wk])
                    s.dma("act", Ok[:], OhD[tk:tk + 16].rearrange("t k e -> k t e"), reads=[OhD], writes=[Ok])
                    for q in range(4):
                        pp = pbank()
                        for j in range(4):
                            t = q * 4 + j
                            s.op("pe", lambda e: e.matmul(pp[:, j * 128:(j + 1) * 128], lhsT=Wk[:, t, :], rhs=Ok[:, t, :], start=True, stop=True),
                                 reads=[Wk, Ok], writes=[pp])
                        pev(WT, WT[:, :, tk + q * 4:tk + q * 4 + 4], pp, pp[:, :].rearrange("p (t e) -> p e t", t=4))
            for e1 in range(128):
                w_ = wbuf()
                wv = w_[:, :].rearrange("p (k c) -> p k c", k=32)
                s.dma("pool", w_[:], uT_r[e1], reads=[uT_r], writes=[w_])
                pp = pbank()
                for kc in range(32):
                    s.op("pe", lambda e: e.matmul(pp[:, 0:TB], lhsT=wv[:, kc, :], rhs=xb[:, kc, :], start=(kc == 0), stop=(kc == 31)),
                         reads=[w_, xb], writes=[pp])
                g_ = gl[e1 % 2]
                s.op("act", lambda e: e.activation(out=g_[:], in_=pp[:, 0:TB], func=AF.Gelu), reads=[pp], writes=[g_])
                s.op("dve", lambda e: e.tensor_tensor(out=WT[:, e1, :], in0=WT[:, e1, :], in1=g_[:], op=ALU.mult), reads=[WT, g_], writes=[WT])
            for dh in range(2):
                for e1 in range(128):
                    w_ = wbuf()
                    s.dma("pool", w_[:, 0:2048], peer_v[e1 * 128:(e1 + 1) * 128, dh * 2048:(dh + 1) * 2048], reads=[peer_v], writes=[w_])
                    for tile in range(2):
                        for dg in range(4):
                            pp = pb[tile * 4 + dg]
                            s.op("pe", lambda e: e.matmul(pp[:, :], lhsT=WT[:, e1, tile * 128:(tile + 1) * 128], rhs=w_[:, dg * 512:(dg + 1) * 512],
                                                          start=(e1 == 0), stop=(e1 == 127)), reads=[WT, w_], writes=[pp])
                for dg in range(4):
                    c0 = dh * 2048 + dg * 512
                    g2_ = gp_[dg % 2]
                    s.dma("act", g2_[:], modx[:, 5 * D + c0:5 * D + c0 + 512], reads=[modx], writes=[g2_])
                    for tile in range(2):
                        pp = pb[tile * 4 + dg]
                        r0 = t0 + tile * 128
                        h_ = hp_[pc["o"] % 3]; o_ = op_[pc["o"] % 3]; pc["o"] += 1
                        s.dma("sp", h_[:], h1D[r0:r0 + 128, c0:c0 + 512], reads=[h1D], writes=[h_])
                        s.op("dve", lambda e: e.tensor_tensor(out=o_[:], in0=pp[:, :], in1=g2_[:], op=ALU.mult), reads=[pp, g2_], writes=[o_])
                        s.op("pool", lambda e: e.tensor_tensor(out=o_[:], in0=o_[:], in1=h_[:], op=ALU.add), reads=[o_, h_], writes=[o_])
                        s.dma("sp", h2D[r0:r0 + 128, c0:c0 + 512], o_[:], reads=[o_], writes=[h2D])
        s.barrier()
      s.stack = st

      with ExitStack() as pf:
        s.stack = pf
        fgb = s.sb("fgb", [128, D], F32)
        fx = [s.sb("fx%d" % i, [128, D], F32) for i in range(2)]
        fsq2 = s.sb("fsq2", [128, D], BF16)
        fs = [s.sb("fs%d" % i, [128, 2], F32) for i in range(2)]
        s.dma("sp", fgb[:], final_g[0:1, :].partition_broadcast(128), reads=[final_g], writes=[fgb])
        for tl in range(16 if dbg != "peer1" else 2):
            x_, s_ = fx[tl % 2], fs[tl % 2]
            s.dma("sp", x_[:], h2D[tl * 128:(tl + 1) * 128, :], reads=[h2D], writes=[x_])
            s.op("act", lambda e: e.activation(out=fsq2[:], in_=x_[:], func=AF.Square, accum_out=s_[:, 0:1]), reads=[x_], writes=[fsq2, s_])
            s.op("dve", lambda e: e.tensor_scalar(out=s_[:, 1:2], in0=s_[:, 0:1], scalar1=1.0 / D, scalar2=EPS, op0=ALU.mult, op1=ALU.add),
                 reads=[s_], writes=[s_])
            s.op("act", lambda e: e.activation(out=s_[:, 1:2], in_=s_[:, 1:2], func=AF.Sqrt), reads=[s_], writes=[s_])
            s.op("dve", lambda e: e.reciprocal(out=s_[:, 1:2], in_=s_[:, 1:2]), reads=[s_], writes=[s_])
            s.op("dve", lambda e: e.scalar_tensor_tensor(out=x_[:], in0=x_[:], scalar=s_[:, 1:2], in1=fgb[:], op0=ALU.mult, op1=ALU.mult),
                 reads=[x_, s_, fgb], writes=[x_])
            s.dma("act", outD[tl * 128:(tl + 1) * 128, :], x_[:], reads=[x_], writes=[outD])
        s.barrier()
      s.stack = st

    s.finish()
    st.close()
    return nc


def make_shared(inp):
    f = lambda a: np.ascontiguousarray(np.asarray(a, dtype=np.float32))
    m = {}
    m["w_mod"] = f(inp["w_mod"][0])
    m["b_mod"] = f(inp["b_mod"])
    m["norm1_g"] = f(inp["norm1_g"])
    w = np.zeros((D, NCOLP), np.float32)
    w[:, :NCOLS] = np.asarray(inp["w_in"][0])
    m["w_in_r"] = f(w.reshape(32, 128, NCH, 128).transpose(2, 1, 0, 3).reshape(NCH, 128, 32 * 128))
    m["ident_in"] = np.eye(128, dtype=np.float32)
    mu = np.asarray(inp["rwkv_mu"][0])
    mu_u = np.zeros((128, 54), np.float32)
    for which in range(3):
        for hp in range(16):
            mu_u[:, which * 16 + hp] = mu[which * RW + hp * 128: which * RW + (hp + 1) * 128]
    for d in range(2):
        mu_u[:96, 48 + d] = mu[6144 + 96 * d: 6144 + 96 * (d + 1)]
        mu_u[:96, 50 + d] = mu[6336 + 96 * d: 6336 + 96 * (d + 1)]
        mu_u[:, 52 + d] = mu[6528 + 128 * d: 6528 + 128 * (d + 1)]
    m["mu_u"] = mu_u
    p = np.arange(128)
    slotm = np.zeros((128, 6), np.float32)
    for k in range(4):
        slotm[:, k] = (p % 4 == k)
    for k in range(2):
        slotm[:, 4 + k] = (p % 2 == k)
    m["slotm"] = slotm
    m["w2r"] = f(np.asarray(inp["rwkv_w2"][0]).transpose(1, 0, 2))
    m["a2r"] = f(np.asarray(inp["rwkv_a2"][0]).transpose(1, 0, 2))
    m["g2r"] = f(np.asarray(inp["rwkv_g2"][0]).reshape(2, 128, RW).transpose(1, 0, 2))
    cols = [np.asarray(inp["rwkv_w0"][0])[0], np.asarray(inp["rwkv_w0"][0])[1],
            np.asarray(inp["rwkv_a0"][0])[0], np.asarray(inp["rwkv_a0"][0])[1],
            np.asarray(inp["rwkv_k_k"][0]), np.asarray(inp["rwkv_k_a"][0]),
            np.asarray(inp["rwkv_r_k"][0]).reshape(RW), np.asarray(inp["rwkv_ln_w"][0]), np.asarray(inp["rwkv_ln_b"][0])]
    m["chp"] = f(np.stack([c.reshape(16, 128).T for c in cols], axis=2))
    o = np.ones((128, 128), np.float32)
    m["msk"] = f(np.stack([np.triu(o, 1), np.triu(o, 0), np.tril(o, -1), np.tril(o, 0)], axis=1))
    m["blk"] = f((p[:, None] // 64 == p[None, :] // 64))
    cwt = np.concatenate([np.asarray(inp["ssm_conv_w"][0]), np.asarray(inp["ssm_conv_b"])], axis=0)
    m["cw"] = f(cwt.reshape(4, 32, 128).transpose(2, 1, 0))
    m["dtp"] = f(np.stack([np.asarray(inp["ssm_dt_bias"][0]).reshape(64), np.asarray(inp["ssm_a_log"][0]).reshape(64)], axis=1))
    m["dbc"] = f(np.repeat(np.asarray(inp["ssm_d"][0]), 64)[None, :])
    m["nwb"] = f(np.asarray(inp["ssm_norm_w"]))
    m["w_out_r"] = f(np.asarray(inp["w_out"][0]).reshape(32, 128, 8, 512).transpose(2, 1, 0, 3).reshape(8, 128, 32 * 512))
    m["norm2_g"] = f(inp["norm2_g"])
    m["final_g"] = f(np.asarray(inp["final_g"])[None, :])
    m["wq_r"] = f(np.asarray(inp["peer_wq"][0]).reshape(32, 128, 16, 128).transpose(2, 1, 0, 3).reshape(16, 128, 32 * 128))
    k1 = np.asarray(inp["peer_k1"][0]); k2 = np.asarray(inp["peer_k2"][0])
    m["kT"] = f(np.stack([k1.transpose(2, 0, 1), k2.transpose(2, 0, 1)], axis=1))
    u = np.asarray(inp["peer_u"][0])
    m["uT_r"] = f(u.reshape(128, 128, 32, 128).transpose(0, 3, 2, 1).reshape(128, 128, 32 * 128))
    m["peer_v"] = f(inp["peer_v"][0])
    return m


def make_inputs(inp, core, shared=None):
    b, hf = core // 2, core % 2
    f = lambda a: np.ascontiguousarray(np.asarray(a, dtype=np.float32))
    m = dict(shared if shared is not None else make_shared(inp))
    m["xs"] = f(np.concatenate([inp["ctx"][b], inp["x"][b]], axis=0))
    cc = np.stack([np.asarray(inp["c"])[b].reshape(32, 128).T, np.asarray(inp["c_ctx"]).reshape(32, 128).T], axis=1)
    m["cc"] = f(cc)
    m["xh"] = f(np.asarray(inp["x"])[b, hf * 2048:(hf + 1) * 2048])
    sel = np.zeros((128, 2), np.float32); sel[:, hf] = 1.0
    m["selc"] = sel
    return m


def kernel(**inputs):
    nc = build_program()
    shared = make_shared(inputs)
    in_maps = [make_inputs(inputs, c, shared) for c in range(8)]
    res = run_bass_kernel_spmd(nc, in_maps, core_ids=list(range(8)))
    out = np.zeros((4, SEQ, D), np.float32)
    for c in range(8):
        b, hf = c // 2, c % 2
        out[b, hf * 2048:(hf + 1) * 2048] = res.results[c]["out"]
    return out
```

```python
import numpy as np
from contextlib import ExitStack
import concourse.bass as bass
import concourse.mybir as mybir
from concourse.bass_utils import run_bass_kernel_spmd

F32 = mybir.dt.float32
BF16 = mybir.dt.bfloat16
AF = mybir.ActivationFunctionType
ALU = mybir.AluOpType
AX = mybir.AxisListType

D = 4096
SEQ = 4096
CTX = 256
TT = SEQ + CTX
NCOLS = 12992
NCH = 102
NCOLP = NCH * 128
RW = 2048
RCOLS = 6784
EPS = 1e-6


class T:
    __slots__ = ("name", "ap", "lw", "rd", "dsem", "dcnt", "excl")

    def __init__(self, name, ap):
        self.name = name
        self.ap = ap
        self.lw = []
        self.rd = {}
        self.dsem = None
        self.dcnt = 0
        self.excl = False

    def __getitem__(self, idx):
        return self.ap[idx]


class S:
    def __init__(self, nc, stack):
        self.nc = nc
        self.stack = stack
        self.eng = {"pe": nc.tensor, "dve": nc.vector, "act": nc.scalar, "pool": nc.gpsimd, "sp": nc.sync}
        self.sems = {}
        self.cnt = {}
        self.known = {e: {} for e in self.eng}
        for e in self.eng:
            self.sems[e] = stack.enter_context(nc.semaphore("s_" + e))
            self.cnt[e] = 0
        self.semobj = dict(self.sems)
        self.all_dma_events = {}
        self.n_inst = 0
        self.uid = 0
        self.semstack = stack
        self.dpool = []
        self.dsc = {}
        self.downers = []

    def sb(self, name, shape, dt):
        t = self.stack.enter_context(self.nc.sbuf_tensor(name, list(shape), dt))
        return T(name, t)

    def ps(self, name, shape, dt=F32):
        t = self.stack.enter_context(self.nc.psum_tensor(name, list(shape), dt))
        tt = T(name, t)
        tt.excl = True
        return tt

    def dram(self, name, shape, dt, kind="Internal"):
        t = self.nc.dram_tensor(name, list(shape), dt, kind=kind)
        return T(name, t.ap())

    def _wait(self, e, evs):
        eng = self.eng[e]
        kn = self.known[e]
        best = {}
        for (k, v) in evs:
            if v > best.get(k, 0):
                best[k] = v
        for k, v in best.items():
            if kn.get(k, 0) >= v:
                continue
            eng.wait_ge(self.semobj[k], v)
            kn[k] = v

    def _deps(self, reads, writes):
        evs = []
        for t in reads:
            evs.extend(t.lw)
            if t.excl:
                evs.extend(t.rd.items())
        for t in writes:
            evs.extend(t.lw)
            evs.extend(t.rd.items())
        return evs

    def op(self, e, fn, reads=(), writes=()):
        evs = self._deps(reads, writes)
        if e == "pe":
            evs = [(k, v) for (k, v) in evs if k != "pe"]
        self._wait(e, evs)
        ins = fn(self.eng[e])
        self.cnt[e] += 1
        ins.then_inc(self.sems[e], 1)
        ev = (e, self.cnt[e])
        for t in writes:
            t.lw = [ev]
            t.rd = {}
        for t in reads:
            if t not in writes:
                t.rd[e] = self.cnt[e]
        self.n_inst += 1
        return ins

    def dma(self, q, out_ap, in_ap, reads=(), writes=(), **kw):
        evs = self._deps(reads, writes)
        self._wait(q, evs)
        wt = writes[0]
        if wt.dsem is None:
            if self.dpool:
                nm = self.dpool.pop()
            else:
                self.uid += 1
                nm = "dq%d" % self.uid
                self.semobj[nm] = self.semstack.enter_context(self.nc.semaphore(nm))
                self.dsc[nm] = 0
            wt.dsem = nm
            self.downers.append(wt)
        self.dsc[wt.dsem] += 16
        wt.dcnt = self.dsc[wt.dsem]
        ins = self.eng[q].dma_start(out=out_ap, in_=in_ap, **kw)
        ins.then_inc(self.semobj[wt.dsem], 16)
        ev = (wt.dsem, wt.dcnt)
        self.all_dma_events[wt.dsem] = wt.dcnt
        for t in writes:
            t.lw = [ev]
            t.rd = {}
        for t in reads:
            t.rd[wt.dsem] = wt.dcnt
        self.n_inst += 1
        return ins

    def barrier(self):
        evs = [(e, c) for e, c in self.cnt.items() if c > 0]
        evs += list(self.all_dma_events.items())
        for e in self.eng:
            self._wait(e, evs)
        for t in self.downers:
            self.dpool.append(t.dsem)
            t.dsem = None
        self.downers = []

    def finish(self):
        evs = [(e, c) for e, c in self.cnt.items() if c > 0]
        evs += list(self.all_dma_events.items())
        self._wait("sp", evs)
        self._wait("pool", evs)


def build_program(dbg=None):
    nc = bass.Bass("TRN2", target_bir_lowering=False)
    st = ExitStack()
    s = S(nc, st)
    skip_mixer = dbg in ("tail", "peer1", "h1t")

    def din(name, shape, dt=F32):
        return s.dram(name, shape, dt, kind="ExternalInput")

    xs = din("xs", [TT, D])
    cc = din("cc", [128, 2, 32])
    w_mod = din("w_mod", [D, 6 * D])
    b_mod = din("b_mod", [1, 6 * D])
    norm1_g = din("norm1_g", [1, D])
    w_in_r = din("w_in_r", [NCH, 128, 32 * 128])
    ident_in = din("ident_in", [128, 128])

    modx = s.dram("modx", [128, 6 * D], F32)
    modc = s.dram("modc", [128, 2 * D], F32)
    kind = "ExternalOutput" if dbg == "cols" else "Internal"
    colsT = s.dram("colsT", [NCOLP, TT], F32, kind=kind)

    pb = [s.ps("pb%d" % i, [128, 512], F32) for i in range(8)]

    ident_f = s.sb("ident_f", [128, 128], F32)
    ident = s.sb("ident", [128, 128], BF16)
    s.dma("sp", ident_f[:], ident_in[:, :], reads=[ident_in], writes=[ident_f])
    s.op("dve", lambda e: e.tensor_copy(out=ident[:], in_=ident_f[:]), reads=[ident_f], writes=[ident])

    with ExitStack() as pa:
        s.stack = pa
        cct = s.sb("cct", [128, 2, 32], F32)
        sil = s.sb("sil", [128, 2, 32], F32)
        sbx = s.sb("sbx", [128, 2, 32, 128], F32)
        s.dma("sp", cct[:], cc[:, :, :], reads=[cc], writes=[cct])
        s.op("act", lambda e: e.activation(out=sil[:], in_=cct[:], func=AF.Silu), reads=[cct], writes=[sil])
        for w in range(2):
            s.op("dve", lambda e: e.tensor_copy(
                out=sbx[:, w], in_=sil[:, w, :].unsqueeze(2).to_broadcast([128, 32, 128])),
                reads=[sil], writes=[sbx])
        wts = [s.sb("wmt%d" % i, [128, 2048], F32) for i in range(3)]
        bmt = [s.sb("bmt%d" % i, [128, 2048], F32) for i in range(2)]
        mo = [s.sb("mo%d" % i, [128, 2048], F32) for i in range(2)]
        moc = [s.sb("moc%d" % i, [128, 2048], F32) for i in range(2)]
        it = 0
        for cg in range(12):
            c0 = cg * 2048
            bt = bmt[cg % 2]
            s.dma("act", bt[:], b_mod[0:1, c0:c0 + 2048].partition_broadcast(128), reads=[b_mod], writes=[bt])
            for kc in range(32):
                wt = wts[it % 3]
                it += 1
                q = "sp" if kc % 2 == 0 else "act"
                s.dma(q, wt[:], w_mod[kc * 128:(kc + 1) * 128, c0:c0 + 2048], reads=[w_mod], writes=[wt])
                for j in range(4):
                    s.op("pe", lambda e: e.matmul(pb[j][:, :], lhsT=sbx[:, 0, kc, :], rhs=wt[:, j * 512:(j + 1) * 512],
                                                  start=(kc == 0), stop=(kc == 31)), reads=[sbx, wt], writes=[pb[j]])
                if cg < 4:
                    for j in range(4):
                        s.op("pe", lambda e: e.matmul(pb[4 + j][:, :], lhsT=sbx[:, 1, kc, :], rhs=wt[:, j * 512:(j + 1) * 512],
                                                      start=(kc == 0), stop=(kc == 31)), reads=[sbx, wt], writes=[pb[4 + j]])
            m = mo[cg % 2]
            for j in range(4):
                s.op("dve", lambda e: e.tensor_tensor(out=m[:, j * 512:(j + 1) * 512], in0=pb[j][:, :],
                                                      in1=bt[:, j * 512:(j + 1) * 512], op=ALU.add),
                     reads=[pb[j], bt], writes=[m])
            s.dma("sp", modx[:, c0:c0 + 2048], m[:], reads=[m], writes=[modx])
            if cg < 4:
                mc = moc[cg % 2]
                for j in range(4):
                    s.op("dve", lambda e: e.tensor_tensor(out=mc[:, j * 512:(j + 1) * 512], in0=pb[4 + j][:, :],
                                                          in1=bt[:, j * 512:(j + 1) * 512], op=ALU.add),
                         reads=[pb[4 + j], bt], writes=[mc])
                s.dma("sp", modc[:, c0:c0 + 2048], mc[:], reads=[mc], writes=[modc])
        s.barrier()
    s.stack = st

    with ExitStack() as pbc:
        s.stack = pbc
        G1 = s.sb("G1", [128, D], F32)
        SH1 = s.sb("SH1", [128, D], F32)
        g1b = s.sb("g1b", [128, D], F32)
        nT = s.sb("nT", [128, 32, 1024], BF16)
        xt = [s.sb("xt%d" % i, [128, D], F32) for i in range(2)]
        xn = [s.sb("xn%d" % i, [128, D], BF16) for i in range(2)]
        sq = s.sb("sqjunk", [128, D], BF16)
        ss = [s.sb("ss%d" % i, [128, 1], F32) for i in range(2)]
        rstd = [s.sb("rstd%d" % i, [128, 1], F32) for i in range(2)]
        wt = [s.sb("wit%d" % i, [128, 32, 128], BF16) for i in range(3)]
        ot = [s.sb("ot%d" % i, [128, 512], F32) for i in range(4)]
        s.dma("sp", g1b[:], norm1_g[0:1, :].partition_broadcast(128), reads=[norm1_g], writes=[g1b])

        def load_mod(src, sc_off, sh_off):
            s.dma("sp", G1[:], src[:, sc_off:sc_off + D], reads=[src], writes=[G1])
            s.dma("act", SH1[:], src[:, sh_off:sh_off + D], reads=[src], writes=[SH1])
            s.op("dve", lambda e: e.scalar_tensor_tensor(out=G1[:], in0=G1[:], scalar=1.0, in1=g1b[:],
                                                         op0=ALU.add, op1=ALU.mult), reads=[G1, g1b], writes=[G1])

        blocks = [(0, 256)] + [(256 + i * 1024, 1024) for i in range(4)]
        if skip_mixer:
            blocks = []
        ti = 0
        wi = 0
        oi = 0
        ev = 0
        for bi, (t0, tn) in enumerate(blocks):
            if bi == 0:
                load_mod(modc, D, 0)
            elif bi == 1:
                load_mod(modx, D, 0)
            for tt in range(tn // 128):
                x_ = xt[ti % 2]
                xn_ = xn[ti % 2]
                ss_ = ss[ti % 2]
                rs_ = rstd[ti % 2]
                ti += 1
                r0 = t0 + tt * 128
                s.dma("sp", x_[:], xs[r0:r0 + 128, :], reads=[xs], writes=[x_])
                s.op("act", lambda e: e.activation(out=sq[:], in_=x_[:], func=AF.Square, accum_out=ss_[:]),
                     reads=[x_], writes=[sq, ss_])
                s.op("dve", lambda e: e.tensor_scalar(out=rs_[:], in0=ss_[:], scalar1=1.0 / D, scalar2=EPS,
                                                      op0=ALU.mult, op1=ALU.add), reads=[ss_], writes=[rs_])
                s.op("act", lambda e: e.activation(out=rs_[:], in_=rs_[:], func=AF.Sqrt), reads=[rs_], writes=[rs_])
                s.op("dve", lambda e: e.reciprocal(out=rs_[:], in_=rs_[:]), reads=[rs_], writes=[rs_])
                s.op("dve", lambda e: e.scalar_tensor_tensor(out=x_[:], in0=x_[:], scalar=rs_[:, 0:1], in1=G1[:],
                                                             op0=ALU.mult, op1=ALU.mult),
                     reads=[x_, rs_, G1], writes=[x_])
                s.op("pool", lambda e: e.tensor_tensor(out=xn_[:], in0=x_[:], in1=SH1[:], op=ALU.add),
                     reads=[x_, SH1], writes=[xn_])
                for q4 in range(4):
                    pt = pb[4 + (ev % 2)]
                    ev += 1
                    ptb = pt[:, :].bitcast(BF16)
                    for j in range(8):
                        kc = q4 * 8 + j
                        s.op("pe", lambda e: e.transpose(out=ptb[:, j * 128:(j + 1) * 128],
                                                         in_=xn_[:, kc * 128:(kc + 1) * 128], identity=ident[:]),
                             reads=[xn_, ident], writes=[pt])
                    eng = "act" if q4 % 2 == 0 else "dve"
                    dst = nT[:, q4 * 8:(q4 + 1) * 8, tt * 128:(tt + 1) * 128]
                    src = ptb.rearrange("p (j t) -> p j t", j=8)
                    if eng == "act":
                        s.op("act", lambda e: e.copy(out=dst, in_=src), reads=[pt], writes=[nT])
                    else:
                        s.op("dve", lambda e: e.tensor_copy(out=dst, in_=src), reads=[pt], writes=[nT])
            ng = (tn + 511) // 512
            gw = min(tn, 512)
            for j in range(NCH):
                w_ = wt[wi % 3]
                wi += 1
                s.dma("pool", w_[:], w_in_r[j].rearrange("p (k c) -> p k c", k=32), reads=[w_in_r], writes=[w_])
                for g in range(ng):
                    pp = pb[(j % 2) * 2 + g]
                    for kc in range(32):
                        s.op("pe", lambda e: e.matmul(pp[:, 0:gw], lhsT=w_[:, kc, :], rhs=nT[:, kc, g * 512:g * 512 + gw],
                                                      start=(kc == 0), stop=(kc == 31)), reads=[w_, nT], writes=[pp])
                    o_ = ot[oi % 4]
                    if oi % 2 == 0:
                        s.op("act", lambda e: e.copy(out=o_[:, 0:gw], in_=pp[:, 0:gw]), reads=[pp], writes=[o_])
                    else:
                        s.op("dve", lambda e: e.tensor_copy(out=o_[:, 0:gw], in_=pp[:, 0:gw]), reads=[pp], writes=[o_])
                    oi += 1
                    s.dma("sp", colsT[j * 128:(j + 1) * 128, t0 + g * 512:t0 + g * 512 + gw], o_[:, 0:gw],
                          reads=[o_], writes=[colsT])
        s.barrier()
    s.stack = st


    KAPPA = float(np.exp(-0.5))
    NB = [(i * 512, min(512, TT - i * 512)) for i in range((TT + 511) // 512)]

    def shift_mix(raw, out, u, np_):
        engs = ["dve", "pool"]
        s.op("dve", lambda e: e.tensor_scalar(out=out[0:np_, :], in0=raw[0:np_, :], scalar1=omm[0:np_, u:u + 1],
                                              scalar2=None, op0=ALU.mult), reads=[raw, omm], writes=[out])

        def acc(eng, o_ap, i_ap, sc):
            s.op(eng, lambda e: e.scalar_tensor_tensor(out=o_ap, in0=i_ap, scalar=sc, in1=o_ap,
                                                       op0=ALU.mult, op1=ALU.add), reads=[raw, musl, out], writes=[out])
        acc("dve", out[0:np_, 1:CTX], raw[0:np_, 0:CTX - 1], musl[0:np_, u, 4:5])
        acc("dve", out[0:np_, 0:CTX - 1], raw[0:np_, 1:CTX], musl[0:np_, u, 5:6])
        lo = out[0:np_, CTX:TT].rearrange("p (r c) -> p r c", c=64)
        lr = raw[0:np_, CTX:TT].rearrange("p (r c) -> p r c", c=64)
        acc("dve", lo[:, :, 1:64], lr[:, :, 0:63], musl[0:np_, u, 0:1])
        acc("dve", lo[:, :, 0:63], lr[:, :, 1:64], musl[0:np_, u, 1:2])
        acc("dve", out[0:np_, CTX + 64:TT], raw[0:np_, CTX:TT - 64], musl[0:np_, u, 2:3])
        acc("dve", out[0:np_, CTX:TT - 64], raw[0:np_, CTX + 64:TT], musl[0:np_, u, 3:4])

    mu_u = din("mu_u", [128, 54])
    slotm = din("slotm", [128, 6])
    w2r = din("w2r", [96, 2, RW])
    a2r = din("a2r", [96, 2, RW])
    g2r = din("g2r", [128, 2, RW])
    chp_in = din("chp", [128, 16, 9])
    msk_in = din("msk", [128, 4, 128])
    blk_in = din("blk", [128, 128])
    sgT = s.dram("sgT", [2, RW, TT], F32)
    asigT = s.dram("asigT", [2, RW, TT], BF16)
    gT = s.dram("gT", [RW, TT], BF16)
    kind = "ExternalOutput" if dbg in ("rwkv", "rwkv1", "ssd1", "ssd") else "Internal"
    if skip_mixer:
        kind = "ExternalInput"
    yT = s.dram("yT", [D, SEQ], BF16, kind=kind)

    omm = s.sb("omm", [128, 54], F32)
    musl = s.sb("musl", [128, 54, 6], F32)
    chp = s.sb("chp_sb", [128, 16, 9], F32)
    omka = s.sb("omka", [128, 16], F32)
    omka2 = s.sb("omka2", [128, 16], F32)
    bonD = s.dram("bonD", [RW, SEQ], BF16)
    msk = s.sb("msk_sb", [128, 4, 128], BF16)
    blk = s.sb("blk_sb", [128, 128], BF16)
    with ExitStack() as pc:
        s.stack = pc
        mut = s.sb("mut", [128, 54], F32)
        slt = s.sb("slt", [128, 6], F32)
        s.dma("sp", mut[:], mu_u[:, :], reads=[mu_u], writes=[mut])
        s.dma("sp", slt[:], slotm[:, :], reads=[slotm], writes=[slt])
        s.dma("sp", chp[:], chp_in[:, :, :], reads=[chp_in], writes=[chp])
        s.dma("pool", msk[:], msk_in[:, :, :], reads=[msk_in], writes=[msk])
        s.dma("pool", blk[:], blk_in[:, :], reads=[blk_in], writes=[blk])
        s.op("dve", lambda e: e.tensor_scalar(out=omm[:], in0=mut[:], scalar1=-1.0, scalar2=1.0,
                                              op0=ALU.mult, op1=ALU.add), reads=[mut], writes=[omm])
        for k in range(6):
            s.op("dve", lambda e: e.tensor_scalar(out=musl[:, :, k], in0=mut[:], scalar1=slt[:, k:k + 1], scalar2=None,
                                                  op0=ALU.mult), reads=[mut, slt], writes=[musl])
        s.op("dve", lambda e: e.tensor_scalar(out=omka[:], in0=chp[:, :, 5], scalar1=-1.0, scalar2=1.0,
                                              op0=ALU.mult, op1=ALU.add), reads=[chp], writes=[omka])
        s.op("dve", lambda e: e.tensor_scalar(out=omka2[:], in0=chp[:, :, 5], scalar1=-2.0, scalar2=2.0,
                                              op0=ALU.mult, op1=ALU.add), reads=[chp], writes=[omka2])
        s.barrier()
    s.stack = st

    with ExitStack() as pd1:
        s.stack = pd1
        tmpA = s.sb("d1A", [128, TT], F32)
        tmpB = s.sb("d1B", [128, TT], F32)
        lr_w = [s.sb("lr_w%d" % d, [96, TT], BF16) for d in range(2)]
        lr_a = [s.sb("lr_a%d" % d, [96, TT], BF16) for d in range(2)]
        lr_g = s.sb("lr_g", [128, 2, TT], BF16)
        w2b = s.sb("w2b", [96, 2, RW], BF16)
        a2b = s.sb("a2b", [96, 2, RW], BF16)
        g2b = s.sb("g2b", [128, 2, RW], BF16)
        s.dma("pool", w2b[:], w2r[:, :, :], reads=[w2r], writes=[w2b])
        s.dma("pool", a2b[:], a2r[:, :, :], reads=[a2r], writes=[a2b])
        s.dma("pool", g2b[:], g2r[:, :, :], reads=[g2r], writes=[g2b])
        units = [(6144 + 96 * d, 96, 48 + d, lr_w[d][:, :], AF.Tanh) for d in range(2)]
        units += [(6336 + 96 * d, 96, 50 + d, lr_a[d][:, :], AF.Copy) for d in range(2)]
        units += [(6528 + 128 * i, 128, 52 + i, lr_g[:, i, :], AF.Sigmoid) for i in range(2)]
        lr_t = {id(lr_w[0][:, :]): lr_w[0]}
        lr_tiles = [lr_w[0], lr_w[1], lr_a[0], lr_a[1], lr_g, lr_g]
        if skip_mixer:
            units = []
        for ui, (row0, np_, u, dst, fn) in enumerate(units):
            s.dma("sp", tmpA[0:np_, :], colsT[row0:row0 + np_, :], reads=[colsT], writes=[tmpA])
            shift_mix(tmpA, tmpB, u, np_)
            s.op("act", lambda e: e.activation(out=dst[0:np_], in_=tmpB[0:np_, :], func=fn),
                 reads=[tmpB], writes=[lr_tiles[ui]])
        sgs = [s.sb("sgs%d" % i, [128, TT], F32) for i in range(2)]
        asg = [s.sb("asg%d" % i, [128, TT], BF16) for i in range(2)]
        gs = s.sb("gs", [128, TT], BF16)
        bk = 0
        for cj in range(0 if skip_mixer else 16):
            csl = slice(cj * 128, (cj + 1) * 128)
            for d in range(2):
                for (b0, bn) in NB:
                    pp = pb[bk % 4]; bk += 1
                    s.op("pe", lambda e: e.matmul(pp[:, 0:bn], lhsT=w2b[:, d, csl], rhs=lr_w[d][:, b0:b0 + bn],
                                                  start=True, stop=True), reads=[w2b, lr_w[d]], writes=[pp])
                    s.op("act", lambda e: e.activation(out=sgs[d][:, b0:b0 + bn], in_=pp[:, 0:bn], func=AF.Sigmoid,
                                                       bias=chp[:, cj, d:d + 1]), reads=[pp, chp], writes=[sgs[d]])
                s.dma("sp", sgT[d, csl, :], sgs[d][:], reads=[sgs[d]], writes=[sgT])
                for (b0, bn) in NB:
                    pp = pb[bk % 4]; bk += 1
                    s.op("pe", lambda e: e.matmul(pp[:, 0:bn], lhsT=a2b[:, d, csl], rhs=lr_a[d][:, b0:b0 + bn],
                                                  start=True, stop=True), reads=[a2b, lr_a[d]], writes=[pp])
                    s.op("act", lambda e: e.activation(out=asg[d][:, b0:b0 + bn], in_=pp[:, 0:bn], func=AF.Sigmoid,
                                                       bias=chp[:, cj, 2 + d:3 + d]), reads=[pp, chp], writes=[asg[d]])
                s.dma("sp", asigT[d, csl, :], asg[d][:], reads=[asg[d]], writes=[asigT])
            for (b0, bn) in NB:
                pp = pb[bk % 4]; bk += 1
                for i in range(2):
                    s.op("pe", lambda e: e.matmul(pp[:, 0:bn], lhsT=g2b[:, i, csl], rhs=lr_g[:, i, b0:b0 + bn],
                                                  start=(i == 0), stop=(i == 1)), reads=[g2b, lr_g], writes=[pp])
                s.op("dve", lambda e: e.tensor_copy(out=gs[:, b0:b0 + bn], in_=pp[:, 0:bn]), reads=[pp], writes=[gs])
            s.dma("sp", gT[csl, :], gs[:], reads=[gs], writes=[gT])
        s.barrier()
    s.stack = st

    NCK = TT // 128
    uT_r = din("uT_r", [128, 128, 32 * 128])
    peer_v = din("peer_v", [16384, D])
    uTb = s.dram("uTb", [128, 128, 32 * 128], BF16)
    vb = s.dram("vb", [16384, D], BF16)
    conv_state = {"n": 0}

    def convert_tables(n):
        for _ in range(n):
            e1 = conv_state["n"]
            if e1 >= 128:
                return
            conv_state["n"] += 1
            s.dma("pool", uTb[e1], uT_r[e1], reads=[uT_r], writes=[uTb])
            s.dma("pool", vb[e1 * 128:(e1 + 1) * 128, :], peer_v[e1 * 128:(e1 + 1) * 128, :], reads=[peer_v], writes=[vb])
    with ExitStack() as pe_:
        s.stack = pe_
        tA = s.sb("eA", [128, TT], F32)
        tB = s.sb("eB", [128, TT], F32)
        r_m = s.sb("r_m", [128, TT], BF16)
        v_m = s.sb("v_m", [128, TT], BF16)
        kkn = s.sb("kkn", [128, TT], BF16)
        k_m = s.sb("k_m", [128, TT], BF16)
        kdx = s.sb("kdx", [128, TT], BF16)
        bdx = s.sb("bdx", [128, TT], BF16)
        kdir = [kdx, kdx]
        bdir = [bdx, bdx]
        Ebuf = s.sb("Ebuf", [128, TT], BF16)
        AR = s.sb("AR", [128, 2, TT], BF16)
        BK = s.sb("BK", [128, 2, TT], BF16)
        BKhT = s.sb("BKhT", [128, 2, NCK, 128], BF16)
        BKh_ap = tB[:, :].bitcast(BF16).rearrange("p (w t) -> p w t", w=2)
        yst_ap = AR[:, 0, 0:SEQ]
        gld_ap = BK[:, 0, 0:SEQ]
        bon_ap = BK[:, 1, 0:SEQ]
        VtA = s.sb("VtA", [128, NCK, 128], BF16)
        Oacc = s.sb("Oacc", [128, 32, 128], F32)
        segm = s.sb("segm", [128, TT], BF16)
        tot = s.sb("tot", [128, NCK], F32)
        PL = s.sb("PL", [128, NCK], F32)
        STf = s.sb("STf", [128, 64], F32)
        STb = s.sb("STb", [128, 64], BF16)
        s.op("pool", lambda e: e.memset(segm[:], 1.0), writes=[segm])
        s.op("pool", lambda e: e.memset(segm[:, :].rearrange("p (c t) -> p c t", t=128)[:, :, 0:1], 0.0), writes=[segm])
        NW = 6
        w_Mb = [s.sb("w_Mb%d" % i, [128, 256], BF16) for i in range(8)]
        w_Mk = [s.sb("w_Mk%d" % i, [128, 256], BF16) for i in range(8)]
        w_AB = [[s.sb("w_AB%d_%d" % (c_, i), [128, 2, 128], BF16) for i in range(3)] for c_ in range(4)]
        w_Xc = [[s.sb("w_XC%d_%d" % (c_, i), [128, 128], BF16) for i in range(3)] for c_ in range(4)]
        w_XF = [s.sb("w_XF%d" % i, [128, 128], BF16) for i in range(8)]
        w_R = [s.sb("w_R%d" % i, [128, 128], BF16) for i in range(2)]
        w_U = [s.sb("w_U%d" % i, [128, 128], BF16) for i in range(2)]
        osb = [s.sb("osb%d" % i, [128, 128], F32) for i in range(2)]
        onb = [s.sb("onb%d" % i, [128, 128], BF16) for i in range(2)]
        stat = [s.sb("stat%d" % i, [128, 8], F32) for i in range(2)]
        cnt = {"bank": 0, "M": 0, "R": 0, "ev": 0}
        for c_ in range(4):
            cnt["AB%d" % c_] = 0
            cnt["XC%d" % c_] = 0

        def nbank():
            cnt["bank"] += 1
            return pb[cnt["bank"] % 7]

        def rot(lst, key):
            cnt[key] += 1
            return lst[cnt[key] % len(lst)]

        def evac_copy(dst_t, dst_ap, src_t, src_ap):
            cnt["ev"] += 1
            if cnt["ev"] % 2 == 0:
                s.op("act", lambda e: e.copy(out=dst_ap, in_=src_ap), reads=[src_t], writes=[dst_t])
            else:
                s.op("dve", lambda e: e.tensor_copy(out=dst_ap, in_=src_ap), reads=[src_t], writes=[dst_t])

        ptr = pb[7]
        ptrb = ptr[:, :].bitcast(BF16)

        for hp in range(16):
            if dbg in ('ssd1', 'ssd') or skip_mixer:
                break
            csl = slice(hp * 128, (hp + 1) * 128)
            convert_tables(8)
            for which, dst in ((0, r_m), (2, v_m), (1, k_m)):
                row0 = which * RW + hp * 128
                s.dma("sp", tA[:], colsT[row0:row0 + 128, :], reads=[colsT], writes=[tA])
                shift_mix(tA, tB, which * 16 + hp, 128)
                if dst is not None:
                    s.op("act", lambda e: e.copy(out=dst[:], in_=tB[:]), reads=[tB], writes=[dst])
            s.op("dve", lambda e: e.tensor_scalar(out=tA[:], in0=tB[:], scalar1=chp[:, hp, 4:5], scalar2=None,
                                                  op0=ALU.mult), reads=[tB, chp], writes=[tA])
            s.op("act", lambda e: e.activation(out=Ebuf[:], in_=tA[:], func=AF.Square), reads=[tA], writes=[Ebuf])
            for (b0, bn) in NB:
                pp = nbank()
                s.op("pe", lambda e: e.matmul(pp[:, 0:bn], lhsT=blk[:], rhs=Ebuf[:, b0:b0 + bn], start=True, stop=True),
                     reads=[blk, Ebuf], writes=[pp])
                s.op("act", lambda e: e.activation(out=tB[:, b0:b0 + bn], in_=pp[:, 0:bn], func=AF.Sqrt),
                     reads=[pp], writes=[tB])
            s.op("dve", lambda e: e.tensor_scalar(out=tB[:], in0=tB[:], scalar1=1e-12, scalar2=None,
                                                  op0=ALU.max), reads=[tB], writes=[tB])
            s.op("dve", lambda e: e.reciprocal(out=tB[:], in_=tB[:]), reads=[tB], writes=[tB])
            s.op("dve", lambda e: e.tensor_tensor(out=kkn[:], in0=tA[:], in1=tB[:], op=ALU.mult),
                 reads=[tA, tB], writes=[kkn])
            s.dma("sp", bdx[:], asigT[0, csl, :], reads=[asigT], writes=[bdx])
            s.dma("sp", kdx[:], asigT[1, csl, :], reads=[asigT], writes=[kdx])
            s.op("pool", lambda e: e.tensor_tensor(out=Ebuf[:], in0=bdx[:], in1=kdx[:], op=ALU.add),
                 reads=[bdx, kdx], writes=[Ebuf])
            s.op("dve", lambda e: e.tensor_scalar(out=Ebuf[:], in0=Ebuf[:], scalar1=chp[:, hp, 5:6], scalar2=omka2[:, hp:hp + 1],
                                                  op0=ALU.mult, op1=ALU.add), reads=[Ebuf, chp, omka2], writes=[Ebuf])
            s.op("dve", lambda e: e.tensor_tensor(out=Ebuf[:], in0=Ebuf[:], in1=k_m[:], op=ALU.mult),
                 reads=[Ebuf, k_m], writes=[Ebuf])
            s.op("dve", lambda e: e.scalar_tensor_tensor(out=Ebuf[:], in0=r_m[:], scalar=chp[:, hp, 6:7], in1=Ebuf[:],
                                                         op0=ALU.mult, op1=ALU.mult), reads=[r_m, chp, Ebuf], writes=[Ebuf])
            for (b0, bn) in [(CTX + i * 512, 512) for i in range(8)]:
                pp = nbank()
                s.op("pe", lambda e: e.matmul(pp[:, 0:bn], lhsT=blk[:], rhs=Ebuf[:, b0:b0 + bn], start=True, stop=True),
                     reads=[blk, Ebuf], writes=[pp])
                s.op("dve", lambda e: e.tensor_tensor(out=yst_ap[:, b0 - CTX:b0 - CTX + bn], in0=pp[:, 0:bn],
                                                      in1=v_m[:, b0:b0 + bn], op=ALU.mult), reads=[pp, v_m], writes=[AR])
            s.dma("sp", bonD[csl, :], yst_ap, reads=[AR], writes=[bonD])
            for q in range((NCK + 7) // 8):
                n8 = min(8, NCK - q * 8)
                for j in range(n8):
                    ci = q * 8 + j
                    s.op("pe", lambda e: e.transpose(out=ptrb[:, j * 128:(j + 1) * 128], in_=v_m[:, ci * 128:(ci + 1) * 128],
                                                     identity=ident[:]), reads=[v_m, ident], writes=[ptr])
                evac_copy(VtA, VtA[:, q * 8:q * 8 + n8, :], ptr, ptrb[:, 0:n8 * 128].rearrange("p (j t) -> p j t", j=n8))

            for d in range(2):
                s.dma("sp", bdx[:], asigT[d, csl, :], reads=[asigT], writes=[bdx])
                s.op("dve", lambda e: e.tensor_scalar(out=kdx[:], in0=bdx[:], scalar1=chp[:, hp, 5:6],
                                                      scalar2=omka[:, hp:hp + 1], op0=ALU.mult, op1=ALU.add),
                     reads=[bdx, chp, omka], writes=[kdx])
                s.op("dve", lambda e: e.tensor_tensor(out=kdx[:], in0=kdx[:], in1=k_m[:], op=ALU.mult),
                     reads=[kdx, k_m], writes=[kdx])
                s.op("pool", lambda e: e.tensor_tensor(out=bdx[:], in0=bdx[:], in1=kkn[:], op=ALU.mult),
                     reads=[bdx, kkn], writes=[bdx])
                s.dma("sp", tA[:], sgT[d, csl, :], reads=[sgT], writes=[tA])
                s.op("dve", lambda e: e.tensor_tensor_scan(out=tB[:], data0=segm[:], data1=tA[:], initial=0.0,
                                                           op0=ALU.mult, op1=ALU.add), reads=[segm, tA], writes=[tB])
                tB3 = tB[:, :].rearrange("p (c t) -> p c t", t=128)
                tA3 = tA[:, :].rearrange("p (c t) -> p c t", t=128)
                s.op("dve", lambda e: e.tensor_copy(out=tot[:], in_=tB3[:, :, 127]), reads=[tB], writes=[tot])
                if d == 1:
                    s.op("dve", lambda e: e.tensor_tensor(out=tB[:], in0=tA[:], in1=tB[:], op=ALU.subtract),
                         reads=[tA, tB], writes=[tB])
                    s.op("dve", lambda e: e.tensor_tensor(out=tB3, in0=tB3, in1=tot[:, :].unsqueeze(2).to_broadcast([128, NCK, 128]),
                                                          op=ALU.add), reads=[tB, tot], writes=[tB])
                s.op("act", lambda e: e.activation(out=PL[:], in_=tot[:], func=AF.Exp, scale=-KAPPA), reads=[tot], writes=[PL])
                s.op("act", lambda e: e.activation(out=Ebuf[:], in_=tB[:], func=AF.Exp, scale=-KAPPA), reads=[tB], writes=[Ebuf])
                s.op("dve", lambda e: e.tensor_tensor(out=AR[:, 1, :], in0=r_m[:], in1=Ebuf[:], op=ALU.mult),
                     reads=[r_m, Ebuf], writes=[AR])
                s.op("act", lambda e: e.activation(out=Ebuf[:], in_=tB[:], func=AF.Exp, scale=KAPPA), reads=[tB], writes=[Ebuf])
                s.op("dve", lambda e: e.tensor_tensor(out=BK[:, 0, :], in0=bdir[d][:], in1=Ebuf[:], op=ALU.mult),
                     reads=[bdir[d], Ebuf], writes=[BK])
                s.op("pool", lambda e: e.tensor_tensor(out=BK[:, 1, :], in0=kdir[d][:], in1=Ebuf[:], op=ALU.mult),
                     reads=[kdir[d], Ebuf], writes=[BK])
                s.op("dve", lambda e: e.tensor_tensor(out=tA[:], in0=tB[:], in1=tA[:], op=ALU.subtract),
                     reads=[tA, tB], writes=[tA])
                s.op("act", lambda e: e.activation(out=Ebuf[:], in_=tA[:], func=AF.Exp, scale=-KAPPA), reads=[tA], writes=[Ebuf])
                s.op("dve", lambda e: e.scalar_tensor_tensor(out=AR[:, 0, :], in0=kkn[:], scalar=-1.0, in1=Ebuf[:],
                                                             op0=ALU.mult, op1=ALU.mult), reads=[kkn, Ebuf], writes=[AR])
                s.op("dve", lambda e: e.tensor_tensor(out=tA3, in0=tot[:, :].unsqueeze(2).to_broadcast([128, NCK, 128]),
                                                      in1=tB3, op=ALU.subtract), reads=[tB, tot], writes=[tA])
                s.op("act", lambda e: e.activation(out=Ebuf[:], in_=tA[:], func=AF.Exp, scale=-KAPPA), reads=[tA], writes=[Ebuf])
                s.op("dve", lambda e: e.tensor_tensor(out=BKh_ap[:, 0, :], in0=bdir[d][:], in1=Ebuf[:], op=ALU.mult),
                     reads=[bdir[d], Ebuf], writes=[tB])
                s.op("pool", lambda e: e.tensor_tensor(out=BKh_ap[:, 1, :], in0=kdir[d][:], in1=Ebuf[:], op=ALU.mult),
                     reads=[kdir[d], Ebuf], writes=[tB])
                for w in range(2):
                    for q in range((NCK + 7) // 8):
                        n8 = min(8, NCK - q * 8)
                        for j in range(n8):
                            ci = q * 8 + j
                            s.op("pe", lambda e: e.transpose(out=ptrb[:, j * 128:(j + 1) * 128],
                                                             in_=BKh_ap[:, w, ci * 128:(ci + 1) * 128], identity=ident[:]),
                                 reads=[tB, ident], writes=[ptr])
                        evac_copy(BKhT, BKhT[:, w, q * 8:q * 8 + n8, :], ptr,
                                  ptrb[:, 0:n8 * 128].rearrange("p (j t) -> p j t", j=n8))
                s.op("pool", lambda e: e.memset(STf[:], 0.0), writes=[STf])
                s.op("pool", lambda e: e.memset(STb[:], 0.0), writes=[STb])
                order = list(range(NCK)) if d == 0 else [1, 0] + list(range(NCK - 1, 1, -1))
                mbk = msk[:, 0:2, :] if d == 0 else msk[:, 2:4, :]
                mn = msk[:, 2, :] if d == 0 else msk[:, 0, :]
                def inv_multi(items):
                    ch = []
                    for ii, (ci, slot) in enumerate(items):
                      tsl = slice(ci * 128, (ci + 1) * 128)
                      if True:
                        for hh in range(2):
                            hs = slice(hh * 64, (hh + 1) * 64)
                            pAC, pB = nbank(), nbank()
                            s.op("pe", lambda e: e.matmul(pAC[:, 0:256].rearrange("p (a t) -> p a t", a=2), lhsT=BK[hs, 0, tsl], rhs=AR[hs, :, tsl], start=True, stop=True),
                                 reads=[BK, AR], writes=[pAC])
                            s.op("pe", lambda e: e.matmul(pAC[:, 256:384], lhsT=AR[hs, 0, tsl], rhs=BK[hs, 0, tsl], start=True, stop=True),
                                 reads=[BK, AR], writes=[pAC])
                            s.op("pe", lambda e: e.matmul(pB[:, 0:256].rearrange("p (a t) -> p a t", a=2), lhsT=BK[hs, 1, tsl], rhs=AR[hs, :, tsl], start=True, stop=True),
                                 reads=[BK, AR], writes=[pB])
                            ch.append({"slot": slot, "hh": hh, "pAC": pAC, "pB": pB, "cid": ii * 2 + hh})
                      for c in ch[-2:]:
                        cid = c["cid"]
                        Mb = w_Mb[c["slot"] * 2 + c["hh"]]
                        Mk = w_Mk[c["slot"] * 2 + c["hh"]]
                        pAC, pB = c["pAC"], c["pB"]
                        AB = rot(w_AB[cid], "AB%d" % cid)
                        s.op("dve", lambda e: e.tensor_tensor(out=Mb[:, :].rearrange("p (a t) -> p a t", a=2),
                                                              in0=pAC[:, 0:256].rearrange("p (a t) -> p a t", a=2),
                                                              in1=mbk, op=ALU.mult), reads=[pAC, msk], writes=[Mb])
                        s.op("dve", lambda e: e.tensor_tensor(out=AB[:, 0, :], in0=pAC[:, 256:384], in1=mn, op=ALU.mult),
                             reads=[pAC, msk], writes=[AB])
                        s.op("dve", lambda e: e.tensor_tensor(out=Mk[:, :].rearrange("p (a t) -> p a t", a=2),
                                                              in0=pB[:, 0:256].rearrange("p (a t) -> p a t", a=2),
                                                              in1=mbk, op=ALU.mult), reads=[pB, msk], writes=[Mk])
                        s.op("pool", lambda e: e.tensor_copy(out=AB[:, 1, :], in_=Mb[:, 0:128]), reads=[Mb], writes=[AB])
                        X = rot(w_Xc[cid], "XC%d" % cid)
                        s.op("pool", lambda e: e.tensor_tensor(out=X[:], in0=Mb[:, 0:128], in1=ident[:], op=ALU.add),
                             reads=[Mb, ident], writes=[X])
                        c["AB"], c["X"], c["Mb"], c["Mk"] = AB, X, Mb, Mk
                    for k in range(7):
                        for c in ch:
                            AB, X = c["AB"], c["X"]
                            pk = nbank()
                            if k >= 1:
                                s.op("pe", lambda e: e.matmul(pk[:, 0:128], lhsT=AB[:, 0, :], rhs=X[:], start=True, stop=True),
                                     reads=[AB, X], writes=[pk])
                            if k < 6:
                                s.op("pe", lambda e: e.matmul(pk[:, 128:256], lhsT=AB[:, 1, :], rhs=AB[:, 0, :], start=True, stop=True),
                                     reads=[AB], writes=[pk])
                                s.op("pe", lambda e: e.matmul(pk[:, 256:384], lhsT=AB[:, 0, :], rhs=AB[:, 1, :], start=True, stop=True),
                                     reads=[AB], writes=[pk])
                            c["pk"] = pk
                        for c in ch:
                            cid = c["cid"]
                            pk = c["pk"]
                            if k < 6:
                                ABn = rot(w_AB[cid], "AB%d" % cid)
                                s.op("act", lambda e: e.copy(out=ABn[:, :, :], in_=pk[:, 128:384].rearrange("p (a t) -> p a t", a=2)),
                                     reads=[pk], writes=[ABn])
                            if k >= 1:
                                X = c["X"]
                                Xn = rot(w_Xc[cid], "XC%d" % cid) if k < 6 else w_XF[c["slot"] * 2 + c["hh"]]
                                s.op("dve", lambda e: e.tensor_tensor(out=Xn[:], in0=pk[:, 0:128], in1=X[:], op=ALU.add),
                                     reads=[pk, X], writes=[Xn])
                                c["X"] = Xn
                            if k < 6:
                                c["AB"] = ABn
                    res = {}
                    for c in ch:
                        r_ = res.setdefault(c["slot"], ([None, None], [None, None], [None, None]))
                        r_[0][c["hh"]] = c["X"]; r_[1][c["hh"]] = c["Mb"]; r_[2][c["hh"]] = c["Mk"]
                    return res

                def state_part(ci, par, inv):
                    Xh, Mbh, Mkh = inv
                    tsl = slice(ci * 128, (ci + 1) * 128)
                    pU = nbank()
                    for hh in range(2):
                        hs = slice(hh * 64, (hh + 1) * 64)
                        s.op("pe", lambda e: e.matmul(pU[:, hs], lhsT=AR[hs, 0, tsl], rhs=STb[hs, :], start=True, stop=False),
                             reads=[AR, STb], writes=[pU])
                        s.op("pe", lambda e: e.matmul(pU[:, hs], lhsT=Mkh[hh][:, 0:128], rhs=VtA[:, ci, hs], start=False, stop=True),
                             reads=[Mkh[hh], VtA], writes=[pU])
                    Rr = w_R[par]
                    evac_copy(Rr, Rr[:], pU, pU[:, 0:128])
                    pU2 = nbank()
                    for hh in range(2):
                        hs = slice(hh * 64, (hh + 1) * 64)
                        s.op("pe", lambda e: e.matmul(pU2[:, hs], lhsT=Xh[hh][:], rhs=Rr[:, hs], start=True, stop=True),
                             reads=[Xh[hh], Rr], writes=[pU2])
                    Ub = w_U[par]
                    evac_copy(Ub, Ub[:], pU2, pU2[:, 0:128])
                    pS = nbank()
                    for hh in range(2):
                        hs = slice(hh * 64, (hh + 1) * 64)
                        s.op("pe", lambda e: e.matmul(pS[hs, 0:64], lhsT=BKhT[:, 0, ci, hs], rhs=Ub[:, hs], start=True, stop=False),
                             reads=[BKhT, Ub], writes=[pS])
                        s.op("pe", lambda e: e.matmul(pS[hs, 0:64], lhsT=BKhT[:, 1, ci, hs], rhs=VtA[:, ci, hs], start=False, stop=True),
                             reads=[BKhT, VtA], writes=[pS])
                    pO = None
                    if ci >= 2:
                        pO = nbank()
                        for hh in range(2):
                            hs = slice(hh * 64, (hh + 1) * 64)
                            s.op("pe", lambda e: e.matmul(pO[:, hs], lhsT=AR[hs, 1, tsl], rhs=STb[hs, :], start=True, stop=False),
                                 reads=[AR, STb], writes=[pO])
                            s.op("pe", lambda e: e.matmul(pO[:, hs], lhsT=Mkh[hh][:, 128:256], rhs=VtA[:, ci, hs], start=False, stop=False),
                                 reads=[Mkh[hh], VtA], writes=[pO])
                            s.op("pe", lambda e: e.matmul(pO[:, hs], lhsT=Mbh[hh][:, 128:256], rhs=Ub[:, hs], start=False, stop=True),
                                 reads=[Mbh[hh], Ub], writes=[pO])
                    s.op("dve", lambda e: e.scalar_tensor_tensor(out=STf[:], in0=STf[:], scalar=PL[:, ci:ci + 1], in1=pS[:, 0:64],
                                                                 op0=ALU.mult, op1=ALU.add), reads=[STf, PL, pS], writes=[STf])
                    s.op("act", lambda e: e.copy(out=STb[:], in_=STf[:]), reads=[STf], writes=[STb])
                    if ci >= 2:
                        if d == 0:
                            evac_copy(Oacc, Oacc[:, ci - 2, :], pO, pO[:, 0:128])
                        else:
                            s.op("dve", lambda e: e.tensor_tensor(out=Oacc[:, ci - 2, :], in0=pO[:, 0:128], in1=Oacc[:, ci - 2, :],
                                                                  op=ALU.add), reads=[pO, Oacc], writes=[Oacc])

                pairs = [order[i:i + 2] for i in range(0, len(order), 2)]
                prev = inv_multi([(c_, i_) for i_, c_ in enumerate(pairs[0])])
                for j, pr in enumerate(pairs):
                    nxt = None
                    if j + 1 < len(pairs):
                        nxt = inv_multi([(c_, ((j + 1) % 2) * 2 + i_) for i_, c_ in enumerate(pairs[j + 1])])
                    for i_, ci in enumerate(pr):
                        state_part(ci, i_, prev[(j % 2) * 2 + i_])
                    prev = nxt
            s.dma("sp", gld_ap, gT[csl, CTX:TT], reads=[gT], writes=[BK])
            s.dma("act", bon_ap, bonD[csl, :], reads=[bonD], writes=[BK])
            for lc in range(32):
                ob = osb[lc % 2]
                on = onb[lc % 2]
                stt = stat[lc % 2]
                for hh in range(2):
                    hs = slice(hh * 64, (hh + 1) * 64)
                    s.op("dve", lambda e: e.tensor_reduce(out=stt[:, hh:hh + 1], in_=Oacc[:, lc, hs], axis=AX.X, op=ALU.add),
                         reads=[Oacc], writes=[stt])
                    s.op("dve", lambda e: e.tensor_scalar(out=stt[:, 2 + hh:3 + hh], in0=stt[:, hh:hh + 1], scalar1=-1.0 / 64,
                                                          scalar2=None, op0=ALU.mult), reads=[stt], writes=[stt])
                    s.op("dve", lambda e: e.tensor_scalar(out=ob[:, hs], in0=Oacc[:, lc, hs], scalar1=stt[:, 2 + hh:3 + hh],
                                                          scalar2=None, op0=ALU.add), reads=[Oacc, stt], writes=[ob])
                    s.op("act", lambda e: e.activation(out=on[:, hs], in_=ob[:, hs], func=AF.Square,
                                                       accum_out=stt[:, 4 + hh:5 + hh]), reads=[ob], writes=[on, stt])
                    s.op("dve", lambda e: e.tensor_scalar(out=stt[:, 6 + hh:7 + hh], in0=stt[:, 4 + hh:5 + hh], scalar1=1.0 / 64,
                                                          scalar2=64e-5, op0=ALU.mult, op1=ALU.add), reads=[stt], writes=[stt])
                    s.op("act", lambda e: e.activation(out=stt[:, 6 + hh:7 + hh], in_=stt[:, 6 + hh:7 + hh], func=AF.Sqrt),
                         reads=[stt], writes=[stt])
                    s.op("dve", lambda e: e.reciprocal(out=stt[:, 6 + hh:7 + hh], in_=stt[:, 6 + hh:7 + hh]), reads=[stt], writes=[stt])
                    s.op("dve", lambda e: e.tensor_scalar(out=on[:, hs], in0=ob[:, hs], scalar1=stt[:, 6 + hh:7 + hh],
                                                          scalar2=None, op0=ALU.mult), reads=[ob, stt], writes=[on])
                s.op("pe", lambda e: e.transpose(out=ptrb[:, 0:128], in_=on[:], identity=ident[:]), reads=[on, ident], writes=[ptr])
                tl = slice(lc * 128, (lc + 1) * 128)
                s.op("act", lambda e: e.activation(out=yst_ap[:, tl], in_=ptrb[:, 0:128], func=AF.Identity,
                                                   scale=chp[:, hp, 7:8], bias=chp[:, hp, 8:9]), reads=[ptr, chp], writes=[AR])
            s.op("dve", lambda e: e.tensor_tensor(out=yst_ap, in0=yst_ap, in1=bon_ap, op=ALU.add), reads=[AR, BK], writes=[AR])
            s.op("dve", lambda e: e.tensor_tensor(out=yst_ap, in0=yst_ap, in1=gld_ap, op=ALU.mult), reads=[AR, BK], writes=[AR])
            s.dma("sp", yT[csl, :], yst_ap, reads=[AR], writes=[yT])
            if dbg == "rwkv1":
                break
        s.barrier()
    s.stack = st


    cw_in = din("cw", [128, 32, 4])
    dtp_in = din("dtp", [64, 2])
    dbc_in = din("dbc", [1, RW])
    nwb_in = din("nwb", [1, RW])
    ZR0, XR0, BR0, CR0, DTR0 = RCOLS, RCOLS + 2048, RCOLS + 4096, RCOLS + 5120, RCOLS + 6144
    with ExitStack() as pg:
        s.stack = pg
        cw = s.sb("cw_sb", [128, 32, 4], F32)
        dtp = s.sb("dtp_sb", [64, 2], F32)
        Aneg = s.sb("Aneg", [64, 1], F32)
        dbc = s.sb("dbc_sb", [128, 256], F32)
        nwb = s.sb("nwb_sb", [128, 256], F32)
        gA = s.sb("gA", [128, TT], F32)
        gB = s.sb("gB", [128, TT], F32)
        cumF = s.sb("cumF", [64, TT], F32)
        totF = s.sb("totF", [64, NCK], F32)
        dtTok = s.sb("dtTok", [128, NCK, 64], F32)
        cumTok = s.sb("cumTok", [128, NCK, 64], F32)
        ehTok = s.sb("ehTok", [128, NCK, 64], F32)
        xg = s.sb("xg", [128, 2, TT], BF16)
        Bg = s.sb("Bg", [128, TT], BF16)
        Cg = s.sb("Cg", [128, TT], BF16)
        XTok = s.sb("XTok", [128, NCK, 256], BF16)
        BTok = s.sb("BTok", [128, NCK, 128], BF16)
        dtF = T("dtF_alias_xg", xg[:, :, :].rearrange("p a t -> p (a t)").bitcast(F32)[0:64, :])
        ehF = T("ehF_alias_XTok", XTok[:, :, :].rearrange("p a t -> p (a t)").bitcast(F32)[0:64, :])
        yacc = s.sb("yacc", [128, 32, 256], F32)
        SSf = s.sb("SSf", [128, 256], F32)
        SSb = s.sb("SSb", [128, 256], BF16)
        cbm = [s.sb("cbm%d" % i, [128, 128], BF16) for i in range(2)]
        NWS = 8
        w_d = [s.sb("wd%d" % i, [128, 128], F32) for i in range(NWS)]
        w_e = [s.sb("we%d" % i, [128, 128], BF16) for i in range(NWS)]
        w_eb = [s.sb("web%d" % i, [128, 128], F32) for i in range(NWS)]
        w_dcb = [s.sb("wdcb%d" % i, [128, 128], BF16) for i in range(NWS)]
        w_ct = [s.sb("wct%d" % i, [128, 128], BF16) for i in range(NWS)]
        w_bh = [s.sb("wbh%d" % i, [128, 128], BF16) for i in range(NWS)]
        fy = [s.sb("fy%d" % i, [128, 256], F32) for i in range(2)]
        fz = [s.sb("fz%d" % i, [128, 256], F32) for i in range(2)]
        fo = [s.sb("fo%d" % i, [128, 256], BF16) for i in range(2)]
        fsq = s.sb("fsq", [128, 256], BF16)
        fst = [s.sb("fst%d" % i, [128, 2], F32) for i in range(2)]
        gc = {"bank": 0, "w": 0, "ev": 0}

        def gbank():
            gc["bank"] += 1
            return pb[gc["bank"] % 7]

        def gev(dst_t, dst_ap, src_t, src_ap):
            gc["ev"] += 1
            if gc["ev"] % 2 == 0:
                s.op("act", lambda e: e.copy(out=dst_ap, in_=src_ap), reads=[src_t], writes=[dst_t])
            else:
                s.op("dve", lambda e: e.tensor_copy(out=dst_ap, in_=src_ap), reads=[src_t], writes=[dst_t])

        ptr = pb[7]
        ptrb = ptr[:, :].bitcast(BF16)
        s.dma("sp", cw[:], cw_in[:, :, :], reads=[cw_in], writes=[cw])
        s.dma("sp", dtp[:], dtp_in[:, :], reads=[dtp_in], writes=[dtp])
        s.op("act", lambda e: e.activation(out=Aneg[:], in_=dtp[:, 1:2], func=AF.Exp), reads=[dtp], writes=[Aneg])
        s.op("dve", lambda e: e.tensor_scalar(out=Aneg[:], in0=Aneg[:], scalar1=-1.0, scalar2=None, op0=ALU.mult),
             reads=[Aneg], writes=[Aneg])
        s.dma("sp", gA[0:64, :], colsT[DTR0:DTR0 + 64, :], reads=[colsT], writes=[gA])
        s.op("act", lambda e: e.activation(out=gB[0:64, :], in_=gA[0:64, :], func=AF.Exp, bias=dtp[:, 0:1]),
             reads=[gA, dtp], writes=[gB])
        s.op("act", lambda e: e.activation(out=dtF[:], in_=gB[0:64, :], func=AF.Ln, bias=1.0), reads=[gB], writes=[dtF])
        s.op("dve", lambda e: e.tensor_scalar(out=gA[0:64, :], in0=dtF[:], scalar1=Aneg[:, 0:1], scalar2=None, op0=ALU.mult),
             reads=[dtF, Aneg], writes=[gA])
        s.op("pool", lambda e: e.memset(gB[0:64, :], 1.0), reads=[], writes=[gB])
        s.op("pool", lambda e: e.memset(gB[0:64, :].rearrange("p (c t) -> p c t", t=128)[:, :, 0:1], 0.0), writes=[gB])
        s.op("dve", lambda e: e.tensor_tensor_scan(out=cumF[:], data0=gB[0:64, :], data1=gA[0:64, :], initial=0.0,
                                                   op0=ALU.mult, op1=ALU.add), reads=[gB, gA], writes=[cumF])
        cum3 = cumF[:, :].rearrange("p (c t) -> p c t", t=128)
        s.op("dve", lambda e: e.tensor_copy(out=totF[:], in_=cum3[:, :, 127]), reads=[cumF], writes=[totF])
        s.op("dve", lambda e: e.tensor_tensor(out=cumF[32:64, :], in0=gA[32:64, :], in1=cumF[32:64, :], op=ALU.subtract),
             reads=[gA, cumF], writes=[cumF])
        s.op("dve", lambda e: e.tensor_tensor(out=cum3[32:64], in0=cum3[32:64],
                                              in1=totF[32:64, :].unsqueeze(2).to_broadcast([32, NCK, 128]), op=ALU.add),
             reads=[cumF, totF], writes=[cumF])
        eh3 = ehF[:, :].rearrange("p (c t) -> p c t", t=128)
        s.op("dve", lambda e: e.tensor_tensor(out=eh3, in0=totF[:, :].unsqueeze(2).to_broadcast([64, NCK, 128]), in1=cum3,
                                              op=ALU.subtract), reads=[cumF, totF], writes=[ehF])
        s.op("act", lambda e: e.activation(out=ehF[:], in_=ehF[:], func=AF.Exp), reads=[ehF], writes=[ehF])
        s.op("dve", lambda e: e.tensor_tensor(out=ehF[:], in0=ehF[:], in1=dtF[:], op=ALU.mult), reads=[ehF, dtF], writes=[ehF])
        for srcF, dstT in ((dtF, dtTok), (cumF, cumTok), (ehF, ehTok)):
            for q in range((NCK + 7) // 8):
                n8 = min(8, NCK - q * 8)
                pp = gbank()
                for j in range(n8):
                    ci = q * 8 + j
                    s.op("pe", lambda e: e.transpose(out=pp[:, j * 64:(j + 1) * 64], in_=srcF[:, ci * 128:(ci + 1) * 128],
                                                     identity=ident_f[0:64, 0:64]), reads=[srcF, ident_f], writes=[pp])
                gev(dstT, dstT[:, q * 8:q * 8 + n8, :], pp, pp[:, 0:n8 * 64].rearrange("p (j t) -> p j t", j=n8))

        s.barrier()
        ysg_ap = xg[:, :, 0:SEQ]
        zg_ap = gA[:, :].bitcast(BF16)[:, 0:2 * SEQ].rearrange("p (i t) -> p i t", i=2)

        def conv_silu(row0, ch, dst_t, dst_ap):
            s.dma("sp", gA[:], colsT[row0:row0 + 128, :], reads=[colsT], writes=[gA])
            s.op("dve", lambda e: e.tensor_scalar(out=gB[:], in0=gA[:], scalar1=cw[:, ch, 1:2], scalar2=cw[:, ch, 3:4],
                                                  op0=ALU.mult, op1=ALU.add), reads=[gA, cw], writes=[gB])
            for (a, b) in ((0, CTX), (CTX, TT)):
                s.op("dve", lambda e: e.scalar_tensor_tensor(out=gB[:, a + 1:b], in0=gA[:, a:b - 1], scalar=cw[:, ch, 0:1],
                                                             in1=gB[:, a + 1:b], op0=ALU.mult, op1=ALU.add),
                     reads=[gA, cw, gB], writes=[gB])
                s.op("dve", lambda e: e.scalar_tensor_tensor(out=gB[:, a:b - 1], in0=gA[:, a + 1:b], scalar=cw[:, ch, 2:3],
                                                             in1=gB[:, a:b - 1], op0=ALU.mult, op1=ALU.add),
                     reads=[gA, cw, gB], writes=[gB])
            s.op("act", lambda e: e.activation(out=dst_ap, in_=gB[:], func=AF.Silu), reads=[gB], writes=[dst_t])

        for g in range(0 if skip_mixer else 8):
            for i in range(2):
                conv_silu(XR0 + g * 256 + i * 128, g * 2 + i, xg, xg[:, i, :])
            conv_silu(BR0 + g * 128, 16 + g, Bg, Bg[:, :])
            conv_silu(CR0 + g * 128, 24 + g, Cg, Cg[:, :])
            for q in range((NCK + 3) // 4):
                n4 = min(4, NCK - q * 4)
                for j in range(n4):
                    ci = q * 4 + j
                    for i in range(2):
                        s.op("pe", lambda e: e.transpose(out=ptrb[:, (j * 2 + i) * 128:(j * 2 + i + 1) * 128],
                                                         in_=xg[:, i, ci * 128:(ci + 1) * 128], identity=ident[:]),
                             reads=[xg, ident], writes=[ptr])
                gev(XTok, XTok[:, q * 4:q * 4 + n4, :], ptr, ptrb[:, 0:n4 * 256].rearrange("p (j t) -> p j t", j=n4))
            for q in range((NCK + 7) // 8):
                n8 = min(8, NCK - q * 8)
                for j in range(n8):
                    ci = q * 8 + j
                    s.op("pe", lambda e: e.transpose(out=ptrb[:, j * 128:(j + 1) * 128], in_=Bg[:, ci * 128:(ci + 1) * 128],
                                                     identity=ident[:]), reads=[Bg, ident], writes=[ptr])
                gev(BTok, BTok[:, q * 8:q * 8 + n8, :], ptr, ptrb[:, 0:n8 * 128].rearrange("p (j t) -> p j t", j=n8))
            for d in range(2):
                s.op("pool", lambda e: e.memset(SSf[:], 0.0), writes=[SSf])
                s.op("pool", lambda e: e.memset(SSb[:], 0.0), writes=[SSb])
                order = list(range(NCK)) if d == 0 else [1, 0] + list(range(NCK - 1, 1, -1))
                mcb = msk[:, 1, :] if d == 0 else msk[:, 3, :]
                for ci in order:
                    tsl = slice(ci * 128, (ci + 1) * 128)
                    pcb = gbank()
                    s.op("pe", lambda e: e.matmul(pcb[:, 0:128], lhsT=Bg[:, tsl], rhs=Cg[:, tsl], start=True, stop=True),
                         reads=[Bg, Cg], writes=[pcb])
                    cb = cbm[(gc["w"] // 4) % 2]
                    s.op("dve", lambda e: e.tensor_tensor(out=cb[:], in0=pcb[:, 0:128], in1=mcb, op=ALU.mult),
                         reads=[pcb, msk], writes=[cb])
                    pbcs = []
                    for hh in range(4):
                        dh = d * 32 + g * 4 + hh
                        pbc = gbank()
                        s.op("pe", lambda e: e.matmul(pbc[:, 0:128], lhsT=ident_f[0:64, dh:dh + 1].to_broadcast([64, 128]),
                                                      rhs=cumF[:, tsl], start=True, stop=True),
                             reads=[ident_f, cumF], writes=[pbc])
                        pbcs.append(pbc)
                    ebs = []
                    wis = []
                    for hh in range(4):
                        dh = d * 32 + g * 4 + hh
                        gc["w"] += 1
                        wi = gc["w"] % NWS
                        wis.append(wi)
                        pbc = pbcs[hh]
                        s.op("dve", lambda e: e.tensor_scalar(out=w_d[wi][:], in0=pbc[:, 0:128], scalar1=cumTok[:, ci, dh:dh + 1],
                                                              scalar2=0.0, op0=ALU.subtract, op1=ALU.min),
                             reads=[pbc, cumTok], writes=[w_d[wi]])
                        s.op("act", lambda e: e.activation(out=w_eb[wi][:], in_=pbc[:, 0:128], func=AF.Exp),
                             reads=[pbc], writes=[w_eb[wi]])
                        s.op("pool", lambda e: e.tensor_scalar(out=w_bh[wi][:], in0=BTok[:, ci, :], scalar1=ehTok[:, ci, dh:dh + 1],
                                                               scalar2=None, op0=ALU.mult), reads=[BTok, ehTok], writes=[w_bh[wi]])
                        ebs.append(w_eb[wi])
                    for hh in range(4):
                        dh = d * 32 + g * 4 + hh
                        wi = wis[hh]
                        s.op("act", lambda e: e.activation(out=w_e[wi][:], in_=w_d[wi][:], func=AF.Exp),
                             reads=[w_d[wi]], writes=[w_e[wi]])
                        s.op("pool", lambda e: e.tensor_tensor(out=w_ct[wi][:], in0=w_eb[wi][:], in1=Cg[:, tsl], op=ALU.mult),
                             reads=[w_eb[wi], Cg], writes=[w_ct[wi]])
                    for hh in range(4):
                        dh = d * 32 + g * 4 + hh
                        wi = wis[hh]
                        s.op("dve", lambda e: e.scalar_tensor_tensor(out=w_dcb[wi][:], in0=w_e[wi][:],
                                                                     scalar=dtTok[:, ci, dh:dh + 1], in1=cb[:],
                                                                     op0=ALU.mult, op1=ALU.mult),
                             reads=[w_e[wi], dtTok, cb], writes=[w_dcb[wi]])
                    py = gbank()
                    pst = gbank()
                    for hh in range(4):
                        wi = wis[hh]
                        hc = slice(hh * 64, (hh + 1) * 64)
                        s.op("pe", lambda e: e.matmul(pst[:, hc], lhsT=w_bh[wi][:], rhs=XTok[:, ci, hc], start=True, stop=True),
                             reads=[w_bh[wi], XTok], writes=[pst])
                    for hh in range(4):
                        wi = wis[hh]
                        hc = slice(hh * 64, (hh + 1) * 64)
                        s.op("pe", lambda e: e.matmul(py[:, hc], lhsT=w_dcb[wi][:], rhs=XTok[:, ci, hc], start=True, stop=False),
                             reads=[w_dcb[wi], XTok], writes=[py])
                        s.op("pe", lambda e: e.matmul(py[:, hc], lhsT=w_ct[wi][:], rhs=SSb[:, hc], start=False, stop=True),
                             reads=[w_ct[wi], SSb], writes=[py])
                    if ci >= 2:
                        if d == 0:
                            gev(yacc, yacc[:, ci - 2, :], py, py[:, 0:256])
                        else:
                            s.op("dve", lambda e: e.tensor_tensor(out=yacc[:, ci - 2, :], in0=py[:, 0:256], in1=yacc[:, ci - 2, :],
                                                                  op=ALU.add), reads=[py, yacc], writes=[yacc])
                    col = 127 if d == 0 else 0
                    for hh in range(4):
                        hc = slice(hh * 64, (hh + 1) * 64)
                        s.op("dve", lambda e: e.scalar_tensor_tensor(out=SSf[:, hc], in0=SSf[:, hc], scalar=ebs[hh][:, col:col + 1],
                                                                     in1=pst[:, hc], op0=ALU.mult, op1=ALU.add),
                             reads=[SSf, ebs[hh], pst], writes=[SSf])
                    s.op("act", lambda e: e.copy(out=SSb[:], in_=SSf[:]), reads=[SSf], writes=[SSb])
            gsl = slice(g * 256, (g + 1) * 256)
            for i in range(2):
                s.dma("pool", zg_ap[:, i, :], colsT[ZR0 + g * 256 + i * 128:ZR0 + g * 256 + (i + 1) * 128, CTX:TT],
                      reads=[colsT], writes=[gA])
            s.dma("act", dbc[:], dbc_in[0:1, gsl].partition_broadcast(128), reads=[dbc_in], writes=[dbc])
            s.dma("act", nwb[:], nwb_in[0:1, gsl].partition_broadcast(128), reads=[nwb_in], writes=[nwb])
            for lc in range(32):
                y_, z_, o_, st_ = fy[lc % 2], fz[lc % 2], fo[lc % 2], fst[lc % 2]
                s.op("pool", lambda e: e.tensor_tensor(out=y_[:], in0=XTok[:, lc + 2, :], in1=dbc[:], op=ALU.mult),
                     reads=[XTok, dbc], writes=[y_])
                s.op("dve", lambda e: e.tensor_tensor(out=y_[:], in0=y_[:], in1=yacc[:, lc, :], op=ALU.add),
                     reads=[y_, yacc], writes=[y_])
                for i in range(2):
                    s.op("pe", lambda e: e.transpose(out=ptrb[:, i * 128:(i + 1) * 128], in_=zg_ap[:, i, lc * 128:(lc + 1) * 128],
                                                     identity=ident[:]), reads=[gA, ident], writes=[ptr])
                s.op("act", lambda e: e.activation(out=z_[:], in_=ptrb[:, 0:256], func=AF.Silu), reads=[ptr], writes=[z_])
                s.op("dve", lambda e: e.tensor_tensor(out=y_[:], in0=y_[:], in1=z_[:], op=ALU.mult), reads=[y_, z_], writes=[y_])
                s.op("act", lambda e: e.activation(out=fsq[:], in_=y_[:], func=AF.Square, accum_out=st_[:, 0:1]),
                     reads=[y_], writes=[fsq, st_])
                s.op("dve", lambda e: e.tensor_scalar(out=st_[:, 1:2], in0=st_[:, 0:1], scalar1=1.0 / 256, scalar2=EPS,
                                                      op0=ALU.mult, op1=ALU.add), reads=[st_], writes=[st_])
                s.op("act", lambda e: e.activation(out=st_[:, 1:2], in_=st_[:, 1:2], func=AF.Sqrt), reads=[st_], writes=[st_])
                s.op("dve", lambda e: e.reciprocal(out=st_[:, 1:2], in_=st_[:, 1:2]), reads=[st_], writes=[st_])
                s.op("dve", lambda e: e.scalar_tensor_tensor(out=o_[:], in0=y_[:], scalar=st_[:, 1:2], in1=nwb[:],
                                                             op0=ALU.mult, op1=ALU.mult), reads=[y_, st_, nwb], writes=[o_])
                for i in range(2):
                    s.op("pe", lambda e: e.transpose(out=ptrb[:, 256 + i * 128:256 + (i + 1) * 128], in_=o_[:, i * 128:(i + 1) * 128],
                                                     identity=ident[:]), reads=[o_, ident], writes=[ptr])
                gev(xg, ysg_ap[:, :, lc * 128:(lc + 1) * 128], ptr, ptrb[:, 256:512].rearrange("p (i t) -> p i t", i=2))
            for i in range(2):
                r0 = RW + g * 256 + i * 128
                s.dma("sp", yT[r0:r0 + 128, :], ysg_ap[:, i, :], reads=[xg], writes=[yT])
            if dbg == "ssd1":
                break
        s.barrier()
    s.stack = st


    convert_tables(128)
    HT = 2048
    xh = din("xh", [HT, D])
    selc_in = din("selc", [128, 2])
    w_out_r = din("w_out_r", [8, 128, 32 * 512])
    norm2_g = din("norm2_g", [1, D])
    final_g = din("final_g", [1, D])
    wq_r = din("wq_r", [16, 128, 32 * 128])
    kT_in = din("kT", [128, 2, 8, 128])
    h1D = s.dram("h1D", [HT, D], F32, kind=("ExternalOutput" if dbg in ("h1", "h1t") else "Internal"))
    h2D = s.dram("h2D", [HT, D], F32)
    xn2T = s.dram("xn2T", [D, HT], BF16, kind=("ExternalOutput" if dbg in ("h1", "h1t") else "Internal"))
    WrD = s.dram("WrD", [256, 128, 128], BF16)
    OhD = s.dram("OhD", [256, 128, 128], BF16)
    outD = s.dram("out", [HT, D], F32, kind="ExternalOutput")
    selc = s.sb("selc_sb", [128, 2], F32)
    s.dma("sp", selc[:], selc_in[:, :], reads=[selc_in], writes=[selc])

    with ExitStack() as ph1:
        s.stack = ph1
        wsl = [s.sb("wsl%d" % i, [128, 32, 512], BF16) for i in range(2)]
        yTb = s.sb("yTb", [128, 32, 1024], BF16)
        la = [s.sb("la%d" % i, [128, 1024], BF16) for i in range(2)]
        lb = [s.sb("lb%d" % i, [128, 1024], BF16) for i in range(2)]
        gt1 = s.sb("gt1", [128, D], F32)
        xr = [s.sb("xr%d" % i, [128, 512], F32) for i in range(3)]
        orow = [s.sb("orow%d" % i, [128, 512], F32) for i in range(3)]
        s.dma("act", gt1[:], modx[:, 2 * D:3 * D], reads=[modx], writes=[gt1])
        wi = 0
        oi = 0
        for tb in range(2):
            for kc in range(32):
                a_, b_ = la[kc % 2], lb[kc % 2]
                s.dma("sp", a_[:], yT[kc * 128:(kc + 1) * 128, tb * 1024:(tb + 1) * 1024], reads=[yT], writes=[a_])
                s.dma("act", b_[:], yT[kc * 128:(kc + 1) * 128, HT + tb * 1024:HT + (tb + 1) * 1024], reads=[yT], writes=[b_])
                s.op("dve", lambda e: e.tensor_scalar(out=yTb[:, kc, :], in0=a_[:], scalar1=selc[:, 0:1], scalar2=None, op0=ALU.mult),
                     reads=[a_, selc], writes=[yTb])
                s.op("dve", lambda e: e.scalar_tensor_tensor(out=yTb[:, kc, :], in0=b_[:], scalar=selc[:, 1:2], in1=yTb[:, kc, :],
                                                             op0=ALU.mult, op1=ALU.add), reads=[b_, selc, yTb], writes=[yTb])
            for dg in range(8):
                w_ = wsl[wi % 2]; wi += 1
                s.dma("pool", w_[:], w_out_r[dg].rearrange("p (k c) -> p k c", k=32), reads=[w_out_r], writes=[w_])
                for tl in range(8):
                    pp = pb[oi % 4]
                    for kc in range(32):
                        s.op("pe", lambda e: e.matmul(pp[:, :], lhsT=yTb[:, kc, tl * 128:(tl + 1) * 128], rhs=w_[:, kc, :],
                                                      start=(kc == 0), stop=(kc == 31)), reads=[yTb, w_], writes=[pp])
                    r0 = tb * 1024 + tl * 128
                    x_ = xr[oi % 3]; o_ = orow[oi % 3]; oi += 1
                    s.dma("act", x_[:], xh[r0:r0 + 128, dg * 512:(dg + 1) * 512], reads=[xh], writes=[x_])
                    s.op("dve", lambda e: e.tensor_tensor(out=o_[:], in0=pp[:, :], in1=gt1[:, dg * 512:(dg + 1) * 512], op=ALU.mult),
                         reads=[pp, gt1], writes=[o_])
                    s.op("pool", lambda e: e.tensor_tensor(out=o_[:], in0=o_[:], in1=x_[:], op=ALU.add), reads=[o_, x_], writes=[o_])
                    s.dma("sp", h1D[r0:r0 + 128, dg * 512:(dg + 1) * 512], o_[:], reads=[o_], writes=[h1D])
        s.barrier()
    s.stack = st

    with ExitStack() as ph2:
        s.stack = ph2
        G2 = s.sb("G2", [128, D], F32)
        SH2 = s.sb("SH2", [128, D], F32)
        g2b_ = s.sb("g2bb", [128, D], F32)
        hx = [s.sb("hx%d" % i, [128, D], F32) for i in range(2)]
        hn = [s.sb("hn%d" % i, [128, D], BF16) for i in range(2)]
        sq2 = s.sb("sq2", [128, D], BF16)
        ss2 = [s.sb("ss2_%d" % i, [128, 2], F32) for i in range(2)]
        nT2 = [s.sb("nT2_%d" % i, [128, 32, 512], BF16) for i in range(2)]
        s.dma("sp", g2b_[:], norm2_g[0:1, :].partition_broadcast(128), reads=[norm2_g], writes=[g2b_])
        s.dma("sp", G2[:], modx[:, 4 * D:5 * D], reads=[modx], writes=[G2])
        s.dma("act", SH2[:], modx[:, 3 * D:4 * D], reads=[modx], writes=[SH2])
        s.op("dve", lambda e: e.scalar_tensor_tensor(out=G2[:], in0=G2[:], scalar=1.0, in1=g2b_[:], op0=ALU.add, op1=ALU.mult),
             reads=[G2, g2b_], writes=[G2])
        ev = 0
        for tl in range(16):
            x_, n_, s_ = hx[tl % 2], hn[tl % 2], ss2[tl % 2]
            nt = nT2[(tl // 4) % 2]
            s.dma("sp", x_[:], h1D[tl * 128:(tl + 1) * 128, :], reads=[h1D], writes=[x_])
            s.op("act", lambda e: e.activation(out=sq2[:], in_=x_[:], func=AF.Square, accum_out=s_[:, 0:1]), reads=[x_], writes=[sq2, s_])
            s.op("dve", lambda e: e.tensor_scalar(out=s_[:, 1:2], in0=s_[:, 0:1], scalar1=1.0 / D, scalar2=EPS, op0=ALU.mult, op1=ALU.add),
                 reads=[s_], writes=[s_])
            s.op("act", lambda e: e.activation(out=s_[:, 1:2], in_=s_[:, 1:2], func=AF.Sqrt), reads=[s_], writes=[s_])
            s.op("dve", lambda e: e.reciprocal(out=s_[:, 1:2], in_=s_[:, 1:2]), reads=[s_], writes=[s_])
            s.op("dve", lambda e: e.scalar_tensor_tensor(out=x_[:], in0=x_[:], scalar=s_[:, 1:2], in1=G2[:], op0=ALU.mult, op1=ALU.mult),
                 reads=[x_, s_, G2], writes=[x_])
            s.op("pool", lambda e: e.tensor_tensor(out=n_[:], in0=x_[:], in1=SH2[:], op=ALU.add), reads=[x_, SH2], writes=[n_])
            for q4 in range(4):
                pt = pb[4 + (ev % 2)]; ev += 1
                ptb = pt[:, :].bitcast(BF16)
                for j in range(8):
                    kc = q4 * 8 + j
                    s.op("pe", lambda e: e.transpose(out=ptb[:, j * 128:(j + 1) * 128], in_=n_[:, kc * 128:(kc + 1) * 128], identity=ident[:]),
                         reads=[n_, ident], writes=[pt])
                dst = nt[:, q4 * 8:(q4 + 1) * 8, (tl % 4) * 128:(tl % 4 + 1) * 128]
                srcp = ptb.rearrange("p (j t) -> p j t", j=8)
                if q4 % 2 == 0:
                    s.op("act", lambda e: e.copy(out=dst, in_=srcp), reads=[pt], writes=[nt])
                else:
                    s.op("dve", lambda e: e.tensor_copy(out=dst, in_=srcp), reads=[pt], writes=[nt])
            if tl % 4 == 3:
                t0 = (tl // 4) * 512
                for kq in range(4):
                    s.dma("sp", xn2T[kq * 1024:(kq + 1) * 1024, t0:t0 + 512].rearrange("(k p) t -> p k t", p=128),
                          nt[:, kq * 8:(kq + 1) * 8, :], reads=[nt], writes=[xn2T])
        s.barrier()
    s.stack = st

    if dbg not in ("h1", "h1t"):
      with ExitStack() as pp_:
        s.stack = pp_
        TB = 256
        xb = s.sb("xb", [128, 32, TB], BF16)
        WT = s.sb("WT", [128, 128, TB], BF16)
        wb3 = [s.sb("wb3_%d" % i, [128, 4096], BF16) for i in range(3)]
        qT = s.sb("qT", [128, 16, TB], BF16)
        kT = s.sb("kT_sb", [128, 2, 8, 128], BF16)
        sc = s.sb("sc", [128, 16, 128], F32)
        vv = s.sb("vv", [128, 16, 16], F32)
        cand = s.sb("cand", [128, 8, 256], F32)
        ec = s.sb("ec", [128, 8, 256], F32)
        E2 = s.sb("E2", [128, 8, 128], F32)
        tmpk = s.sb("tmpk", [128, 256], F32)
        m8 = s.sb("m8", [128, 8], F32)
        th = s.sb("th", [128, 8], F32)
        negm = s.sb("negm", [128, 8, 3], F32)
        Zs = s.sb("Zs", [128, 8], F32)
        c1 = s.sb("c1", [128, 8, 16], F32)
        thr = s.sb("thr", [128, 8, 16], F32)
        wst = [s.sb("wst%d" % i, [128, 16, 128], BF16) for i in range(2)]
        ost = [s.sb("ost%d" % i, [128, 16, 128], BF16) for i in range(2)]
        Wk = s.sb("Wk", [128, 16, 128], BF16)
        Ok = s.sb("Ok", [128, 16, 128], BF16)
        gl = [s.sb("gl%d" % i, [128, TB], BF16) for i in range(2)]
        hp_ = [s.sb("hp%d" % i, [128, 512], F32) for i in range(3)]
        gp_ = [s.sb("gp%d" % i, [128, 512], F32) for i in range(2)]
        op_ = [s.sb("op%d" % i, [128, 512], F32) for i in range(3)]
        s.dma("pool", kT[:], kT_in[:, :, :, :], reads=[kT_in], writes=[kT])
        pc = {"w": 0, "bank": 0, "ev": 0, "o": 0}

        def pbank():
            pc["bank"] += 1
            return pb[pc["bank"] % 8]

        def wbuf():
            pc["w"] += 1
            return wb3[pc["w"] % 3]

        def pev(dst_t, dst_ap, src_t, src_ap):
            pc["ev"] += 1
            if pc["ev"] % 2 == 0:
                s.op("act", lambda e: e.copy(out=dst_ap, in_=src_ap), reads=[src_t], writes=[dst_t])
            else:
                s.op("dve", lambda e: e.tensor_copy(out=dst_ap, in_=src_ap), reads=[src_t], writes=[dst_t])

        NBLK = HT // TB if dbg != "peer1" else 1
        for blk_i in range(NBLK):
            t0 = blk_i * TB
            for kq in range(4):
                s.dma("sp", xb[:, kq * 8:(kq + 1) * 8, :],
                      xn2T[kq * 1024:(kq + 1) * 1024, t0:t0 + TB].rearrange("(k p) t -> p k t", p=128), reads=[xn2T], writes=[xb])
            for qc in range(16):
                w_ = wbuf()
                wv = w_[:, :].rearrange("p (k c) -> p k c", k=32)
                s.dma("pool", w_[:], wq_r[qc], reads=[wq_r], writes=[w_])
                pp = pbank()
                for kc in range(32):
                    s.op("pe", lambda e: e.matmul(pp[:, 0:TB], lhsT=wv[:, kc, :], rhs=xb[:, kc, :], start=(kc == 0), stop=(kc == 31)),
                         reads=[w_, xb], writes=[pp])
                pev(qT, qT[:, qc, :], pp, pp[:, 0:TB])
            for tile in range(TB // 128):
                tt = tile * 128
                for q4 in range(4):
                    pp = pbank()
                    for j in range(4):
                        hw = q4 * 4 + j
                        s.op("pe", lambda e: e.matmul(pp[:, j * 128:(j + 1) * 128], lhsT=qT[:, hw, tt:tt + 128],
                                                      rhs=kT[:, hw % 2, hw // 2, :], start=True, stop=True), reads=[qT, kT], writes=[pp])
                    pev(sc, sc[:, q4 * 4:(q4 + 1) * 4, :], pp, pp[:, :].rearrange("p (j k) -> p j k", j=4))
                for hw in range(16):
                    s.op("dve", lambda e: e.max(out=vv[:, hw, 0:8], in_=sc[:, hw, :]), reads=[sc], writes=[vv])
                    s.op("dve", lambda e: e.match_replace(out=tmpk[:, 0:128], in_to_replace=vv[:, hw, 0:8], in_values=sc[:, hw, :],
                                                          imm_value=-1e30), reads=[sc, vv], writes=[tmpk])
                    s.op("dve", lambda e: e.max(out=vv[:, hw, 8:16], in_=tmpk[:, 0:128]), reads=[tmpk], writes=[vv])
                v4 = vv[:, :, :].rearrange("p (h w) r -> p h w r", w=2)
                cand4 = cand[:, :, :].rearrange("p h (i j) -> p h i j", j=16)
                s.op("dve", lambda e: e.tensor_tensor(out=cand4, in0=v4[:, :, 0, :].unsqueeze(3).to_broadcast([128, 8, 16, 16]),
                                                      in1=v4[:, :, 1, :].unsqueeze(2).to_broadcast([128, 8, 16, 16]), op=ALU.add),
                     reads=[vv], writes=[cand])
                for h in range(8):
                    s.op("dve", lambda e: e.max(out=m8[:], in_=cand[:, h, :]), reads=[cand], writes=[m8])
                    s.op("dve", lambda e: e.match_replace(out=tmpk[:], in_to_replace=m8[:], in_values=cand[:, h, :], imm_value=-1e30),
                         reads=[cand, m8], writes=[tmpk])
                    s.op("dve", lambda e: e.max(out=m8[:], in_=tmpk[:]), reads=[tmpk], writes=[m8])
                    s.op("dve", lambda e: e.tensor_copy(out=th[:, h:h + 1], in_=m8[:, 7:8]), reads=[m8], writes=[th])
                s.op("dve", lambda e: e.tensor_scalar(out=negm[:, :, 0:2], in0=v4[:, :, :, 0], scalar1=-1.0, scalar2=None, op0=ALU.mult),
                     reads=[vv], writes=[negm])
                s.op("dve", lambda e: e.tensor_tensor(out=negm[:, :, 2], in0=negm[:, :, 0], in1=negm[:, :, 1], op=ALU.add),
                     reads=[negm], writes=[negm])
                for h in range(8):
                    s.op("act", lambda e: e.activation(out=ec[:, h, :], in_=cand[:, h, :], func=AF.Exp, bias=negm[:, h, 2:3]),
                         reads=[cand, negm], writes=[ec])
                    s.op("dve", lambda e: e.scalar_tensor_tensor(out=tmpk[:], in0=cand[:, h, :], scalar=th[:, h:h + 1], in1=ec[:, h, :],
                                                                 op0=ALU.is_ge, op1=ALU.mult, accum_out=Zs[:, h:h + 1]),
                         reads=[cand, th, ec], writes=[tmpk, Zs])
                    s.op("act", lambda e: e.activation(out=E2[:, h, :], in_=sc[:, 2 * h + 1, :], func=AF.Exp, bias=negm[:, h, 1:2]),
                         reads=[sc, negm], writes=[E2])
                s.op("dve", lambda e: e.reciprocal(out=Zs[:], in_=Zs[:]), reads=[Zs], writes=[Zs])
                s.op("dve", lambda e: e.tensor_tensor(out=c1[:], in0=v4[:, :, 0, :], in1=negm[:, :, 0:1].to_broadcast([128, 8, 16]), op=ALU.add),
                     reads=[vv, negm], writes=[c1])
                s.op("act", lambda e: e.activation(out=c1[:], in_=c1[:], func=AF.Exp), reads=[c1], writes=[c1])
                s.op("dve", lambda e: e.tensor_tensor(out=c1[:], in0=c1[:], in1=Zs[:, :].unsqueeze(2).to_broadcast([128, 8, 16]), op=ALU.mult),
                     reads=[c1, Zs], writes=[c1])
                s.op("dve", lambda e: e.tensor_tensor(out=thr[:], in0=th[:, :].unsqueeze(2).to_broadcast([128, 8, 16]), in1=v4[:, :, 0, :],
                                                      op=ALU.subtract), reads=[th, vv], writes=[thr])
                for h in range(8):
                    ws_, os_ = wst[h % 2], ost[h % 2]
                    for r in range(16):
                        s.op("dve", lambda e: e.scalar_tensor_tensor(out=ws_[:, r, :], in0=sc[:, 2 * h + 1, :], scalar=thr[:, h, r:r + 1],
                                                                     in1=E2[:, h, :], op0=ALU.is_ge, op1=ALU.mult),
                             reads=[sc, thr, E2], writes=[ws_])
                        s.op("pool", lambda e: e.tensor_scalar(out=os_[:, r, :], in0=sc[:, 2 * h, :], scalar1=vv[:, 2 * h, r:r + 1],
                                                               scalar2=c1[:, h, r:r + 1], op0=ALU.is_equal, op1=ALU.mult),
                             reads=[sc, vv, c1], writes=[os_])
                    s.dma("sp", WrD[tt:tt + 128, h * 16:(h + 1) * 16, :], ws_[:], reads=[ws_], writes=[WrD])
                    s.dma("act", OhD[tt:tt + 128, h * 16:(h + 1) * 16, :], os_[:], reads=[os_], writes=[OhD])
                for sub in range(8):
                    tk = tt + sub * 16
                    s.dma("sp", Wk[:], WrD[tk:tk + 16].rearrange("t k e -> k t e"), reads=[WrD], writes=[Wk])
                    s.dma("act", Ok[:], OhD[tk:tk + 16].rearrange("t k e -> k t e"), reads=[OhD], writes=[Ok])
                    for q in range(4):
                        pp = pbank()
                        for j in range(4):
                            t = q * 4 + j
                            s.op("pe", lambda e: e.matmul(pp[:, j * 128:(j + 1) * 128], lhsT=Wk[:, t, :], rhs=Ok[:, t, :], start=True, stop=True),
                                 reads=[Wk, Ok], writes=[pp])
                        pev(WT, WT[:, :, tk + q * 4:tk + q * 4 + 4], pp, pp[:, :].rearrange("p (t e) -> p e t", t=4))
            for e1 in range(128):
                w_ = wbuf()
                wv = w_[:, :].rearrange("p (k c) -> p k c", k=32)
                s.dma("sp" if e1 % 2 == 0 else "act", w_[:], uTb[e1], reads=[uTb], writes=[w_])
                pp = pbank()
                for kc in range(32):
                    s.op("pe", lambda e: e.matmul(pp[:, 0:TB], lhsT=wv[:, kc, :], rhs=xb[:, kc, :], start=(kc == 0), stop=(kc == 31)),
                         reads=[w_, xb], writes=[pp])
                g_ = gl[e1 % 2]
                s.op("act", lambda e: e.activation(out=g_[:], in_=pp[:, 0:TB], func=AF.Gelu), reads=[pp], writes=[g_])
                s.op("dve", lambda e: e.tensor_tensor(out=WT[:, e1, :], in0=WT[:, e1, :], in1=g_[:], op=ALU.mult), reads=[WT, g_], writes=[WT])
            for dh in range(2):
                for e1 in range(128):
                    w_ = wbuf()
                    s.dma("sp" if e1 % 2 == 0 else "act", w_[:, 0:2048], vb[e1 * 128:(e1 + 1) * 128, dh * 2048:(dh + 1) * 2048], reads=[vb], writes=[w_])
                    for tile in range(2):
                        for dg in range(4):
                            pp = pb[tile * 4 + dg]
                            s.op("pe", lambda e: e.matmul(pp[:, :], lhsT=WT[:, e1, tile * 128:(tile + 1) * 128], rhs=w_[:, dg * 512:(dg + 1) * 512],
                                                          start=(e1 == 0), stop=(e1 == 127)), reads=[WT, w_], writes=[pp])
                for dg in range(4):
                    c0 = dh * 2048 + dg * 512
                    g2_ = gp_[dg % 2]
                    s.dma("act", g2_[:], modx[:, 5 * D + c0:5 * D + c0 + 512], reads=[modx], writes=[g2_])
                    for tile in range(2):
                        pp = pb[tile * 4 + dg]
                        r0 = t0 + tile * 128
                        h_ = hp_[pc["o"] % 3]; o_ = op_[pc["o"] % 3]; pc["o"] += 1
                        s.dma("sp", h_[:], h1D[r0:r0 + 128, c0:c0 + 512], reads=[h1D], writes=[h_])
                        s.op("dve", lambda e: e.tensor_tensor(out=o_[:], in0=pp[:, :], in1=g2_[:], op=ALU.mult), reads=[pp, g2_], writes=[o_])
                        s.op("pool", lambda e: e.tensor_tensor(out=o_[:], in0=o_[:], in1=h_[:], op=ALU.add), reads=[o_, h_], writes=[o_])
                        s.dma("sp", h2D[r0:r0 + 128, c0:c0 + 512], o_[:], reads=[o_], writes=[h2D])
        s.barrier()
      s.stack = st

      with ExitStack() as pf:
        s.stack = pf
        fgb = s.sb("fgb", [128, D], F32)
        fx = [s.sb("fx%d" % i, [128, D], F32) for i in range(2)]
        fsq2 = s.sb("fsq2", [128, D], BF16)
        fs = [s.sb("fs%d" % i, [128, 2], F32) for i in range(2)]
        s.dma("sp", fgb[:], final_g[0:1, :].partition_broadcast(128), reads=[final_g], writes=[fgb])
        for tl in range(16 if dbg != "peer1" else 2):
            x_, s_ = fx[tl % 2], fs[tl % 2]
            s.dma("sp", x_[:], h2D[tl * 128:(tl + 1) * 128, :], reads=[h2D], writes=[x_])
            s.op("act", lambda e: e.activation(out=fsq2[:], in_=x_[:], func=AF.Square, accum_out=s_[:, 0:1]), reads=[x_], writes=[fsq2, s_])
            s.op("dve", lambda e: e.tensor_scalar(out=s_[:, 1:2], in0=s_[:, 0:1], scalar1=1.0 / D, scalar2=EPS, op0=ALU.mult, op1=ALU.add),
                 reads=[s_], writes=[s_])
            s.op("act", lambda e: e.activation(out=s_[:, 1:2], in_=s_[:, 1:2], func=AF.Sqrt), reads=[s_], writes=[s_])
            s.op("dve", lambda e: e.reciprocal(out=s_[:, 1:2], in_=s_[:, 1:2]), reads=[s_], writes=[s_])
            s.op("dve", lambda e: e.scalar_tensor_tensor(out=x_[:], in0=x_[:], scalar=s_[:, 1:2], in1=fgb[:], op0=ALU.mult, op1=ALU.mult),
                 reads=[x_, s_, fgb], writes=[x_])
            s.dma("act", outD[tl * 128:(tl + 1) * 128, :], x_[:], reads=[x_], writes=[outD])
        s.barrier()
      s.stack = st

    s.finish()
    st.close()
    return nc


def make_shared(inp):
    f = lambda a: np.ascontiguousarray(np.asarray(a, dtype=np.float32))
    m = {}
    m["w_mod"] = f(inp["w_mod"][0])
    m["b_mod"] = f(inp["b_mod"])
    m["norm1_g"] = f(inp["norm1_g"])
    w = np.zeros((D, NCOLP), np.float32)
    w[:, :NCOLS] = np.asarray(inp["w_in"][0])
    m["w_in_r"] = f(w.reshape(32, 128, NCH, 128).transpose(2, 1, 0, 3).reshape(NCH, 128, 32 * 128))
    m["ident_in"] = np.eye(128, dtype=np.float32)
    mu = np.asarray(inp["rwkv_mu"][0])
    mu_u = np.zeros((128, 54), np.float32)
    for which in range(3):
        for hp in range(16):
            mu_u[:, which * 16 + hp] = mu[which * RW + hp * 128: which * RW + (hp + 1) * 128]
    for d in range(2):
        mu_u[:96, 48 + d] = mu[6144 + 96 * d: 6144 + 96 * (d + 1)]
        mu_u[:96, 50 + d] = mu[6336 + 96 * d: 6336 + 96 * (d + 1)]
        mu_u[:, 52 + d] = mu[6528 + 128 * d: 6528 + 128 * (d + 1)]
    m["mu_u"] = mu_u
    p = np.arange(128)
    slotm = np.zeros((128, 6), np.float32)
    for k in range(4):
        slotm[:, k] = (p % 4 == k)
    for k in range(2):
        slotm[:, 4 + k] = (p % 2 == k)
    m["slotm"] = slotm
    m["w2r"] = f(np.asarray(inp["rwkv_w2"][0]).transpose(1, 0, 2))
    m["a2r"] = f(np.asarray(inp["rwkv_a2"][0]).transpose(1, 0, 2))
    m["g2r"] = f(np.asarray(inp["rwkv_g2"][0]).reshape(2, 128, RW).transpose(1, 0, 2))
    cols = [np.asarray(inp["rwkv_w0"][0])[0], np.asarray(inp["rwkv_w0"][0])[1],
            np.asarray(inp["rwkv_a0"][0])[0], np.asarray(inp["rwkv_a0"][0])[1],
            np.asarray(inp["rwkv_k_k"][0]), np.asarray(inp["rwkv_k_a"][0]),
            np.asarray(inp["rwkv_r_k"][0]).reshape(RW), np.asarray(inp["rwkv_ln_w"][0]), np.asarray(inp["rwkv_ln_b"][0])]
    m["chp"] = f(np.stack([c.reshape(16, 128).T for c in cols], axis=2))
    o = np.ones((128, 128), np.float32)
    m["msk"] = f(np.stack([np.triu(o, 1), np.triu(o, 0), np.tril(o, -1), np.tril(o, 0)], axis=1))
    m["blk"] = f((p[:, None] // 64 == p[None, :] // 64))
    cwt = np.concatenate([np.asarray(inp["ssm_conv_w"][0]), np.asarray(inp["ssm_conv_b"])], axis=0)
    m["cw"] = f(cwt.reshape(4, 32, 128).transpose(2, 1, 0))
    m["dtp"] = f(np.stack([np.asarray(inp["ssm_dt_bias"][0]).reshape(64), np.asarray(inp["ssm_a_log"][0]).reshape(64)], axis=1))
    m["dbc"] = f(np.repeat(np.asarray(inp["ssm_d"][0]), 64)[None, :])
    m["nwb"] = f(np.asarray(inp["ssm_norm_w"]))
    m["w_out_r"] = f(np.asarray(inp["w_out"][0]).reshape(32, 128, 8, 512).transpose(2, 1, 0, 3).reshape(8, 128, 32 * 512))
    m["norm2_g"] = f(inp["norm2_g"])
    m["final_g"] = f(np.asarray(inp["final_g"])[None, :])
    m["wq_r"] = f(np.asarray(inp["peer_wq"][0]).reshape(32, 128, 16, 128).transpose(2, 1, 0, 3).reshape(16, 128, 32 * 128))
    k1 = np.asarray(inp["peer_k1"][0]); k2 = np.asarray(inp["peer_k2"][0])
    m["kT"] = f(np.stack([k1.transpose(2, 0, 1), k2.transpose(2, 0, 1)], axis=1))
    u = np.asarray(inp["peer_u"][0])
    m["uT_r"] = f(u.reshape(128, 128, 32, 128).transpose(0, 3, 2, 1).reshape(128, 128, 32 * 128))
    m["peer_v"] = f(inp["peer_v"][0])
    return m


def make_inputs(inp, core, shared=None):
    b, hf = core // 2, core % 2
    f = lambda a: np.ascontiguousarray(np.asarray(a, dtype=np.float32))
    m = dict(shared if shared is not None else make_shared(inp))
    m["xs"] = f(np.concatenate([inp["ctx"][b], inp["x"][b]], axis=0))
    cc = np.stack([np.asarray(inp["c"])[b].reshape(32, 128).T, np.asarray(inp["c_ctx"]).reshape(32, 128).T], axis=1)
    m["cc"] = f(cc)
    m["xh"] = f(np.asarray(inp["x"])[b, hf * 2048:(hf + 1) * 2048])
    sel = np.zeros((128, 2), np.float32); sel[:, hf] = 1.0
    m["selc"] = sel
    return m


def kernel(**inputs):
    nc = build_program()
    shared = make_shared(inputs)
    in_maps = [make_inputs(inputs, c, shared) for c in range(8)]
    res = run_bass_kernel_spmd(nc, in_maps, core_ids=list(range(8)))
    out = np.zeros((4, SEQ, D), np.float32)
    for c in range(8):
        b, hf = c // 2, c % 2
        out[b, hf * 2048:(hf + 1) * 2048] = res.results[c]["out"]
    return out
```
